# Optimizing a Trainium2 kernel written in Bass

```python
import jax, jax.numpy as jnp
from jax import lax
import numpy as np

D_MODEL = 4096
BATCH = 4
SEQ = 2048
DEPTH = 2

F32 = jnp.float32
CTX_LEN = 256
GRID_W = 64
ATTN_HEADS = D_MODEL // 256
ATTN_KV_HEADS = ATTN_HEADS // 4
HEAD_DIM = 128
GQA_GROUP = ATTN_HEADS // ATTN_KV_HEADS
Q_BLOCK = 128
ROPE_THETA = 10000.0
ROPE_PAIRS = HEAD_DIM // 4
HG_HEADS = D_MODEL // 256
HG_DK = 128
HG_DV = 128
HG_CHUNK = 32
ML_HEADS = D_MODEL // 512
ML_DQK = 128
ML_DV = 256
ML_CHUNK = 64
N_BRANCH = 3
BRANCH_WIDTH = ATTN_HEADS * HEAD_DIM
N_EXPERTS = 16
N_GROUPS = 4
EXPERTS_PER_GROUP = N_EXPERTS // N_GROUPS
TOP_K = 2
D_EXPERT = D_MODEL // 4
EPS = 1e-6
DEEPNORM_ALPHA = (2 * DEPTH) ** 0.25
DEEPNORM_BETA = (8 * DEPTH) ** -0.25
N_MOD = 6

IN_SEGMENTS = (
    ('attn_q', ATTN_HEADS * HEAD_DIM),
    ('attn_k', ATTN_KV_HEADS * HEAD_DIM),
    ('attn_v', ATTN_KV_HEADS * HEAD_DIM),
    ('hg_q', HG_HEADS * HG_DK),
    ('hg_f_fwd', HG_HEADS * HG_DK),
    ('hg_f_bwd', HG_HEADS * HG_DK),
    ('hg_i', HG_HEADS * HG_DV),
    ('hg_g', HG_HEADS * HG_DV),
    ('ml_q', ML_HEADS * ML_DQK),
    ('ml_k', ML_HEADS * ML_DQK),
    ('ml_v', ML_HEADS * ML_DV),
    ('ml_gates', 4 * ML_HEADS),
    ('ml_o', ML_HEADS * ML_DV),
    ('merge', N_BRANCH * D_MODEL),
)
IN_COLS = sum(width for _, width in IN_SEGMENTS)
CTX_STATE_SEGMENTS = ('attn_k', 'attn_v', 'hg_f_fwd', 'hg_f_bwd', 'hg_i', 'ml_k', 'ml_v', 'ml_gates')

kernel_name = 'hybrid_gqa_hgrn2_mlstm_moe_dit'


def segment_offsets():
    offs, start = {}, 0
    for name, width in IN_SEGMENTS:
        offs[name] = (start, start + width)
        start += width
    return offs


def in_proj(h, w, names):
    offs = segment_offsets()
    if names is None:
        y = h @ w
        return {n: y[..., a:b] for n, (a, b) in offs.items()}
    return {n: h @ w[:, offs[n][0]:offs[n][1]] for n in names}


def split_heads(a, n):
    return a.reshape(a.shape[:-1] + (n, a.shape[-1] // n))


def layer_norm(x, g, b):
    xf = x.astype(F32)
    mu = xf.mean(-1, keepdims=True)
    var = jnp.square(xf - mu).mean(-1, keepdims=True)
    return ((xf - mu) * lax.rsqrt(var + EPS) * g + b).astype(x.dtype)


def rms_norm(x, g):
    xf = x.astype(F32)
    return (xf * lax.rsqrt(jnp.mean(xf * xf, -1, keepdims=True) + EPS) * g).astype(x.dtype)


def axial_rope_tables(n_tokens):
    rows = n_tokens // GRID_W
    row_id = jnp.repeat(jnp.arange(rows), GRID_W).astype(F32)
    col_id = jnp.tile(jnp.arange(GRID_W), rows).astype(F32)
    inv_freq = ROPE_THETA ** (-jnp.arange(ROPE_PAIRS, dtype=F32) / ROPE_PAIRS)
    ang = jnp.stack([row_id[:, None] * inv_freq, col_id[:, None] * inv_freq], axis=1)
    return jnp.cos(ang), jnp.sin(ang)


def apply_axial_rope(x, cos, sin):
    b, s, h, _ = x.shape
    xr = x.astype(F32).reshape(b, s, h, 2, 2, ROPE_PAIRS)
    x1, x2 = xr[..., 0, :], xr[..., 1, :]
    c, sn = cos[None, :, None], sin[None, :, None]
    out = jnp.stack([x1 * c - x2 * sn, x2 * c + x1 * sn], axis=-2)
    return out.reshape(b, s, h, HEAD_DIM).astype(x.dtype)


def attend(q, k, v):
    b, lq = q.shape[0], q.shape[1]
    qg = q.reshape(b, lq, ATTN_KV_HEADS, GQA_GROUP, HEAD_DIM)
    s = jnp.einsum('bqhgd,bkhd->bhgqk', qg, k).astype(F32) * (HEAD_DIM ** -0.5)
    p = jax.nn.softmax(s, axis=-1).astype(v.dtype)
    o = jnp.einsum('bhgqk,bkhd->bqhgd', p, v)
    return o.reshape(b, lq, ATTN_HEADS * HEAD_DIM)


def blocked_attention(q, k, v):
    b, s = q.shape[0], q.shape[1]
    qb = q.reshape(b, s // Q_BLOCK, Q_BLOCK, ATTN_HEADS, HEAD_DIM).swapaxes(0, 1)
    o = lax.map(lambda qi: attend(qi, k, v), qb)
    return o.swapaxes(0, 1).reshape(b, s, ATTN_HEADS * HEAD_DIM)


def to_chunks(a, size):
    b, t, h = a.shape[0], a.shape[1], a.shape[2]
    a = a.reshape((b, t // size, size, h) + a.shape[3:])
    return jnp.moveaxis(a, (1, 3), (0, 2))


def from_chunks(a):
    a = jnp.moveaxis(a, (0, 2), (1, 3))
    return a.reshape((a.shape[0], a.shape[1] * a.shape[2], a.shape[3]) + a.shape[4:])


def hgrn2_scan(q, k, v, log_f, s0, reverse):
    with_out = q is not None
    if reverse:
        k, v, log_f = jnp.flip(k, 1), jnp.flip(v, 1), jnp.flip(log_f, 1)
        q = jnp.flip(q, 1) if with_out else None
    kb, vb = to_chunks(k, HG_CHUNK), to_chunks(v, HG_CHUNK)
    bcum = jnp.cumsum(to_chunks(log_f, HG_CHUNK), axis=3)
    bend = bcum[..., -1:, :]
    k_end = kb * jnp.exp(bend - bcum)
    decay = jnp.exp(bend[..., 0, :])

    def step(s, inp):
        s_new = inp[1][..., None] * s + jnp.einsum('bhsd,bhse->bhde', inp[0], inp[2])
        if not with_out:
            return s_new, None
        return s_new, jnp.einsum('bhtd,bhde->bhte', inp[3], s)

    if not with_out:
        s_fin, _ = lax.scan(step, s0, (k_end, decay, vb))
        return None, s_fin
    qb = to_chunks(q, HG_CHUNK)
    s_fin, o_inter = lax.scan(step, s0, (k_end, decay, vb, qb * jnp.exp(bcum)))
    causal = jnp.tril(jnp.ones((HG_CHUNK, HG_CHUNK), dtype=bool))
    scores = jnp.einsum('nbhtd,nbhsd->nbhts', qb * jnp.exp(bcum - bend), k_end)
    scores = jnp.where(causal, scores, 0.0)
    o = from_chunks(jnp.einsum('nbhts,nbhse->nbhte', scores, vb) + o_inter)
    if reverse:
        o = jnp.flip(o, 1)
    return o, s_fin


def hgrn2_forget(f_pre, lb):
    z = f_pre.astype(F32)
    log_f = jnp.logaddexp(jnp.log(lb), jnp.log1p(-lb) + jax.nn.log_sigmoid(z))
    k = (1.0 - lb) * jax.nn.sigmoid(-z)
    return split_heads(log_f, HG_HEADS), split_heads(k, HG_HEADS)


def hgrn2_mixer(p, pc, last, lower, norm_g):
    def query(pd):
        return split_heads(jax.nn.silu(pd['hg_q'].astype(F32)) * HG_DK ** -0.5, HG_HEADS)

    def readout(o, pd):
        y = rms_norm(o, norm_g) * jax.nn.silu(split_heads(pd['hg_g'].astype(F32), HG_HEADS))
        return y.reshape(y.shape[:2] + (-1,)).astype(pd['hg_g'].dtype)

    v = split_heads(p['hg_i'].astype(F32), HG_HEADS)
    vc = split_heads(pc['hg_i'].astype(F32), HG_HEADS)
    q = query(p)
    qc = None if last else query(pc)
    s0 = jnp.zeros((vc.shape[0], HG_HEADS, HG_DK, HG_DV), F32)
    outs, outs_c = [], []
    for d, (name, reverse) in enumerate((('hg_f_fwd', False), ('hg_f_bwd', True))):
        log_fc, kc = hgrn2_forget(pc[name], lower[d])
        oc, s_ctx = hgrn2_scan(qc, kc, vc, log_fc, s0, reverse)
        log_f, k = hgrn2_forget(p[name], lower[d])
        o, _ = hgrn2_scan(q, k, v, log_f, s_ctx, reverse)
        outs.append(o)
        outs_c.append(oc)
    y = readout(outs[0] + outs[1], p)
    yc = None if last else readout(outs_c[0] + outs_c[1], pc)
    return y, yc


def mlstm_scan(q, k, v, ig, fg, state, reverse):
    with_out = q is not None
    if reverse:
        k, v, ig, fg = jnp.flip(k, 1), jnp.flip(v, 1), jnp.flip(ig, 1), jnp.flip(fg, 1)
        q = jnp.flip(q, 1) if with_out else None
    kb, vb, ib = to_chunks(k, ML_CHUNK), to_chunks(v, ML_CHUNK), to_chunks(ig, ML_CHUNK)
    bcum = jnp.cumsum(jax.nn.log_sigmoid(to_chunks(fg, ML_CHUNK)), axis=-1)
    bend = bcum[..., -1]
    a_end = bend[..., None] - bcum + ib
    causal = jnp.tril(jnp.ones((ML_CHUNK, ML_CHUNK), dtype=bool))

    def step(carry, inp):
        c_st, n_st, m = carry
        kk, vv, bb, be, ae, ii = inp[:6]
        m_new = jnp.maximum(be + m, ae.max(-1))
        w_old = jnp.exp(be + m - m_new)
        w_s = jnp.exp(ae - m_new[..., None])
        c_new = w_old[..., None, None] * c_st + jnp.einsum('bhs,bhsd,bhse->bhde', w_s, kk, vv)
        n_new = w_old[..., None] * n_st + jnp.einsum('bhs,bhsd->bhd', w_s, kk)
        if not with_out:
            return (c_new, n_new, m_new), None
        qq = inp[6]
        dlog = bb[..., :, None] - bb[..., None, :] + ii[..., None, :]
        dlog = jnp.where(causal, dlog, -jnp.inf)
        m_t = jnp.maximum(bb + m[..., None], dlog.max(-1))
        w_prev = jnp.exp(bb + m[..., None] - m_t)
        sc = jnp.einsum('bhtd,bhsd->bhts', qq, kk) * jnp.exp(dlog - m_t[..., None])
        num = jnp.einsum('bhts,bhse->bhte', sc, vv) + w_prev[..., None] * jnp.einsum('bhtd,bhde->bhte', qq, c_st)
        den = sc.sum(-1) + w_prev * jnp.einsum('bhtd,bhd->bht', qq, n_st)
        hh = num / jnp.maximum(jnp.abs(den), jnp.exp(-m_t))[..., None]
        return (c_new, n_new, m_new), hh

    if not with_out:
        state, _ = lax.scan(step, state, (kb, vb, bcum, bend, a_end, ib))
        return None, state
    state, hs = lax.scan(step, state, (kb, vb, bcum, bend, a_end, ib, to_chunks(q, ML_CHUNK)))
    h = from_chunks(hs)
    if reverse:
        h = jnp.flip(h, 1)
    return h, state


def mlstm_mixer(p, pc, last, gate_bias, norm_g):
    def prep(pd):
        k = split_heads(pd['ml_k'].astype(F32), ML_HEADS) * ML_DQK ** -0.5
        v = split_heads(pd['ml_v'].astype(F32), ML_HEADS)
        g = pd['ml_gates'].astype(F32) + gate_bias
        return k, v, g.reshape(g.shape[:2] + (4, ML_HEADS))

    def readout(h, pd):
        y = rms_norm(h, norm_g) * jax.nn.sigmoid(split_heads(pd['ml_o'].astype(F32), ML_HEADS))
        return y.reshape(y.shape[:2] + (-1,)).astype(pd['ml_o'].dtype)

    k, v, g = prep(p)
    kc, vc, gc = prep(pc)
    q = split_heads(p['ml_q'].astype(F32), ML_HEADS)
    qc = None if last else split_heads(pc['ml_q'].astype(F32), ML_HEADS)
    b = kc.shape[0]
    state0 = (jnp.zeros((b, ML_HEADS, ML_DQK, ML_DV), F32), jnp.zeros((b, ML_HEADS, ML_DQK), F32),
              jnp.zeros((b, ML_HEADS), F32))
    outs, outs_c = [], []
    for d, reverse in enumerate((False, True)):
        hc, st = mlstm_scan(qc, kc, vc, gc[:, :, d], gc[:, :, 2 + d], state0, reverse)
        hl, _ = mlstm_scan(q, k, v, g[:, :, d], g[:, :, 2 + d], st, reverse)
        outs.append(hl)
        outs_c.append(hc)
    y = readout(outs[0] + outs[1], p)
    yc = None if last else readout(outs_c[0] + outs_c[1], pc)
    return y, yc


def merge_branches(gate_pre, branches, w_branch_l, w_out_l):
    gates = jax.nn.sigmoid(gate_pre.reshape(gate_pre.shape[:2] + (N_BRANCH, gate_pre.shape[-1] // N_BRANCH)))
    proj = jnp.einsum('btnw,nwd->btnd', jnp.stack(branches, axis=2), w_branch_l)
    return jnp.sum(gates * proj, axis=2) @ w_out_l


def token_mixers(h, hc, last, cos, sin, w_in_l, ml_bias_l, q_norm_l, k_norm_l, hg_lower_l,
                 hg_norm_l, ml_norm_l, w_branch_l, w_out_l):
    p = in_proj(h, w_in_l, None)
    pc = in_proj(hc, w_in_l, CTX_STATE_SEGMENTS if last else None)
    q = apply_axial_rope(rms_norm(split_heads(p['attn_q'], ATTN_HEADS), q_norm_l), cos, sin)
    k = apply_axial_rope(rms_norm(split_heads(p['attn_k'], ATTN_KV_HEADS), k_norm_l), cos, sin)
    v = split_heads(p['attn_v'], ATTN_KV_HEADS)
    kc = rms_norm(split_heads(pc['attn_k'], ATTN_KV_HEADS), k_norm_l)
    vc = split_heads(pc['attn_v'], ATTN_KV_HEADS)
    y_a = blocked_attention(q, jnp.concatenate([kc, k], axis=1), jnp.concatenate([vc, v], axis=1))
    y_b, yc_b = hgrn2_mixer(p, pc, last, hg_lower_l, hg_norm_l)
    y_c, yc_c = mlstm_mixer(p, pc, last, ml_bias_l, ml_norm_l)
    y = merge_branches(p['merge'], (y_a, y_b, y_c), w_branch_l, w_out_l)
    if last:
        return y, None
    qc = rms_norm(split_heads(pc['attn_q'], ATTN_HEADS), q_norm_l)
    yc_a = attend(qc, kc, vc)
    yc = merge_branches(pc['merge'], (yc_a, yc_b, yc_c), w_branch_l, w_out_l)
    return y, yc


def moe_ffn(t, w_router, b_router, w_gate, w_up, w_down):
    probs = jax.nn.softmax((t @ w_router).astype(F32) + b_router, axis=-1)
    grouped = probs.reshape(-1, N_GROUPS, EXPERTS_PER_GROUP)
    group_score = lax.top_k(grouped, TOP_K)[0].sum(-1)
    best = jnp.argmax(group_score, axis=-1)
    in_group = jnp.take_along_axis(grouped, best[:, None, None], axis=1)[:, 0]
    top_w, top_i = lax.top_k(in_group, TOP_K)
    top_w = top_w / top_w.sum(-1, keepdims=True)
    expert_id = best[:, None] * EXPERTS_PER_GROUP + top_i
    combine = jnp.sum(jax.nn.one_hot(expert_id, N_EXPERTS, dtype=F32) * top_w[..., None], axis=1).astype(t.dtype)
    out = jnp.zeros_like(t)
    for e in range(N_EXPERTS):
        hid = jax.nn.silu(t @ w_gate[e]) * (t @ w_up[e])
        out = out + combine[:, e:e + 1] * (hid @ w_down[e])
    return out


def setup_inputs(seed: int = 0) -> dict:
    key = jax.random.key(seed)
    ks = iter(jax.random.split(key, 32))

    def nrm(shape, scale):
        return jax.random.normal(next(ks), shape, F32) * scale

    d = D_MODEL
    gate_bias = jnp.concatenate([nrm((DEPTH, 2 * ML_HEADS), 0.1),
                                 3.0 + nrm((DEPTH, 2 * ML_HEADS), 0.5)], -1)
    return {
        'x': nrm((BATCH, SEQ, d), 1.0),
        'c': nrm((BATCH, d), 1.0),
        'ctx': nrm((BATCH, CTX_LEN, d), 1.0),
        'c_ctx': nrm((d,), 1.0),
        'w_mod': nrm((DEPTH, d, N_MOD * d), 0.5 * d ** -0.5),
        'b_mod': nrm((DEPTH, N_MOD * d), 0.02),
        'w_in': nrm((DEPTH, d, IN_COLS), d ** -0.5),
        'ml_gate_bias': gate_bias,
        'attn_q_norm': 1.0 + nrm((DEPTH, HEAD_DIM), 0.02),
        'attn_k_norm': 1.0 + nrm((DEPTH, HEAD_DIM), 0.02),
        'hg_lb_logits': nrm((2, DEPTH, HG_HEADS * HG_DK), 0.5),
        'hg_norm': 1.0 + nrm((DEPTH, HG_DV), 0.02),
        'ml_norm': 1.0 + nrm((DEPTH, ML_DV), 0.02),
        'w_branch': nrm((DEPTH, N_BRANCH, BRANCH_WIDTH, d), BRANCH_WIDTH ** -0.5 * DEEPNORM_BETA),
        'w_out': nrm((DEPTH, d, d), d ** -0.5 * DEEPNORM_BETA),
        'ln1_g': 1.0 + nrm((DEPTH, d), 0.02),
        'ln1_b': nrm((DEPTH, d), 0.02),
        'ln2_g': 1.0 + nrm((DEPTH, d), 0.02),
        'ln2_b': nrm((DEPTH, d), 0.02),
        'w_router': nrm((d, N_EXPERTS), d ** -0.5),
        'b_router': nrm((N_EXPERTS,), 0.01),
        'w_exp_gate': nrm((DEPTH, N_EXPERTS, d, D_EXPERT), d ** -0.5),
        'w_exp_up': nrm((DEPTH, N_EXPERTS, d, D_EXPERT), d ** -0.5),
        'w_exp_down': nrm((DEPTH, N_EXPERTS, D_EXPERT, d), D_EXPERT ** -0.5 * DEEPNORM_BETA),
    }


def reference(x, c, ctx, c_ctx, w_mod, b_mod, w_in, ml_gate_bias, attn_q_norm, attn_k_norm,
              hg_lb_logits, hg_norm, ml_norm, w_branch, w_out, ln1_g, ln1_b, ln2_g, ln2_b,
              w_router, b_router, w_exp_gate, w_exp_up, w_exp_down):
    b, s, d = x.shape
    cos, sin = axial_rope_tables(s)
    lb_p = jax.nn.softmax(hg_lb_logits.astype(F32), axis=1)
    hg_lower = jnp.cumsum(lb_p, axis=1) - lb_p[:, :1]
    s_lat = jax.nn.silu(c)
    s_ctx = jax.nn.silu(c_ctx)
    for l in range(DEPTH):
        last = l == DEPTH - 1
        mod = (s_lat @ w_mod[l] + b_mod[l]).reshape(b, N_MOD, 1, d)
        n_ctx_mod = 2 if last else N_MOD
        mod_c = (s_ctx @ w_mod[l][:, :n_ctx_mod * d] + b_mod[l][:n_ctx_mod * d]).reshape(n_ctx_mod, d)
        h = x * (1.0 + mod[:, 1]) + mod[:, 0]
        hc = ctx * (1.0 + mod_c[1]) + mod_c[0]
        y, yc = token_mixers(h, hc, last, cos, sin, w_in[l], ml_gate_bias[l], attn_q_norm[l],
                             attn_k_norm[l], hg_lower[:, l], hg_norm[l], ml_norm[l], w_branch[l], w_out[l])
        x = layer_norm(DEEPNORM_ALPHA * x + mod[:, 2] * y, ln1_g[l], ln1_b[l])
        h2 = x * (1.0 + mod[:, 4]) + mod[:, 3]
        if last:
            f_lat = moe_ffn(h2.reshape(-1, d), w_router, b_router, w_exp_gate[l], w_exp_up[l],
                            w_exp_down[l]).reshape(b, s, d)
        else:
            ctx = layer_norm(DEEPNORM_ALPHA * ctx + mod_c[2] * yc, ln1_g[l], ln1_b[l])
            h2c = ctx * (1.0 + mod_c[4]) + mod_c[3]
            tokens = jnp.concatenate([h2.reshape(-1, d), h2c.reshape(-1, d)], axis=0)
            f_all = moe_ffn(tokens, w_router, b_router, w_exp_gate[l], w_exp_up[l], w_exp_down[l])
            f_lat = f_all[:b * s].reshape(b, s, d)
            f_ctx = f_all[b * s:].reshape(ctx.shape)
            ctx = layer_norm(DEEPNORM_ALPHA * ctx + mod_c[5] * f_ctx, ln2_g[l], ln2_b[l])
        x = layer_norm(DEEPNORM_ALPHA * x + mod[:, 5] * f_lat, ln2_g[l], ln2_b[l])
    return x
```

```python
import numpy as np
import concourse.bass as bass
import concourse.mybir as mybir
from concourse.bass_utils import run_bass_kernel_spmd
from contextlib import ExitStack

F32 = mybir.dt.float32
BF16 = mybir.dt.bfloat16
AF = mybir.ActivationFunctionType
ALU = mybir.AluOpType
AX = mybir.AxisListType

ENGS = ("tensor", "vector", "scalar", "gpsimd", "sync")
MAXV = 8000
NDMASEM = {"sync": 56, "gpsimd": 16, "tensor": 4, "vector": 4, "scalar": 4}
EPS = 1e-6
PRUNE_WAR = True
DMA_WINDOW = 10
SETTLE_N = 16
SETTLE_ROWS = 16
STORE_Q = "sync"
ODD_POOL = True


class Op:
    __slots__ = ("eng", "fn", "deps", "signals", "sem", "val", "is_dma", "prev_same_sem", "throttle")

    def __init__(self, eng, fn, is_dma):
        self.eng = eng
        self.fn = fn
        self.deps = []
        self.signals = False
        self.sem = None
        self.val = 0
        self.is_dma = is_dma
        self.prev_same_sem = None
        self.throttle = None


class Sched:
    def __init__(self, nc, es):
        self.nc = nc
        self.es = es
        self.ops = {e: [] for e in ENGS}
        self.last_w = {}
        self.readers = {}
        self.sems = {}
        self.dma_sems = {}
        self.dma_cnt = {e: 0 for e in ENGS}
        self.dma_last = {}
        self.dma_hist = {}
        self.final_waits = []
        self.bar = []
        self.since_bar = []

    def _newsem(self, name):
        return self.es.enter_context(self.nc.semaphore(name))

    def op(self, eng, fn, reads=(), writes=(), dma=False, odd=False):
        o = Op(eng, fn, dma)
        deps = list(self.bar)
        for r in reads:
            w = self.last_w.get(r)
            if w is not None:
                deps.append(w)
        for r in writes:
            w = self.last_w.get(r)
            if w is not None:
                deps.append(w)
            lastrd = {}
            for rd in self.readers.get(r, ()):
                if rd.is_dma or not PRUNE_WAR:
                    deps.append(rd)
                else:
                    lastrd[rd.eng] = rd
            deps.extend(lastrd.values())
        seen = set()
        for d in deps:
            if id(d) in seen:
                continue
            seen.add(id(d))
            if d.eng == "tensor" and eng == "tensor" and not d.is_dma and not dma:
                continue
            o.deps.append(d)
            d.signals = True
        for r in writes:
            self.last_w[r] = o
            self.readers[r] = []
        for r in reads:
            self.readers.setdefault(r, []).append(o)
        if dma:
            pool = eng + ("_odd" if odd else "")
            self.dma_cnt.setdefault(pool, 0)
            k = self.dma_cnt[pool] % (4 if odd else NDMASEM[eng])
            self.dma_cnt[pool] += 1
            key = (pool, k)
            hist = self.dma_hist.setdefault(eng, [])
            o.throttle = hist[-DMA_WINDOW] if len(hist) >= DMA_WINDOW else None
            hist.append(o)
            o.prev_same_sem = self.dma_last.get(key)
            self.dma_last[key] = o
            o.sem = key
            o.signals = True
        self.ops[eng].append(o)
        self.since_bar.append(o)
        return o

    def barrier(self):
        last = {}
        dmas = []
        for o in self.since_bar:
            if o.is_dma:
                dmas.append(o)
            else:
                last[o.eng] = o
        self.bar = list(last.values()) + dmas
        for o in self.bar:
            o.signals = True
        self.since_bar = []
        self.last_w = {}
        self.readers = {}

    def finish(self, ops):
        self.final_waits.extend(ops)
        for o in ops:
            o.signals = True

    def emit(self):
        nc = self.nc
        cnt = {e: 0 for e in ENGS}
        dcnt = {}
        for e in ENGS:
            for o in self.ops[e]:
                if o.is_dma:
                    dcnt[o.sem] = dcnt.get(o.sem, 0) + 1
                    o.val = 16 * dcnt[o.sem]
                    if o.sem not in self.dma_sems:
                        self.dma_sems[o.sem] = self._newsem("d%s%d" % (o.sem[0], o.sem[1]))
                elif o.signals:
                    n = cnt[e]
                    cnt[e] += 1
                    key = (e, n // MAXV)
                    if key not in self.sems:
                        self.sems[key] = self._newsem("c%s%d" % (e[:2], key[1]))
                    o.sem = key
                    o.val = n % MAXV + 1
        allsem = dict(self.sems)
        allsem.update(self.dma_sems)
        self.stats = {"compute_sems": len(self.sems), "dma_sems": len(self.dma_sems), "signals": dict(cnt),
                      "max_dma_val": max([16 * v for v in dcnt.values()] + [0])}

        def run(ename):
            def body(eng):
                seen = {}

                def wait(d):
                    if seen.get(d.sem, 0) >= d.val:
                        return
                    seen[d.sem] = d.val
                    eng.wait_ge(allsem[d.sem], d.val)

                for o in self.ops[ename]:
                    for d in o.deps:
                        wait(d)
                    if o.is_dma and o.prev_same_sem is not None:
                        wait(o.prev_same_sem)
                    if o.is_dma and o.throttle is not None:
                        wait(o.throttle)
                    ins = o.fn(eng)
                    if o.signals:
                        ins.then_inc(allsem[o.sem], 16 if o.is_dma else 1)
                if ename == "sync":
                    for d in self.final_waits:
                        wait(d)
            return body

        with nc.Block() as block:
            for e in ENGS:
                if self.ops[e] or (e == "sync" and self.final_waits):
                    getattr(block, e)(run(e))


class Cfg:
    def __init__(self, D=4096, LAT=2048, CTX=256, NB=1, DEPTH=2):
        self.D, self.LAT, self.CTX, self.NB, self.DEPTH = D, LAT, CTX, NB, DEPTH
        self.TOK = LAT + CTX
        self.KC = D // 128
        self.AH = D // 256
        self.AKV = self.AH // 4
        self.HGH = D // 256
        self.MLH = D // 512
        self.BW = self.AH * 128
        self.NE = 16
        self.DE = D // 4
        segs = [("attn_q", self.AH * 128), ("attn_k", self.AKV * 128), ("attn_v", self.AKV * 128),
                ("hg_q", self.HGH * 128), ("hg_f_fwd", self.HGH * 128), ("hg_f_bwd", self.HGH * 128),
                ("hg_i", self.HGH * 128), ("hg_g", self.HGH * 128), ("ml_q", self.MLH * 128),
                ("ml_k", self.MLH * 128), ("ml_v", self.MLH * 256), ("ml_gates", 4 * self.MLH),
                ("ml_o", self.MLH * 256), ("merge", 3 * D)]
        self.segs = segs
        self.off = {}
        s = 0
        for n, w in segs:
            self.off[n] = (s, w)
            s += w
        self.IN_COLS = s
        self.alpha = float((2 * DEPTH) ** 0.25)

    def tblocks(self, width=512):
        out = []
        t = 0
        while t < self.LAT:
            w = min(width, self.LAT - t)
            out.append((t, w, False))
            t += w
        t = self.LAT
        while t < self.TOK:
            w = min(width, self.TOK - t)
            out.append((t, w, True))
            t += w
        return out


def host_consts(cfg):
    c = {}
    ident = np.eye(128, dtype=np.float32)
    ones = np.ones((128, 128), np.float32)
    Rm = np.zeros((128, 128), np.float32)
    for a in range(2):
        for p in range(32):
            m0 = a * 64 + p
            m1 = a * 64 + 32 + p
            Rm[m1, m0] = -1.0
            Rm[m0, m1] = 1.0
    s = np.arange(128)[:, None]
    t = np.arange(128)[None, :]
    same = (s // 32) == (t // 32)
    maskF = (same & (s <= t)).astype(np.float32)
    maskB = (same & (s >= t)).astype(np.float32)
    c["cmat"] = np.concatenate([ident, ones, Rm, maskF, maskB], axis=1)
    sel = np.zeros((32, 32 * 128), np.float32)
    for k in range(32):
        sel[k, k * 128:(k + 1) * 128] = 1.0
    c["sel"] = sel
    rm = np.ones((128, cfg.TOK), np.float32)
    rm[:, ::32] = 0.0
    c["rmask"] = rm
    tt = np.arange(cfg.LAT)
    row = (tt // 64).astype(np.float32)
    col = (tt % 64).astype(np.float32)
    inv = (10000.0 ** (-np.arange(32, dtype=np.float32) / 32)).astype(np.float32)
    ang = np.stack([row[:, None] * inv, col[:, None] * inv], axis=1).astype(np.float32)
    cosT = np.zeros((128, cfg.LAT), np.float32)
    sinT = np.zeros((128, cfg.LAT), np.float32)
    for a in range(2):
        for h in range(2):
            cosT[a * 64 + h * 32:a * 64 + h * 32 + 32, :] = np.cos(ang[:, a, :]).T
            sinT[a * 64 + h * 32:a * 64 + h * 32 + 32, :] = np.sin(ang[:, a, :]).T
    c["rope"] = np.stack([cosT, sinT], 0).astype(np.float32)
    return c


class B:
    pass


def build(cfg, dbg=(), stop_after=None):
    nc = bass.Bass("TRN2", target_bir_lowering=False)
    D, KC, TOK, LAT, CTX, NB = cfg.D, cfg.KC, cfg.TOK, cfg.LAT, cfg.CTX, cfg.NB
    NR = NB + 1
    L = cfg.DEPTH

    def din(name, shape, dt=F32):
        return nc.dram_tensor(name, list(shape), dt, kind="ExternalInput").ap()

    I = {}
    I["x"] = din("x", [NB, LAT, D])
    I["c"] = din("c", [NB, D])
    I["ctx"] = din("ctx", [NB, CTX, D])
    I["c_ctx"] = din("c_ctx", [D])
    I["w_mod"] = din("w_mod", [L, D, 6 * D])
    I["b_mod"] = din("b_mod", [L, 6 * D])
    I["w_in"] = din("w_in", [L, D, cfg.IN_COLS])
    I["ml_gate_bias"] = din("ml_gate_bias", [L, 4 * cfg.MLH])
    I["attn_q_norm"] = din("attn_q_norm", [L, 128])
    I["attn_k_norm"] = din("attn_k_norm", [L, 128])
    I["hg_lb_logits"] = din("hg_lb_logits", [2, L, cfg.HGH * 128])
    I["hg_norm"] = din("hg_norm", [L, 128])
    I["ml_norm"] = din("ml_norm", [L, 256])
    I["w_branch"] = din("w_branch", [L, 3, cfg.BW, D])
    I["w_out"] = din("w_out", [L, D, D])
    for n in ("ln1_g", "ln1_b", "ln2_g", "ln2_b"):
        I[n] = din(n, [L, D])
    I["w_router"] = din("w_router", [D, 16])
    I["b_router"] = din("b_router", [16])
    I["w_exp_gate"] = din("w_exp_gate", [L, 16, D, cfg.DE])
    I["w_exp_up"] = din("w_exp_up", [L, 16, D, cfg.DE])
    I["w_exp_down"] = din("w_exp_down", [L, 16, cfg.DE, D])
    I["cmat"] = din("cmat", [128, 640])
    I["sel"] = din("sel", [32, 32 * 128])
    I["rmask"] = din("rmask", [128, TOK])
    I["rope"] = din("rope", [2, 128, LAT])
    out = nc.dram_tensor("out", [NB, LAT, D], F32, kind="ExternalOutput").ap()

    dbg_out = {}

    def dscr(name, shape, dt):
        if name in dbg:
            ap = nc.dram_tensor(name, list(shape), dt, kind="ExternalOutput").ap()
            dbg_out[name] = ap
            return ap
        return nc.dram_tensor(name, list(shape), dt, kind="Internal").ap()

    G = {}
    G["xT"] = dscr("xT", [NB, D, TOK], F32)
    G["hT"] = dscr("hT", [NB, D, TOK], BF16)
    G["qraw"] = dscr("qraw", [NB, cfg.AH * 128, TOK], F32)
    G["kraw"] = dscr("kraw", [NB, cfg.AKV * 128, TOK], F32)
    G["v_tok"] = dscr("v_tok", [NB, TOK, cfg.AKV * 128], BF16)
    G["QT"] = dscr("QT", [NB, cfg.AH * 128, TOK], BF16)
    G["KT"] = dscr("KT", [NB, cfg.AKV * 128, TOK], BF16)
    G["hgq"] = dscr("hgq", [NB, cfg.HGH * 128, TOK], F32)
    G["hgff"] = dscr("hgff", [NB, cfg.HGH * 128, TOK], F32)
    G["hgfb"] = dscr("hgfb", [NB, cfg.HGH * 128, TOK], F32)
    G["hgi_tok"] = dscr("hgi_tok", [NB, TOK, cfg.HGH * 128], BF16)
    G["hgg"] = dscr("hgg", [NB, cfg.HGH * 128, TOK], F32)
    G["mlq"] = dscr("mlq", [NB, cfg.MLH * 128, TOK], F32)
    G["mlk"] = dscr("mlk", [NB, cfg.MLH * 128, TOK], F32)
    G["mlv_tok"] = dscr("mlv_tok", [NB, TOK, cfg.MLH * 256], BF16)
    G["mlg"] = dscr("mlg", [NB, 4 * cfg.MLH, TOK], F32)
    G["mlo"] = dscr("mlo", [NB, cfg.MLH * 256, TOK], F32)
    G["gate"] = dscr("gate", [NB, 3 * D, TOK], BF16)
    G["yT"] = dscr("yT", [NB, 3 * cfg.BW, TOK], BF16)
    G["mergedT"] = dscr("mergedT", [NB, D, TOK], BF16)
    G["y2T"] = dscr("y2T", [NB, D, TOK], F32)
    G["combT"] = dscr("combT", [NB, 16, TOK], F32)
    G["faccT"] = dscr("faccT", [NB, D, TOK], F32)
    G["hid"] = dscr("hid", [NB, cfg.DE, TOK], BF16)

    with ExitStack() as es:
        S = Sched(nc, es)
        def P(name, shape, dt=F32):
            return es.enter_context(nc.sbuf_tensor("sb_" + name, list(shape), dt))

        cmat = P("cmat", [128, 640])
        cmatb = P("cmatb", [128, 640], BF16)
        sel = P("sel", [32, 32 * 128])
        ident = cmat[:, 0:128]
        ones = cmat[:, 128:256]
        Rm = cmat[:, 256:384]
        identb = cmatb[:, 0:128]
        onesb = cmatb[:, 128:256]
        maskFb = cmatb[:, 384:512]
        maskBb = cmatb[:, 512:640]
        sT = P("sT", [128, KC, NR])
        modT = P("modT", [128, 6 * KC, NR])
        mod1p = P("mod1p", [128, 6 * KC, NR])
        modga = P("modga", [128, 6 * KC, NR])
        bmT = P("bmT", [128, 6 * KC])
        lnv = P("lnv", [128, 4, KC])
        qn_g = P("qn_g", [128, 4])
        mln_g = P("mln_g", [128, 2])
        lbv = P("lbv", [128, 2, 3, cfg.HGH])
        mlb = P("mlb", [32, 2])
        wr = P("wr", [128, KC, 16])
        brb = P("brb", [128, 16])
        ARENA_BYTES = 176 * 1024
        SETTLE = SETTLE_N
        settle_t = P("settle", [SETTLE_ROWS, 16])
        arena = es.enter_context(nc.sbuf_tensor("arena", [128, ARENA_BYTES // 4], F32))
        arena_off = [0]
        arena_cnt = [0]

        a_base = nc.sbuf_base - ARENA_BYTES if False else None

        ps = [es.enter_context(nc.psum_tensor("ps%d" % i, [128, 512], F32)) for i in range(7)]
        psb = es.enter_context(nc.psum_tensor("psb", [128, 1024], BF16))
        ps_rr = [0]

        def nextps(lo=0, hi=7):
            i = lo + ps_rr[0] % (hi - lo)
            ps_rr[0] += 1
            return i

        class Arena:
            def __init__(self):
                self.off = 0

            def reset(self):
                S.barrier()
                self.off = 0
                for i in range(SETTLE):
                    dma("sync", settle_t[:, :], I["cmat"][0:SETTLE_ROWS, 0:16], r=["settle"], w=["settle"])
                if SETTLE:
                    S.barrier()

            def tile(self, shape, dt=F32):
                n = int(np.prod(shape[1:]))
                esz = 4 if dt == F32 else 2
                nbytes = (n * esz + 31) // 32 * 32
                assert self.off + nbytes <= ARENA_BYTES, ("arena overflow", self.off, nbytes)
                w0 = self.off // 4
                flat = arena[:, w0:w0 + nbytes // 4]
                self.off += nbytes
                if dt != F32:
                    flat = flat.bitcast(dt)
                flat = flat[:, 0:n]
                if len(shape) == 3:
                    flat = flat.rearrange("p (a b) -> p a b", a=shape[1])
                elif len(shape) == 4:
                    flat = flat.rearrange("p (a b c) -> p a b c", a=shape[1], b=shape[2])
                if shape[0] < 128:
                    flat = flat[0:shape[0]]
                arena_cnt[0] += 1
                return flat, ("ar", arena_cnt[0])

        A = Arena()

        def dma(q, out_, in_, r=(), w=(), **kw):
            r = [k for k in r if not (isinstance(k, tuple) and k[0] in G)]
            w = [k for k in w if not (isinstance(k, tuple) and k[0] in G)]

            def outer(ap_):
                for d_ in ap_.shape:
                    if d_ > 1:
                        return d_
                return 1
            odd = ODD_POOL and ((outer(out_) % 16 != 0) or (outer(in_) % 16 != 0))
            if odd:
                odd_log.append((tuple(out_.shape), tuple(in_.shape)))
            return S.op(q, lambda e: e.dma_start(out=out_, in_=in_, **kw), reads=r, writes=w, dma=True, odd=odd)

        def mm(out_, lhsT, rhs, start, stop, r=(), w=()):
            return S.op("tensor", lambda e: e.matmul(out_, lhsT=lhsT, rhs=rhs, start=start, stop=stop), reads=r, writes=w)

        def tr(out_, in_, idn, r=(), w=()):
            return S.op("tensor", lambda e: e.transpose(out_, in_, idn), reads=r, writes=w)

        def act(out_, in_, func, r=(), w=(), eng="scalar", **kw):
            return S.op("scalar", lambda e: e.activation(out=out_, in_=in_, func=func, **kw), reads=r, writes=w)

        def tt(out_, in0, in1, op, r=(), w=(), eng="vector"):
            return S.op(eng, lambda e: e.tensor_tensor(out=out_, in0=in0, in1=in1, op=op), reads=r, writes=w)

        def ts(out_, in0, s1, s2, op0, op1=None, r=(), w=(), eng="vector"):
            if op1 is None:
                return S.op(eng, lambda e: e.tensor_scalar(out=out_, in0=in0, scalar1=s1, scalar2=None, op0=op0), reads=r, writes=w)
            return S.op(eng, lambda e: e.tensor_scalar(out=out_, in0=in0, scalar1=s1, scalar2=s2, op0=op0, op1=op1), reads=r, writes=w)

        def stt(out_, in0, sc, in1, op0, op1, r=(), w=()):
            return S.op("vector", lambda e: e.scalar_tensor_tensor(out=out_, in0=in0, scalar=sc, in1=in1, op0=op0, op1=op1), reads=r, writes=w)

        def cp(out_, in_, r=(), w=(), eng="vector"):
            if eng == "scalar":
                return S.op("scalar", lambda e: e.copy(out=out_, in_=in_), reads=r, writes=w)
            return S.op(eng, lambda e: e.tensor_copy(out=out_, in_=in_), reads=r, writes=w)

        def recip(out_, in_, r=(), w=()):
            return S.op("vector", lambda e: e.reciprocal(out=out_, in_=in_), reads=r, writes=w)

        odd_log = []

        def PSK(i):
            return ("ps", i)

        dumped = set()

        def dbgdump(name, ap_, shape, dt, key):
            if name not in dbg or name in dumped:
                return
            dumped.add(name)
            o_ = nc.dram_tensor(name, list(shape), dt, kind="ExternalOutput").ap()
            dbg_out[name] = o_
            dma("sync", o_, ap_, r=[key], w=[("dbg", name)])

        dma("sync", cmat[:], I["cmat"][:, :], w=["cmat"])
        dma("sync", sel[:], I["sel"][:, :], w=["sel"])
        cp(cmatb[:], cmat[:], r=["cmat"], w=["cmatb"])

        def load_vecT(dst, vec_ap, n, key):
            t, tk = A.tile([n, 128])
            dma("sync", t, vec_ap.rearrange("(k p) -> k p", p=128), w=[tk])
            pi = nextps()
            tr(ps[pi][:, 0:n], t, ident[0:n, 0:n], r=[tk, "cmat"], w=[PSK(pi)])
            cp(dst, ps[pi][:, 0:n], r=[PSK(pi)], w=[key])

        def stage_s():
            A.reset()
            for rr in range(NR):
                src = I["c"][rr] if rr < NB else I["c_ctx"]
                t, tk = A.tile([KC, 128])
                dma("sync", t, src.rearrange("(k p) -> k p", p=128), w=[tk])
                pi = nextps()
                tr(ps[pi][:, 0:KC], t, ident[0:KC, 0:KC], r=[tk, "cmat"], w=[PSK(pi)])
                act(sT[:, :, rr], ps[pi][:, 0:KC], AF.Silu, r=[PSK(pi)], w=["sT"])

        def stage_mod(l):
            A.reset()
            n6 = 6 * KC
            j0 = 0
            while j0 < n6:
                nn = min(96, n6 - j0)
                load_vecT(bmT[:, j0:j0 + nn], I["b_mod"][l, j0 * 128:(j0 + nn) * 128], nn, "bmT")
                j0 += nn
            for i, nme in enumerate(("ln1_g", "ln1_b", "ln2_g", "ln2_b")):
                load_vecT(lnv[:, i, :], I[nme][l], KC, "lnv")
            wt = [A.tile([128, KC, 512]) for _ in range(2)]
            ncb = (6 * D) // 512
            for cb in range(ncb):
                w_t, wk = wt[cb % 2]
                for kk in range(0, KC, 8):
                    ke = min(KC, kk + 8)
                    dma("gpsimd" if (kk // 8) % 2 else "sync", w_t[:, kk:ke, :],
                        I["w_mod"][l, kk * 128:ke * 128, cb * 512:(cb + 1) * 512].rearrange("(k p) c -> p k c", p=128),
                        w=[(wk, kk // 8)])
                pi = nextps()
                for j in range(4):
                    for k in range(KC):
                        mm(ps[pi][:, j * NR:(j + 1) * NR], w_t[:, k, j * 128:(j + 1) * 128], sT[:, k, :],
                           k == 0, k == KC - 1, r=[(wk, k // 8), "sT"], w=[PSK(pi)])
                for j in range(4):
                    jj = cb * 4 + j
                    ts(modT[:, jj, :], ps[pi][:, j * NR:(j + 1) * NR], bmT[:, jj:jj + 1], None, ALU.add,
                       r=[PSK(pi), "bmT"], w=["modT"])
            ts(mod1p[:], modT[:], 1.0, None, ALU.add, r=["modT"], w=["mod1p"])
            ts(modga[:], modT[:], 1.0 / cfg.alpha, None, ALU.mult, r=["modT"], w=["modga"])

        def stage_init():
            A.reset()
            xin = [A.tile([128, 4, D]) for _ in range(1)]
            xo = [A.tile([128, KC, 512]) for _ in range(1)]
            for b in range(NB):
                for (t0, tn, isc) in cfg.tblocks(512):
                    xi, xik = xin[0]
                    nsub = tn // 128
                    src = I["ctx"][b, t0 - LAT:t0 - LAT + tn, :] if isc else I["x"][b, t0:t0 + tn, :]
                    dma("sync", xi[:, 0:nsub, :], src.rearrange("(s p) d -> p s d", p=128), w=[xik])
                    xt, xtk = xo[0]
                    for k in range(KC):
                        pi = nextps()
                        for s_ in range(nsub):
                            tr(ps[pi][:, s_ * 128:(s_ + 1) * 128], xi[:, s_, k * 128:(k + 1) * 128], ident,
                               r=[xik, "cmat"], w=[PSK(pi)])
                        if k % 2 == 0:
                            cp(xt[:, k, 0:tn], ps[pi][:, 0:tn], r=[PSK(pi)], w=[xtk])
                        else:
                            cp(xt[:, k, 0:tn], ps[pi][:, 0:tn], r=[PSK(pi)], w=[xtk], eng="scalar")
                    dma(STORE_Q, G["xT"][b, :, t0:t0 + tn].rearrange("(k p) t -> p k t", p=128), xt[:, :, 0:tn],
                        r=[xtk], w=[("xT", b)])

        def stage_modulate(i_shift, i_scale):
            A.reset()
            xb = [A.tile([128, KC, 256]) for _ in range(2)]
            hb = [A.tile([128, KC, 256], BF16) for _ in range(2)]
            it = 0
            for b in range(NB):
                for (t0, tn, isc) in cfg.tblocks(256):
                    xt, xk = xb[it % 2]
                    ht, hk = hb[it % 2]
                    it += 1
                    row = NB if isc else b
                    dma("sync", xt[:, :, 0:tn], G["xT"][b, :, t0:t0 + tn].rearrange("(k p) t -> p k t", p=128),
                        r=[("xT", b)], w=[xk])
                    for k in range(KC):
                        ts(ht[:, k, 0:tn], xt[:, k, 0:tn], mod1p[:, i_scale * KC + k, row:row + 1],
                           modT[:, i_shift * KC + k, row:row + 1], ALU.mult, ALU.add,
                           r=[xk, "mod1p", "modT"], w=[hk], eng="vector")
                    dma(STORE_Q, G["hT"][b, :, t0:t0 + tn].rearrange("(k p) t -> p k t", p=128), ht[:, :, 0:tn],
                        r=[hk], w=[("hT", b)])

        def linear(Kc, src_fn, src_keys, w_fn, colblocks, kgroups=None, wcols=512, nabuf=2):
            if kgroups is None:
                kgroups = [(0, Kc)]
            wb = [A.tile([128, Kc, wcols], BF16) for _ in range(2)]
            ab = [A.tile([128, Kc, 512], BF16) for _ in range(nabuf)]
            ai = 0
            tiles = []
            cur = []
            curw = 0
            for cbk in colblocks:
                if cur and (curw + cbk[1] > wcols or cbk[2] != cur[-1][2] or cbk[0] != cur[-1][0] + cur[-1][1]):
                    tiles.append(cur)
                    cur, curw = [], 0
                cur.append(cbk)
                curw += cbk[1]
            if cur:
                tiles.append(cur)
            for wi, tl in enumerate(tiles):
                w_t, wk = wb[wi % 2]
                c_lo = tl[0][0]
                c_w = sum(x[1] for x in tl)
                step = max(1, 8192 // max(c_w, 1) // 4)
                for kk in range(0, Kc, 8):
                    ke = min(Kc, kk + 8)
                    dma("gpsimd", w_t[:, kk:ke, 0:c_w], w_fn(kk * 128, ke * 128, c_lo, c_lo + c_w).rearrange("(k p) c -> p k c", p=128),
                        w=[(wk, kk // 8)])
                for b in range(NB):
                    for (t0, tn, isc) in cfg.tblocks(512):
                        a_t, ak = ab[ai % nabuf]
                        ai += 1
                        dma("sync", a_t[:, :, 0:tn], src_fn(b, t0, tn).rearrange("(k p) t -> p k t", p=128),
                            r=[(kname, b) for kname in src_keys], w=[ak])
                        for (c0, cw, layout, epi) in tl:
                            lc = c0 - c_lo
                            if layout == "F":
                                pis = []
                                for (k0, k1) in kgroups:
                                    pi = nextps()
                                    pis.append(pi)
                                    for k in range(k0, k1):
                                        mm(ps[pi][0:cw, 0:tn], w_t[:, k, lc:lc + cw], a_t[:, k, 0:tn], k == k0, k == k1 - 1,
                                           r=[(wk, k // 8), ak], w=[PSK(pi)])
                                epi(b, c0, cw, t0, tn, pis)
                            else:
                                for s_ in range(tn // 128):
                                    pi = nextps()
                                    for k in range(Kc):
                                        mm(ps[pi][:, 0:cw], a_t[:, k, s_ * 128:(s_ + 1) * 128], w_t[:, k, lc:lc + cw], k == 0, k == Kc - 1,
                                           r=[(wk, k // 8), ak], w=[PSK(pi)])
                                    epi(b, c0, cw, t0 + s_ * 128, 128, [pi])

        stg = {"bufs": None, "i": 0}

        def staging(dt):
            if stg["bufs"] is None or stg["arena"] != arena_cnt[0] - stg["n"]:
                pass
            return None

        def make_store_epi(dst, key, dt, layout, col_base, func=None, scale=None, pool=None):
            if pool is None:
                pool = {"bufs": [A.tile([128, 512], F32) for _ in range(4)], "cnt": [0]}
            bufs = [((t if dt == F32 else t.bitcast(BF16)[:, 0:512]), k) for (t, k) in pool["bufs"]]
            cnt = pool["cnt"]

            def epi(b, c0, cw, t0, tn, pis):
                pi = pis[0]
                st, sk = bufs[cnt[0] % 4]
                cnt[0] += 1
                if layout == "F":
                    src = ps[pi][0:cw, 0:tn]
                    dsts = st[0:cw, 0:tn]
                    dram = dst[b, c0 - col_base:c0 - col_base + cw, t0:t0 + tn]
                else:
                    src = ps[pi][:, 0:cw]
                    dsts = st[:, 0:cw]
                    dram = dst[b, t0:t0 + tn, c0 - col_base:c0 - col_base + cw]
                if func is not None:
                    act(dsts, src, func, r=[PSK(pi)], w=[sk])
                elif cnt[0] % 2:
                    cp(dsts, src, r=[PSK(pi)], w=[sk])
                else:
                    cp(dsts, src, r=[PSK(pi)], w=[sk], eng="scalar")
                dma("sync", dram, dsts, r=[sk], w=[(key, b)])
            return epi

        def stage_inproj(l):
            A.reset()
            spec = {
                "attn_q": ("qraw", F32, "F", None), "attn_k": ("kraw", F32, "F", None), "attn_v": ("v_tok", BF16, "T", None),
                "hg_q": ("hgq", F32, "F", None), "hg_f_fwd": ("hgff", F32, "F", None), "hg_f_bwd": ("hgfb", F32, "F", None),
                "hg_i": ("hgi_tok", BF16, "T", None), "hg_g": ("hgg", F32, "F", None),
                "ml_q": ("mlq", F32, "F", None), "ml_k": ("mlk", F32, "F", None), "ml_v": ("mlv_tok", BF16, "T", None),
                "ml_gates": ("mlg", F32, "F", None), "ml_o": ("mlo", F32, "F", None), "merge": ("gate", BF16, "F", AF.Sigmoid),
            }
            colblocks = []
            pool = {"bufs": [A.tile([128, 512], F32) for _ in range(4)], "cnt": [0]}
            for name, width in cfg.segs:
                o0, _ = cfg.off[name]
                gname, dt, lay, fn = spec[name]
                epi = make_store_epi(G[gname], gname, dt, lay, o0, func=fn, pool=pool)
                step = 128 if lay == "F" else 512
                c = 0
                while c < width:
                    cw = min(step, width - c)
                    colblocks.append((o0 + c, cw, lay, epi))
                    c += cw
            linear(KC, lambda b, t0, tn: G["hT"][b, :, t0:t0 + tn], ["hT"],
                   lambda k0, k1, c0, c1: I["w_in"][l, k0:k1, c0:c1], colblocks)


        def vmemset(ap_, val, w):
            return S.op("vector", lambda e: e.memset(ap_, val), writes=w)

        def scan(out_, d0, d1, r, w):
            return S.op("vector", lambda e: e.tensor_tensor_scan(out=out_, data0=d0, data1=d1, initial=0.0, op0=ALU.mult, op1=ALU.add), reads=r, writes=w)

        def load_col(dst, vec_ap, n, key):
            dma("sync", dst, vec_ap.rearrange("(p o) -> p o", o=1), w=[key])

        def rstd_from(ps_ap, n, tmp, out_, rk, wk_):
            ts(tmp, ps_ap, 1.0 / n, EPS, ALU.mult, ALU.add, r=rk, w=[wk_ + "_t"])
            act(out_, tmp, AF.Sqrt, r=[wk_ + "_t"], w=[wk_]); recip(out_, out_, r=[wk_], w=[wk_])

        def stage_qk(l):
            A.reset()
            load_col(qn_g[:, 0:1], I["attn_q_norm"][l], 128, "qn_g0")
            load_col(qn_g[:, 1:2], I["attn_k_norm"][l], 128, "qn_g1")
            T_ = lambda dt=F32: A.tile([128, 512], dt)
            q_t, sq_t, r1_t, rs_t, qn_t, cs_t, sn_t, a_t, b_t = [T_() for _ in range(9)]
            o_t = T_(BF16)
            for b in range(NB):
                for (src, dst, nh, gc) in (("qraw", "QT", cfg.AH, 0), ("kraw", "KT", cfg.AKV, 1)):
                    for h in range(nh):
                        for (t0, tn, isc) in cfg.tblocks(512):
                            dma("sync", q_t[0][:, 0:tn], G[src][b, h * 128:(h + 1) * 128, t0:t0 + tn], r=[(src, b)], w=[q_t[1]])
                            act(sq_t[0][:, 0:tn], q_t[0][:, 0:tn], AF.Square, r=[q_t[1]], w=[sq_t[1]])
                            pi = nextps()
                            mm(ps[pi][:, 0:tn], ones, sq_t[0][:, 0:tn], True, True, r=["cmat", sq_t[1]], w=[PSK(pi)])
                            ts(r1_t[0][:, 0:tn], ps[pi][:, 0:tn], 1.0 / 128, EPS, ALU.mult, ALU.add, r=[PSK(pi)], w=[r1_t[1]])
                            act(rs_t[0][:, 0:tn], r1_t[0][:, 0:tn], AF.Sqrt, r=[r1_t[1]], w=[rs_t[1]]); recip(rs_t[0][:, 0:tn], rs_t[0][:, 0:tn], r=[rs_t[1]], w=[rs_t[1]])
                            stt(qn_t[0][:, 0:tn], q_t[0][:, 0:tn], qn_g[:, gc:gc + 1], rs_t[0][:, 0:tn], ALU.mult, ALU.mult,
                                r=[q_t[1], rs_t[1], "qn_g%d" % gc], w=[qn_t[1]])
                            if not isc:
                                dma("sync", cs_t[0][:, 0:tn], I["rope"][0, :, t0:t0 + tn], w=[cs_t[1]])
                                dma("sync", sn_t[0][:, 0:tn], I["rope"][1, :, t0:t0 + tn], w=[sn_t[1]])
                                pr = nextps()
                                mm(ps[pr][:, 0:tn], Rm, qn_t[0][:, 0:tn], True, True, r=["cmat", qn_t[1]], w=[PSK(pr)])
                                tt(a_t[0][:, 0:tn], qn_t[0][:, 0:tn], cs_t[0][:, 0:tn], ALU.mult, r=[qn_t[1], cs_t[1]], w=[a_t[1]])
                                tt(b_t[0][:, 0:tn], ps[pr][:, 0:tn], sn_t[0][:, 0:tn], ALU.mult, r=[PSK(pr), sn_t[1]], w=[b_t[1]])
                                tt(o_t[0][:, 0:tn], a_t[0][:, 0:tn], b_t[0][:, 0:tn], ALU.add, r=[a_t[1], b_t[1]], w=[o_t[1]])
                            else:
                                cp(o_t[0][:, 0:tn], qn_t[0][:, 0:tn], r=[qn_t[1]], w=[o_t[1]])
                            dma("gpsimd", G[dst][b, h * 128:(h + 1) * 128, t0:t0 + tn], o_t[0][:, 0:tn], r=[o_t[1]], w=[(dst, b)])

        def stage_attn(l):
            A.reset()
            NSB = TOK // 128
            kt, ktk = A.tile([128, TOK], BF16)
            vt, vtk = A.tile([128, NSB, 128], BF16)
            qts = [A.tile([128, TOK], BF16) for _ in range(2)]
            ebs = [A.tile([128, 512], BF16) for _ in range(3)]
            rt, rtk = A.tile([128, 512])
            obs = [A.tile([128, 512], BF16) for _ in range(2)]
            cnt = [0, 0, 0]
            for b in range(NB):
                for hk in range(cfg.AKV):
                    dma("sync", kt, G["KT"][b, hk * 128:(hk + 1) * 128, :], r=[("KT", b)], w=[ktk])
                    dma("sync", vt, G["v_tok"][b, :, hk * 128:(hk + 1) * 128].rearrange("(j p) e -> p j e", p=128), r=[("v_tok", b)], w=[vtk])
                    for g_ in range(4):
                        h = hk * 4 + g_
                        qt, qtk = qts[h % 2]
                        dma("sync", qt, G["QT"][b, h * 128:(h + 1) * 128, :], r=[("QT", b)], w=[qtk])
                        for (t0, tn, isc) in cfg.tblocks(512):
                            sbl = list(range(LAT // 128, NSB)) if isc else list(range(NSB))
                            pN = 3 + cnt[1] % 2
                            pD = 5 + cnt[1] % 2
                            cnt[1] += 1
                            for ix, j in enumerate(sbl):
                                pS = cnt[0] % 3
                                eb, ebk = ebs[cnt[0] % 3]
                                cnt[0] += 1
                                mm(ps[pS][:, 0:tn], kt[:, j * 128:(j + 1) * 128], qt[:, t0:t0 + tn], True, True, r=[ktk, qtk], w=[PSK(pS)])
                                act(eb[:, 0:tn], ps[pS][:, 0:tn], AF.Exp, r=[PSK(pS)], w=[ebk], scale=float(128 ** -0.5))
                                mm(ps[pN][:, 0:tn], vt[:, j, :], eb[:, 0:tn], ix == 0, ix == len(sbl) - 1, r=[vtk, ebk], w=[PSK(pN)])
                                mm(ps[pD][:, 0:tn], onesb, eb[:, 0:tn], ix == 0, ix == len(sbl) - 1, r=["cmatb", ebk], w=[PSK(pD)])
                            recip(rt[:, 0:tn], ps[pD][:, 0:tn], r=[PSK(pD)], w=[rtk])
                            ob, obk = obs[cnt[2] % 2]
                            cnt[2] += 1
                            tt(ob[:, 0:tn], ps[pN][:, 0:tn], rt[:, 0:tn], ALU.mult, r=[PSK(pN), rtk], w=[obk])
                            dma("gpsimd", G["yT"][b, h * 128:(h + 1) * 128, t0:t0 + tn], ob[:, 0:tn], r=[obk], w=[("yT", b)])

        def gla_alloc(EV):
            T = {}
            for n in ("q", "lf", "kk", "pre", "bc", "d1", "tmp"):
                T[n] = A.tile([128, TOK])
            for n in ("Qs", "At", "Ke"):
                T[n] = A.tile([128, TOK], BF16)
            NSB = TOK // 128
            T["Ketok"] = A.tile([128, NSB, 128], BF16)
            T["V"] = A.tile([128, NSB, EV], BF16)
            T["dec"] = A.tile([128, TOK // 32])
            T["S"] = A.tile([128, EV])
            T["Sbf"] = [A.tile([128, EV], BF16) for _ in range(8)]
            T["WT"] = [A.tile([128, 128], BF16) for _ in range(2)]
            T["rm"] = A.tile([128, TOK])
            dma("sync", T["rm"][0], I["rmask"][:, :], w=[T["rm"][1]])
            return T

        def gla_dir(T, EV, dirn, post):
            NSB = TOK // 128
            NCH = TOK // 32
            q, qk_ = T["q"]; lf, lfk = T["lf"]; kk, kkk = T["kk"]; pre, prek = T["pre"]
            bc, bck = T["bc"]; d1, d1k = T["d1"]; tmp, tmpk = T["tmp"]
            Qs, Qsk = T["Qs"]; At, Atk = T["At"]; Ke, Kek = T["Ke"]; Ktok, Ktokk = T["Ketok"]
            V, Vk = T["V"]; dec, deck = T["dec"]; Sf, Sfk = T["S"]; rm, rmk = T["rm"]
            scan(pre, rm, lf, r=[rmk, lfk], w=[prek])
            pre3 = pre.rearrange("p (c s) -> p c s", s=32)
            totb = pre3[:, :, 31:32].to_broadcast([128, NCH, 32])
            v3 = lambda ap_: ap_.rearrange("p (c s) -> p c s", s=32)
            if dirn == 0:
                cp(bc, pre, r=[prek], w=[bck], eng="scalar")
            else:
                tt(tmp, lf, pre, ALU.subtract, r=[lfk, prek], w=[tmpk])
                tt(v3(bc), v3(tmp), totb, ALU.add, r=[tmpk, prek], w=[bck])
            tt(v3(d1), v3(bc), totb, ALU.subtract, r=[bck, prek], w=[d1k])
            act(tmp, bc, AF.Exp, r=[bck], w=[tmpk])
            tt(Qs, tmp, q, ALU.mult, r=[tmpk, qk_], w=[Qsk])
            act(tmp, d1, AF.Exp, r=[d1k], w=[tmpk])
            tt(At, tmp, q, ALU.mult, r=[tmpk, qk_], w=[Atk])
            act(tmp, d1, AF.Exp, r=[d1k], w=[tmpk], scale=-1.0)
            tt(Ke, tmp, kk, ALU.mult, r=[tmpk, kkk], w=[Kek])
            act(dec, pre3[:, :, 31], AF.Exp, r=[prek], w=[deck])
            for j0 in range(0, NSB, 8):
                j1 = min(NSB, j0 + 8)
                for j in range(j0, j1):
                    tr(psb[:, (j - j0) * 128:(j - j0 + 1) * 128], Ke[:, j * 128:(j + 1) * 128], identb, r=[Kek, "cmatb"], w=["psb"])
                cp(Ktok[:, j0:j1, :], psb[:, 0:(j1 - j0) * 128].rearrange("p (j d) -> p j d", d=128), r=["psb"], w=[Ktokk])
            sfx = "_d%d" % dirn
            dbgdump("g_pre" + sfx, pre, [128, TOK], F32, prek)
            dbgdump("g_bc" + sfx, bc, [128, TOK], F32, bck)
            dbgdump("g_d1" + sfx, d1, [128, TOK], F32, d1k)
            dbgdump("g_Qs" + sfx, Qs, [128, TOK], BF16, Qsk)
            dbgdump("g_At" + sfx, At, [128, TOK], BF16, Atk)
            dbgdump("g_Ke" + sfx, Ke, [128, TOK], BF16, Kek)
            dbgdump("g_dec" + sfx, dec, [128, TOK // 32], F32, deck)
            dbgdump("g_Ktok" + sfx, Ktok, [128, NSB, 128], BF16, Ktokk)
            dbgdump("g_lf" + sfx, lf, [128, TOK], F32, lfk)
            dbgdump("g_kk" + sfx, kk, [128, TOK], F32, kkk)
            dbgdump("g_q" + sfx, q, [128, TOK], F32, qk_)
            vmemset(Sf, 0.0, [Sfk])
            lat_b = list(range(0, LAT // 128))
            ctx_b = list(range(LAT // 128, NSB))
            order = (ctx_b + lat_b) if dirn == 0 else (ctx_b[::-1] + lat_b[::-1])
            corder = [0, 1, 2, 3] if dirn == 0 else [3, 2, 1, 0]
            maskb = maskFb if dirn == 0 else maskBb
            NEH = EV // 128
            ring = [0]
            wcnt = [0]
            for blk in order:
                for c in corder:
                    p0, p1 = (64, 128) if c == 3 else (32 * c, 32 * c + 32)
                    mm(ps[c][:, 0:EV], Ktok[p0:p1, blk, :], V[p0:p1, blk, :], True, True, r=[Ktokk, Vk], w=[PSK(c)])
                sb_of = {}
                for c in corder:
                    sb, sbk = T["Sbf"][ring[0] % 8]
                    ring[0] += 1
                    sb_of[c] = (sb, sbk)
                    cp(sb, Sf, r=[Sfk], w=[sbk], eng="scalar")
                    ci = blk * 4 + c
                    stt(Sf, Sf, dec[:, ci:ci + 1], ps[c][:, 0:EV], ALU.mult, ALU.add, r=[Sfk, deck, PSK(c)], w=[Sfk])
                    if c == 3:
                        tt(Sf, Sf, ps[2][:, 0:EV], ALU.subtract, r=[Sfk, PSK(2)], w=[Sfk])
                mm(ps[4][:, 0:128], Ke[:, blk * 128:(blk + 1) * 128], At[:, blk * 128:(blk + 1) * 128], True, True, r=[Kek, Atk], w=[PSK(4)])
                wt_, wtk = T["WT"][wcnt[0] % 2]
                wcnt[0] += 1
                tt(wt_, ps[4][:, 0:128], maskb, ALU.mult, r=[PSK(4), "cmatb"], w=[wtk])
                ehs = [NEH - 1] + list(range(NEH - 1)) if NEH == 3 else list(range(NEH))
                for eh in ehs:
                    pO = 5 + (wcnt[0] + eh) % 2
                    mm(ps[pO][:, 0:128], V[:, blk, eh * 128:(eh + 1) * 128], wt_, True, False, r=[Vk, wtk], w=[PSK(pO)])
                    for ix, c in enumerate(corder):
                        sb, sbk = sb_of[c]
                        mm(ps[pO][:, 32 * c:32 * c + 32], sb[:, eh * 128:(eh + 1) * 128],
                           Qs[:, blk * 128 + 32 * c:blk * 128 + 32 * c + 32], False, ix == 3, r=[sbk, Qsk], w=[PSK(pO)])
                    post(dirn, eh, pO, blk)

        def stage_hgrn(l):
            A.reset()
            T = gla_alloc(128)
            acc, acck = A.tile([128, TOK])
            gs, gsk = A.tile([128, TOK])
            sq, sqk = A.tile([128, TOK])
            yb, ybk = A.tile([128, TOK], BF16)
            r1, r1k = A.tile([128, 512]); rs, rsk = A.tile([128, 512])
            load_col(qn_g[:, 2:3], I["hg_norm"][l], 128, "qn_g2")
            for d_ in range(2):
                if l == 0:
                    vmemset(lbv[:, d_, 0, :], 0.0, ["lbv%d" % d_])
                    vmemset(lbv[:, d_, 1, :], 1.0, ["lbv%d" % d_])
                else:
                    load_vecT(lbv[:, d_, 0, :], I["hg_lb_logits"][d_, 0], cfg.HGH, "lbv%d" % d_)
                    load_vecT(lbv[:, d_, 1, :], I["hg_lb_logits"][d_, 1], cfg.HGH, "lbv%d" % d_)
                    tt(lbv[:, d_, 2, :], lbv[:, d_, 0, :], lbv[:, d_, 1, :], ALU.subtract, r=["lbv%d" % d_], w=["lbv%d" % d_])
                    act(lbv[:, d_, 2, :], lbv[:, d_, 2, :], AF.Exp, r=["lbv%d" % d_], w=["lbv%d" % d_])
                    ts(lbv[:, d_, 2, :], lbv[:, d_, 2, :], 1.0, None, ALU.add, r=["lbv%d" % d_], w=["lbv%d" % d_])
                    recip(lbv[:, d_, 0, :], lbv[:, d_, 2, :], r=["lbv%d" % d_], w=["lbv%d" % d_])
                    ts(lbv[:, d_, 1, :], lbv[:, d_, 0, :], -1.0, 1.0, ALU.mult, ALU.add, r=["lbv%d" % d_], w=["lbv%d" % d_])

            def post(dirn, eh, pO, blk):
                sl = acc[:, blk * 128:(blk + 1) * 128]
                if dirn == 0:
                    cp(sl, ps[pO][:, 0:128], r=[PSK(pO)], w=[acck], eng="scalar")
                else:
                    tt(sl, sl, ps[pO][:, 0:128], ALU.add, r=[PSK(pO), acck], w=[acck])

            for b in range(NB):
                for h in range(cfg.HGH):
                    rows = slice(h * 128, (h + 1) * 128)
                    q, qk_ = T["q"]
                    dma("sync", T["tmp"][0], G["hgq"][b, rows, :], r=[("hgq", b)], w=[T["tmp"][1]])
                    act(q, T["tmp"][0], AF.Silu, r=[T["tmp"][1]], w=[qk_])
                    ts(q, q, float(128 ** -0.5), None, ALU.mult, r=[qk_], w=[qk_])
                    dma("sync", T["V"][0], G["hgi_tok"][b, :, rows].rearrange("(j p) e -> p j e", p=128), r=[("hgi_tok", b)], w=[T["V"][1]])
                    for d_ in range(2):
                        lf, lfk = T["lf"]; kk, kkk = T["kk"]; tmp, tmpk = T["tmp"]
                        dma("sync", tmp, G["hgff" if d_ == 0 else "hgfb"][b, rows, :], r=[("hgff" if d_ == 0 else "hgfb", b)], w=[tmpk])
                        act(lf, tmp, AF.Exp, r=[tmpk], w=[lfk], scale=-1.0)
                        ts(lf, lf, 1.0, None, ALU.add, r=[lfk], w=[lfk])
                        recip(kk, lf, r=[lfk], w=[kkk])
                        ts(kk, kk, lbv[:, d_, 1, h:h + 1], lbv[:, d_, 0, h:h + 1], ALU.mult, ALU.add, r=[kkk, "lbv%d" % d_], w=[kkk])
                        act(lf, kk, AF.Ln, r=[kkk], w=[lfk])
                        ts(kk, kk, -1.0, 1.0, ALU.mult, ALU.add, r=[kkk], w=[kkk])
                        gla_dir(T, 128, d_, post)
                    dma("sync", T["tmp"][0], G["hgg"][b, rows, :], r=[("hgg", b)], w=[T["tmp"][1]])
                    act(gs, T["tmp"][0], AF.Silu, r=[T["tmp"][1]], w=[gsk])
                    act(sq, acc, AF.Square, r=[acck], w=[sqk])
                    for (t0, tn, isc) in cfg.tblocks(512):
                        pi = nextps()
                        mm(ps[pi][:, 0:tn], ones, sq[:, t0:t0 + tn], True, True, r=["cmat", sqk], w=[PSK(pi)])
                        ts(r1[:, 0:tn], ps[pi][:, 0:tn], 1.0 / 128, EPS, ALU.mult, ALU.add, r=[PSK(pi)], w=[r1k])
                        act(rs[:, 0:tn], r1[:, 0:tn], AF.Sqrt, r=[r1k], w=[rsk]); recip(rs[:, 0:tn], rs[:, 0:tn], r=[rsk], w=[rsk])
                        stt(r1[:, 0:tn], acc[:, t0:t0 + tn], qn_g[:, 2:3], rs[:, 0:tn], ALU.mult, ALU.mult, r=[acck, rsk, "qn_g2"], w=[r1k])
                        tt(yb[:, t0:t0 + tn], r1[:, 0:tn], gs[:, t0:t0 + tn], ALU.mult, r=[r1k, gsk], w=[ybk])
                    dma("gpsimd", G["yT"][b, cfg.BW + h * 128:cfg.BW + (h + 1) * 128, :], yb, r=[ybk], w=[("yT", b)])

        def stage_mlstm(l):
            A.reset()
            EV = 384
            T = gla_alloc(EV)
            NG = 4 * cfg.MLH
            accs = [A.tile([128, TOK]) for _ in range(2)]
            sq, sqk = A.tile([128, TOK])
            yb, ybk = A.tile([128, TOK], BF16)
            gI, gIk = A.tile([32, TOK]); gL, gLk = A.tile([32, TOK])
            r1, r1k = A.tile([128, 512]); rs, rsk = A.tile([128, 512])
            rinv, rinvk = A.tile([128, 128]); hn, hnk = A.tile([128, 128])
            dma("sync", mln_g[:, 0:2], I["ml_norm"][l].rearrange("(e p) -> p e", p=128), w=["mln_g"], allow_slow_non_contiguous=True)
            vmemset(mlb[:], 0.0, ["mlb"])
            dma("sync", mlb[0:NG, 0:1], I["ml_gate_bias"][l].rearrange("(p o) -> p o", o=1), r=["mlb"], w=["mlb"])
            ts(mlb[:, 1:2], mlb[:, 0:1], -1.0, None, ALU.mult, r=["mlb"], w=["mlb"])

            def post(dirn, eh, pO, blk):
                if eh == 2:
                    ts(hn, ps[pO][:, 0:128], -1.0, None, ALU.mult, r=[PSK(pO)], w=[hnk])
                    tt(rinv, hn, ps[pO][:, 0:128], ALU.max, r=[PSK(pO), hnk], w=[rinvk])
                    ts(rinv, rinv, 1.0, None, ALU.max, r=[rinvk], w=[rinvk])
                    recip(rinv, rinv, r=[rinvk], w=[rinvk])
                    return
                acc, acck = accs[eh]
                sl = acc[:, blk * 128:(blk + 1) * 128]
                if dirn == 0:
                    tt(sl, ps[pO][:, 0:128], rinv, ALU.mult, r=[PSK(pO), rinvk], w=[acck])
                else:
                    tt(hn, ps[pO][:, 0:128], rinv, ALU.mult, r=[PSK(pO), rinvk], w=[hnk])
                    tt(sl, sl, hn, ALU.add, r=[hnk, acck], w=[acck])

            for b in range(NB):
                vmemset(gI, 0.0, [gIk]); vmemset(gL, 0.0, [gLk])
                dma("sync", gI[0:NG, :], G["mlg"][b, :, :], r=[("mlg", b), gIk], w=[gIk])
                act(gL, gI, AF.Exp, r=[gIk, "mlb"], w=[gLk], bias=mlb[:, 1:2], scale=-1.0)
                ts(gL, gL, 1.0, None, ALU.add, r=[gLk], w=[gLk])
                act(gL, gL, AF.Ln, r=[gLk], w=[gLk])
                ts(gL, gL, -1.0, None, ALU.mult, r=[gLk], w=[gLk])
                act(gI, gI, AF.Exp, r=[gIk, "mlb"], w=[gIk], bias=mlb[:, 0:1], scale=1.0)
                for h in range(cfg.MLH):
                    rows = slice(h * 128, (h + 1) * 128)
                    V, Vk = T["V"]
                    vmemset(V[:, :, 256:384], 1.0, [Vk])
                    dma("sync", V[:, :, 0:256], G["mlv_tok"][b, :, h * 256:(h + 1) * 256].rearrange("(j p) e -> p j e", p=128), r=[("mlv_tok", b), Vk], w=[Vk])
                    dma("sync", T["q"][0], G["mlq"][b, rows, :], r=[("mlq", b)], w=[T["q"][1]])
                    for d_ in range(2):
                        lf, lfk = T["lf"]; kk, kkk = T["kk"]; tmp, tmpk = T["tmp"]
                        dma("sync", tmp, G["mlk"][b, rows, :], r=[("mlk", b)], w=[tmpk])
                        ri = d_ * cfg.MLH + h
                        rf = (2 + d_) * cfg.MLH + h
                        for (t0, tn, isc) in cfg.tblocks(512):
                            pi = nextps()
                            mm(ps[pi][:, 0:tn], sel[:, rf * 128:(rf + 1) * 128], gL[:, t0:t0 + tn], True, True, r=["sel", gLk], w=[PSK(pi)])
                            cp(lf[:, t0:t0 + tn], ps[pi][:, 0:tn], r=[PSK(pi)], w=[lfk], eng="scalar")
                            pj = nextps()
                            mm(ps[pj][:, 0:tn], sel[:, ri * 128:(ri + 1) * 128], gI[:, t0:t0 + tn], True, True, r=["sel", gIk], w=[PSK(pj)])
                            stt(kk[:, t0:t0 + tn], tmp[:, t0:t0 + tn], float(128 ** -0.5), ps[pj][:, 0:tn], ALU.mult, ALU.mult, r=[tmpk, PSK(pj)], w=[kkk])
                        gla_dir(T, EV, d_, post)
                    for eh in range(2):
                        act(sq, accs[eh][0], AF.Square, r=[accs[eh][1]], w=[sqk]) if eh == 0 else None
                    sq2, sq2k = T["pre"]
                    act(sq2, accs[1][0], AF.Square, r=[accs[1][1]], w=[sq2k])
                    for eh in range(2):
                        dma("sync", T["tmp"][0], G["mlo"][b, h * 256 + eh * 128:h * 256 + (eh + 1) * 128, :], r=[("mlo", b)], w=[T["tmp"][1]])
                        act(T["bc"][0], T["tmp"][0], AF.Sigmoid, r=[T["tmp"][1]], w=[T["bc"][1]])
                        for (t0, tn, isc) in cfg.tblocks(512):
                            pi = nextps()
                            mm(ps[pi][:, 0:tn], ones, sq[:, t0:t0 + tn], True, False, r=["cmat", sqk], w=[PSK(pi)])
                            mm(ps[pi][:, 0:tn], ones, sq2[:, t0:t0 + tn], False, True, r=["cmat", sq2k], w=[PSK(pi)])
                            ts(r1[:, 0:tn], ps[pi][:, 0:tn], 1.0 / 256, EPS, ALU.mult, ALU.add, r=[PSK(pi)], w=[r1k])
                            act(rs[:, 0:tn], r1[:, 0:tn], AF.Sqrt, r=[r1k], w=[rsk]); recip(rs[:, 0:tn], rs[:, 0:tn], r=[rsk], w=[rsk])
                            stt(r1[:, 0:tn], accs[eh][0][:, t0:t0 + tn], mln_g[:, eh:eh + 1], rs[:, 0:tn], ALU.mult, ALU.mult, r=[accs[eh][1], rsk, "mln_g"], w=[r1k])
                            tt(yb[:, t0:t0 + tn], r1[:, 0:tn], T["bc"][0][:, t0:t0 + tn], ALU.mult, r=[r1k, T["bc"][1]], w=[ybk])
                        r0 = 2 * cfg.BW + h * 256 + eh * 128
                        dma("gpsimd", G["yT"][b, r0:r0 + 128, :], yb, r=[ybk], w=[("yT", b)])


        def stage_merge(l):
            A.reset()
            gts = [A.tile([128, 512], BF16) for _ in range(3)]
            m0, m0k = A.tile([128, 512]); m1, m1k = A.tile([128, 512])
            ob, obk = A.tile([128, 512], BF16)
            KCb = cfg.BW // 128

            def epi(b, c0, cw, t0, tn, pis):
                for n in range(3):
                    dma("sync", gts[n][0][0:cw, 0:tn], G["gate"][b, n * D + c0:n * D + c0 + cw, t0:t0 + tn], r=[("gate", b)], w=[gts[n][1]])
                tt(m0[0:cw, 0:tn], ps[pis[0]][0:cw, 0:tn], gts[0][0][0:cw, 0:tn], ALU.mult, r=[PSK(pis[0]), gts[0][1]], w=[m0k])
                tt(m1[0:cw, 0:tn], ps[pis[1]][0:cw, 0:tn], gts[1][0][0:cw, 0:tn], ALU.mult, r=[PSK(pis[1]), gts[1][1]], w=[m1k])
                tt(m0[0:cw, 0:tn], m0[0:cw, 0:tn], m1[0:cw, 0:tn], ALU.add, r=[m0k, m1k], w=[m0k])
                tt(m1[0:cw, 0:tn], ps[pis[2]][0:cw, 0:tn], gts[2][0][0:cw, 0:tn], ALU.mult, r=[PSK(pis[2]), gts[2][1]], w=[m1k])
                tt(ob[0:cw, 0:tn], m0[0:cw, 0:tn], m1[0:cw, 0:tn], ALU.add, r=[m0k, m1k], w=[obk])
                dma("sync", G["mergedT"][b, c0:c0 + cw, t0:t0 + tn], ob[0:cw, 0:tn], r=[obk], w=[("mergedT", b)])

            cbs = [(c, 128, "F", epi) for c in range(0, D, 128)]
            wbr = I["w_branch"][l].rearrange("n w d -> (n w) d")
            linear(3 * KCb, lambda b, t0, tn: G["yT"][b, :, t0:t0 + tn], ["yT"],
                   lambda k0, k1, c0, c1: wbr[k0:k1, c0:c1], cbs,
                   kgroups=[(0, KCb), (KCb, 2 * KCb), (2 * KCb, 3 * KCb)], wcols=256, nabuf=1)

        def stage_wout(l):
            A.reset()
            epi = make_store_epi(G["y2T"], "y2T", F32, "F", 0)
            cbs = [(c, 128, "F", epi) for c in range(0, D, 128)]
            linear(KC, lambda b, t0, tn: G["mergedT"][b, :, t0:t0 + tn], ["mergedT"],
                   lambda k0, k1, c0, c1: I["w_out"][l, k0:k1, c0:c1], cbs)

        def stage_ln(l, which):
            A.reset()
            TB = 256
            gi = 2 if which == 0 else 5
            lg_i, lb_i = (0, 1) if which == 0 else (2, 3)
            srcn = "y2T" if which == 0 else "faccT"
            x_t, xk = A.tile([128, KC, TB]); y_t, yk = A.tile([128, KC, TB])
            sqs = [A.tile([128, TB]) for _ in range(2)]
            mean, mk = A.tile([128, TB]); rstd, rk_ = A.tile([128, TB]); t1s = [A.tile([128, TB]) for _ in range(2)]
            if which == 0:
                h2f, h2fk = A.tile([128, KC, TB]); h2b, h2bk = A.tile([128, KC, TB], BF16)
                lgT, lgTk = A.tile([16, TB]); cT, cTk = A.tile([16, TB])
                lg, lgk = A.tile([128, 16]); ee, eek = A.tile([128, 16]); selm, selmk = A.tile([128, 16])
                mx, mxk = A.tile([128, 4]); psm, psmk = A.tile([128, 4]); gsc, gsck = A.tile([128, 4]); gsel, gselk = A.tile([128, 4])
                cntt, cnttk = A.tile([128, 4]); cmpt, cmptk = A.tile([128, 4]); den, denk = A.tile([128, 4])
                dma("sync", wr[:], I["w_router"].rearrange("(k p) e -> p k e", p=128), w=["wr"])
                dma("sync", brb[:], I["b_router"].partition_broadcast(128), w=["brb"])
            for b in range(NB):
                for (t0, tn, isc) in cfg.tblocks(TB):
                    row = NB if isc else b
                    dma("sync", x_t[:, :, 0:tn], G["xT"][b, :, t0:t0 + tn].rearrange("(k p) t -> p k t", p=128), r=[("xT", b)], w=[xk])
                    dma(STORE_Q, y_t[:, :, 0:tn], G[srcn][b, :, t0:t0 + tn].rearrange("(k p) t -> p k t", p=128), r=[(srcn, b)], w=[yk])
                    for k in range(KC):
                        stt(x_t[:, k, 0:tn], y_t[:, k, 0:tn], modga[:, gi * KC + k, row:row + 1], x_t[:, k, 0:tn], ALU.mult, ALU.add,
                            r=[xk, yk, "modga"], w=[xk])
                    for k in range(KC):
                        mm(ps[0][:, 0:tn], ones, x_t[:, k, 0:tn], k == 0, k == KC - 1, r=["cmat", xk], w=[PSK(0)])
                    for k in range(KC):
                        sq, sqk = sqs[k % 2]
                        act(sq[:, 0:tn], x_t[:, k, 0:tn], AF.Square, r=[xk], w=[sqk])
                        mm(ps[1][:, 0:tn], ones, sq[:, 0:tn], k == 0, k == KC - 1, r=["cmat", sqk], w=[PSK(1)])
                    ts(mean[:, 0:tn], ps[0][:, 0:tn], 1.0 / D, None, ALU.mult, r=[PSK(0)], w=[mk])
                    tt(rstd[:, 0:tn], mean[:, 0:tn], mean[:, 0:tn], ALU.mult, r=[mk], w=[rk_])
                    stt(rstd[:, 0:tn], ps[1][:, 0:tn], 1.0 / D, rstd[:, 0:tn], ALU.mult, ALU.subtract, r=[PSK(1), rk_], w=[rk_])
                    ts(rstd[:, 0:tn], rstd[:, 0:tn], EPS / (cfg.alpha ** 2), None, ALU.add, r=[rk_], w=[rk_])
                    act(rstd[:, 0:tn], rstd[:, 0:tn], AF.Sqrt, r=[rk_], w=[rk_])
                    recip(rstd[:, 0:tn], rstd[:, 0:tn], r=[rk_], w=[rk_])
                    for k in range(KC):
                        t1, t1k = t1s[k % 2]
                        tt(t1[:, 0:tn], x_t[:, k, 0:tn], mean[:, 0:tn], ALU.subtract, r=[xk, mk], w=[t1k])
                        tt(t1[:, 0:tn], t1[:, 0:tn], rstd[:, 0:tn], ALU.mult, r=[t1k, rk_], w=[t1k])
                        ts(x_t[:, k, 0:tn], t1[:, 0:tn], lnv[:, lg_i, k:k + 1], lnv[:, lb_i, k:k + 1], ALU.mult, ALU.add, r=[t1k, "lnv"], w=[xk])
                    dma("sync", G["xT"][b, :, t0:t0 + tn].rearrange("(k p) t -> p k t", p=128), x_t[:, :, 0:tn], r=[xk], w=[("xT", b)])
                    if which != 0:
                        continue
                    for k in range(KC):
                        ts(h2f[:, k, 0:tn], x_t[:, k, 0:tn], mod1p[:, 4 * KC + k, row:row + 1], modT[:, 3 * KC + k, row:row + 1], ALU.mult, ALU.add,
                           r=[xk, "mod1p", "modT"], w=[h2fk])
                    cp(h2b[:, :, 0:tn], h2f[:, :, 0:tn], r=[h2fk], w=[h2bk], eng="scalar")
                    dma(STORE_Q, G["hT"][b, :, t0:t0 + tn].rearrange("(k p) t -> p k t", p=128), h2b[:, :, 0:tn], r=[h2bk], w=[("hT", b)])
                    for k in range(KC):
                        mm(ps[2][0:16, 0:tn], wr[:, k, :], h2f[:, k, 0:tn], k == 0, k == KC - 1, r=["wr", h2fk], w=[PSK(2)])
                    cp(lgT[:, 0:tn], ps[2][0:16, 0:tn], r=[PSK(2)], w=[lgTk])
                    for s_ in range(tn // 128):
                        tr(ps[3][:, 0:16], lgT[:, s_ * 128:(s_ + 1) * 128], ident[0:16, 0:16], r=[lgTk, "cmat"], w=[PSK(3)])
                        tt(lg, ps[3][:, 0:16], brb[:], ALU.add, r=[PSK(3), "brb"], w=[lgk])
                        S.op("vector", lambda e: e.reduce_max(out=mx[:, 0:1], in_=lg, axis=AX.X), reads=[lgk], writes=[mxk])
                        ts(mx[:, 0:1], mx[:, 0:1], -1.0, None, ALU.mult, r=[mxk], w=[mxk])
                        act(ee, lg, AF.Exp, r=[lgk, mxk], w=[eek], bias=mx[:, 0:1], scale=1.0)
                        e3 = ee.rearrange("p (g i) -> p g i", i=4)
                        first = True
                        for i_ in range(4):
                            for j_ in range(i_ + 1, 4):
                                tt(psm, e3[:, :, i_], e3[:, :, j_], ALU.add, r=[eek], w=[psmk])
                                if first:
                                    cp(gsc, psm, r=[psmk], w=[gsck]); first = False
                                else:
                                    tt(gsc, gsc, psm, ALU.max, r=[gsck, psmk], w=[gsck])
                        S.op("vector", lambda e: e.reduce_max(out=mx[:, 1:2], in_=gsc, axis=AX.X), reads=[gsck], writes=[mxk])
                        ts(gsel, gsc, mx[:, 1:2], None, ALU.is_ge, r=[gsck, mxk], w=[gselk])
                        s3 = selm.rearrange("p (g i) -> p g i", i=4)
                        for i_ in range(4):
                            vmemset(cntt, 0.0, [cnttk])
                            for j_ in range(4):
                                if j_ == i_:
                                    continue
                                tt(cmpt, e3[:, :, j_], e3[:, :, i_], ALU.is_gt, r=[eek], w=[cmptk])
                                tt(cntt, cntt, cmpt, ALU.add, r=[cnttk, cmptk], w=[cnttk])
                            ts(cntt, cntt, 1.5, None, ALU.is_lt, r=[cnttk], w=[cnttk])
                            tt(s3[:, :, i_], cntt, gsel, ALU.mult, r=[cnttk, gselk], w=[selmk])
                        tt(selm, selm, ee, ALU.mult, r=[selmk, eek], w=[selmk])
                        S.op("vector", lambda e: e.reduce_sum(out=den[:, 0:1], in_=selm, axis=AX.X), reads=[selmk], writes=[denk])
                        recip(den[:, 0:1], den[:, 0:1], r=[denk], w=[denk])
                        ts(selm, selm, den[:, 0:1], None, ALU.mult, r=[selmk, denk], w=[selmk])
                        tr(ps[4][0:16, 0:128], selm, ident, r=[selmk, "cmat"], w=[PSK(4)])
                        cp(cT[:, s_ * 128:(s_ + 1) * 128], ps[4][0:16, 0:128], r=[PSK(4)], w=[cTk])
                    dma("sync", G["combT"][b, :, t0:t0 + tn], cT[:, 0:tn], r=[cTk], w=[("combT", b)])

        def stage_moe(l):
            DEc = cfg.DE // 128
            for e_ in range(16):
                A.reset()
                cbt, cbk = A.tile([32, 512]); cbb, cbbk = A.tile([128, 512])
                sg, sgk = A.tile([128, 512]); hb, hbk = A.tile([128, 512], BF16)
                vmemset(cbt, 0.0, [cbk])
                act(sg[:, 0:128], ones, AF.Silu, r=["cmat"], w=[sgk])
                state = {"key": None}

                def epi_gu(b, c0, cw, t0, tn, pis, e_=e_):
                    if state["key"] != (b, t0):
                        state["key"] = (b, t0)
                        dma("sync", cbt[0:16, 0:tn], G["combT"][b, :, t0:t0 + tn], r=[("combT", b), cbk], w=[cbk])
                        mm(ps[6][:, 0:tn], sel[:, e_ * 128:(e_ + 1) * 128], cbt[:, 0:tn], True, True, r=["sel", cbk], w=[PSK(6)])
                        cp(cbb[:, 0:tn], ps[6][:, 0:tn], r=[PSK(6)], w=[cbbk])
                    act(sg[0:cw, 0:tn], ps[pis[0]][0:cw, 0:tn], AF.Silu, r=[PSK(pis[0])], w=[sgk])
                    tt(sg[0:cw, 0:tn], sg[0:cw, 0:tn], ps[pis[1]][0:cw, 0:tn], ALU.mult, r=[sgk, PSK(pis[1])], w=[sgk])
                    tt(hb[0:cw, 0:tn], sg[0:cw, 0:tn], cbb[0:cw, 0:tn], ALU.mult, r=[sgk, cbbk], w=[hbk])
                    dma("sync", G["hid"][b, c0:c0 + cw, t0:t0 + tn], hb[0:cw, 0:tn], r=[hbk], w=[("hid", b)])

                gu_linear(l, e_, epi_gu)
                if "stop_e0" in dbg and l == 1:
                    return
                A.reset()
                ac, ack = A.tile([128, 512]); o2, o2k = A.tile([128, 512])

                def epi_d(b, c0, cw, t0, tn, pis, e_=e_):
                    if e_ == 0:
                        cp(o2[0:cw, 0:tn], ps[pis[0]][0:cw, 0:tn], r=[PSK(pis[0])], w=[o2k])
                    else:
                        dma("sync", ac[0:cw, 0:tn], G["faccT"][b, c0:c0 + cw, t0:t0 + tn], r=[("faccT", b)], w=[ack])
                        tt(o2[0:cw, 0:tn], ac[0:cw, 0:tn], ps[pis[0]][0:cw, 0:tn], ALU.add, r=[ack, PSK(pis[0])], w=[o2k])
                    dma("sync", G["faccT"][b, c0:c0 + cw, t0:t0 + tn], o2[0:cw, 0:tn], r=[o2k], w=[("faccT", b)])

                cbs = [(c, 128, "F", epi_d) for c in range(0, D, 128)]
                linear(DEc, lambda b, t0, tn: G["hid"][b, :, t0:t0 + tn], ["hid"],
                       lambda k0, k1, c0, c1, e_=e_: I["w_exp_down"][l, e_, k0:k1, c0:c1], cbs)
                if "snap" in dbg and l == 1:
                    if "snap" not in dbg_out:
                        dbg_out["snap"] = nc.dram_tensor("snap", [16, 128, 8], F32, kind="ExternalOutput").ap()
                        dbg_out["snaph"] = nc.dram_tensor("snaph", [16, 128, 8], BF16, kind="ExternalOutput").ap()
                    S.barrier()
                    dma("sync", dbg_out["snap"][e_], G["faccT"][0, 0:128, 0:8], w=[("snap", e_)])
                    dma("sync", dbg_out["snaph"][e_], G["hid"][0, 0:128, 0:8], w=[("snaph", e_)])

        def gu_linear(l, e_, epi):
            wg = [A.tile([128, KC, 256], BF16) for _ in range(2)]
            ab = [A.tile([128, KC, 512], BF16) for _ in range(2)]
            ai = 0
            for hi, h0 in enumerate(range(0, cfg.DE, 128)):
                w_t, wk = wg[hi % 2]
                for kk in range(0, KC, 8):
                    ke = min(KC, kk + 8)
                    dma("gpsimd", w_t[:, kk:ke, 0:128], I["w_exp_gate"][l, e_, kk * 128:ke * 128, h0:h0 + 128].rearrange("(k p) c -> p k c", p=128), w=[(wk, "g", kk // 8)])
                    dma("gpsimd", w_t[:, kk:ke, 128:256], I["w_exp_up"][l, e_, kk * 128:ke * 128, h0:h0 + 128].rearrange("(k p) c -> p k c", p=128), w=[(wk, "u", kk // 8)])
                for b in range(NB):
                    for (t0, tn, isc) in cfg.tblocks(512):
                        a_t, ak = ab[ai % 2]
                        ai += 1
                        dma("sync", a_t[:, :, 0:tn], G["hT"][b, :, t0:t0 + tn].rearrange("(k p) t -> p k t", p=128), r=[("hT", b)], w=[ak])
                        pg, pu = 0 + (ai % 2) * 2, 1 + (ai % 2) * 2
                        for k in range(KC):
                            mm(ps[pg][:, 0:tn], w_t[:, k, 0:128], a_t[:, k, 0:tn], k == 0, k == KC - 1, r=[(wk, "g", k // 8), ak], w=[PSK(pg)])
                        for k in range(KC):
                            mm(ps[pu][:, 0:tn], w_t[:, k, 128:256], a_t[:, k, 0:tn], k == 0, k == KC - 1, r=[(wk, "u", k // 8), ak], w=[PSK(pu)])
                        epi(b, h0, 128, t0, tn, [pg, pu])

        def stage_final():
            A.reset()
            xt, xtk = A.tile([128, KC, 512]); ot, otk = A.tile([128, D])
            outs = []
            for b in range(NB):
                for (t0, tn, isc) in cfg.tblocks(512):
                    if isc:
                        continue
                    dma("sync", xt[:, :, 0:tn], G["xT"][b, :, t0:t0 + tn].rearrange("(k p) t -> p k t", p=128), r=[("xT", b)], w=[xtk])
                    for s_ in range(tn // 128):
                        for k4 in range(0, KC, 4):
                            pi = nextps()
                            for k in range(k4, min(KC, k4 + 4)):
                                tr(ps[pi][:, (k - k4) * 128:(k - k4 + 1) * 128], xt[:, k, s_ * 128:(s_ + 1) * 128], ident, r=[xtk, "cmat"], w=[PSK(pi)])
                            nk = min(KC, k4 + 4) - k4
                            cp(ot[:, k4 * 128:(k4 + nk) * 128], ps[pi][:, 0:nk * 128], r=[PSK(pi)], w=[otk], eng="scalar" if (k4 // 4) % 2 else "vector")
                        outs.append(dma("sync", out[b, t0 + s_ * 128:t0 + (s_ + 1) * 128, :], ot[:, :], r=[otk], w=[("out", b)]))
            S.finish(outs)

        stages = []
        stages.append(("s", stage_s))
        stages.append(("init", stage_init))
        for l in range(L):
            stages.append(("mod%d" % l, lambda l=l: stage_mod(l)))
            stages.append(("modulate%d" % l, lambda: stage_modulate(0, 1)))
            stages.append(("inproj%d" % l, lambda l=l: stage_inproj(l)))
            stages.append(("qk%d" % l, lambda l=l: stage_qk(l)))
            stages.append(("attn%d" % l, lambda l=l: stage_attn(l)))
            stages.append(("hgrn%d" % l, lambda l=l: stage_hgrn(l)))
            stages.append(("mlstm%d" % l, lambda l=l: stage_mlstm(l)))
            stages.append(("merge%d" % l, lambda l=l: stage_merge(l)))
            stages.append(("wout%d" % l, lambda l=l: stage_wout(l)))
            stages.append(("ln1_%d" % l, lambda l=l: stage_ln(l, 0)))
            stages.append(("moe%d" % l, lambda l=l: stage_moe(l)))
            stages.append(("ln2_%d" % l, lambda l=l: stage_ln(l, 1)))
        stages.append(("final", stage_final))
        for name, fn in stages:
            fn()
            if dbg and 'verbose' in dbg:
                print(name, {e: (len(v), sum(1 for o in v if o.signals)) for e, v in S.ops.items()}, flush=True)
            if stop_after == name:
                break

        S.barrier()
        if stop_after not in (None, "final"):
            fin = S.op("sync", lambda e: e.dma_start(out=out[0, 0:1, 0:16], in_=I["x"][0, 0:1, 0:16]), dma=True)
            S.finish([fin])
        S.emit()
        if dbg and 'verbose' in dbg:
            print('odd DMAs', len(odd_log), sorted(set(odd_log))[:20])
    return nc, dbg_out


N_CORES_USED = 4


def kernel(**inputs):
    cfg = Cfg(D=4096, LAT=2048, CTX=256, NB=1, DEPTH=2)
    nc, _ = build(cfg)
    consts = host_consts(cfg)
    in_maps = []
    for b in range(N_CORES_USED):
        m = {}
        for k, v in inputs.items():
            v = np.asarray(v)
            if k in ("x", "c", "ctx"):
                m[k] = np.ascontiguousarray(v[b:b + 1])
            else:
                m[k] = v
        m.update(consts)
        in_maps.append(m)
    res = run_bass_kernel_spmd(nc, in_maps, core_ids=list(range(N_CORES_USED)))
    return np.concatenate([np.asarray(r["out"]) for r in res.results], axis=0).astype(np.float32)
```

```python
import numpy as np
import concourse.bass as bass
import concourse.mybir as mybir
from concourse.bass_utils import run_bass_kernel_spmd
from contextlib import ExitStack

F32 = mybir.dt.float32
BF16 = mybir.dt.bfloat16
AF = mybir.ActivationFunctionType
ALU = mybir.AluOpType
AX = mybir.AxisListType

ENGS = ("tensor", "vector", "scalar", "gpsimd", "sync")
MAXV = 8000
NDMASEM = {"sync": 56, "gpsimd": 16, "tensor": 4, "vector": 4, "scalar": 4}
EPS = 1e-6
PRUNE_WAR = True
DMA_WINDOW = 24
SETTLE_N = 16
SETTLE_ROWS = 16
STORE_Q = "sync"
ODD_POOL = True


class Op:
    __slots__ = ("eng", "fn", "deps", "signals", "sem", "val", "is_dma", "prev_same_sem", "throttle")

    def __init__(self, eng, fn, is_dma):
        self.eng = eng
        self.fn = fn
        self.deps = []
        self.signals = False
        self.sem = None
        self.val = 0
        self.is_dma = is_dma
        self.prev_same_sem = None
        self.throttle = None


class Sched:
    def __init__(self, nc, es):
        self.nc = nc
        self.es = es
        self.ops = {e: [] for e in ENGS}
        self.last_w = {}
        self.readers = {}
        self.sems = {}
        self.dma_sems = {}
        self.dma_cnt = {e: 0 for e in ENGS}
        self.dma_last = {}
        self.dma_hist = {}
        self.final_waits = []
        self.bar = []
        self.since_bar = []

    def _newsem(self, name):
        return self.es.enter_context(self.nc.semaphore(name))

    def op(self, eng, fn, reads=(), writes=(), dma=False, odd=False):
        o = Op(eng, fn, dma)
        deps = list(self.bar)
        for r in reads:
            w = self.last_w.get(r)
            if w is not None:
                deps.append(w)
        for r in writes:
            w = self.last_w.get(r)
            if w is not None:
                deps.append(w)
            lastrd = {}
            for rd in self.readers.get(r, ()):
                if rd.is_dma or not PRUNE_WAR:
                    deps.append(rd)
                else:
                    lastrd[rd.eng] = rd
            deps.extend(lastrd.values())
        seen = set()
        for d in deps:
            if id(d) in seen:
                continue
            seen.add(id(d))
            if d.eng == "tensor" and eng == "tensor" and not d.is_dma and not dma:
                continue
            o.deps.append(d)
            d.signals = True
        for r in writes:
            self.last_w[r] = o
            self.readers[r] = []
        for r in reads:
            self.readers.setdefault(r, []).append(o)
        if dma:
            pool = eng + ("_odd" if odd else "")
            self.dma_cnt.setdefault(pool, 0)
            k = self.dma_cnt[pool] % (4 if odd else NDMASEM[eng])
            self.dma_cnt[pool] += 1
            key = (pool, k)
            hist = self.dma_hist.setdefault(eng, [])
            o.throttle = hist[-DMA_WINDOW] if len(hist) >= DMA_WINDOW else None
            hist.append(o)
            o.prev_same_sem = self.dma_last.get(key)
            self.dma_last[key] = o
            o.sem = key
            o.signals = True
        self.ops[eng].append(o)
        self.since_bar.append(o)
        return o

    def barrier(self):
        last = {}
        dmas = []
        for o in self.since_bar:
            if o.is_dma:
                dmas.append(o)
            else:
                last[o.eng] = o
        self.bar = list(last.values()) + dmas
        for o in self.bar:
            o.signals = True
        self.since_bar = []
        self.last_w = {}
        self.readers = {}

    def finish(self, ops):
        self.final_waits.extend(ops)
        for o in ops:
            o.signals = True

    def emit(self):
        nc = self.nc
        cnt = {e: 0 for e in ENGS}
        dcnt = {}
        for e in ENGS:
            for o in self.ops[e]:
                if o.is_dma:
                    dcnt[o.sem] = dcnt.get(o.sem, 0) + 1
                    o.val = 16 * dcnt[o.sem]
                    if o.sem not in self.dma_sems:
                        self.dma_sems[o.sem] = self._newsem("d%s%d" % (o.sem[0], o.sem[1]))
                elif o.signals:
                    n = cnt[e]
                    cnt[e] += 1
                    key = (e, n // MAXV)
                    if key not in self.sems:
                        self.sems[key] = self._newsem("c%s%d" % (e[:2], key[1]))
                    o.sem = key
                    o.val = n % MAXV + 1
        allsem = dict(self.sems)
        allsem.update(self.dma_sems)
        self.stats = {"compute_sems": len(self.sems), "dma_sems": len(self.dma_sems), "signals": dict(cnt),
                      "max_dma_val": max([16 * v for v in dcnt.values()] + [0])}

        def run(ename):
            def body(eng):
                seen = {}

                def wait(d):
                    if seen.get(d.sem, 0) >= d.val:
                        return
                    seen[d.sem] = d.val
                    eng.wait_ge(allsem[d.sem], d.val)

                for o in self.ops[ename]:
                    for d in o.deps:
                        wait(d)
                    if o.is_dma and o.prev_same_sem is not None:
                        wait(o.prev_same_sem)
                    if o.is_dma and o.throttle is not None:
                        wait(o.throttle)
                    ins = o.fn(eng)
                    if o.signals:
                        ins.then_inc(allsem[o.sem], 16 if o.is_dma else 1)
                if ename == "sync":
                    for d in self.final_waits:
                        wait(d)
            return body

        with nc.Block() as block:
            for e in ENGS:
                if self.ops[e] or (e == "sync" and self.final_waits):
                    getattr(block, e)(run(e))


class Cfg:
    def __init__(self, D=4096, LAT=2048, CTX=256, NB=1, DEPTH=2):
        self.D, self.LAT, self.CTX, self.NB, self.DEPTH = D, LAT, CTX, NB, DEPTH
        self.TOK = LAT + CTX
        self.KC = D // 128
        self.AH = D // 256
        self.AKV = self.AH // 4
        self.HGH = D // 256
        self.MLH = D // 512
        self.BW = self.AH * 128
        self.NE = 16
        self.DE = D // 4
        segs = [("attn_q", self.AH * 128), ("attn_k", self.AKV * 128), ("attn_v", self.AKV * 128),
                ("hg_q", self.HGH * 128), ("hg_f_fwd", self.HGH * 128), ("hg_f_bwd", self.HGH * 128),
                ("hg_i", self.HGH * 128), ("hg_g", self.HGH * 128), ("ml_q", self.MLH * 128),
                ("ml_k", self.MLH * 128), ("ml_v", self.MLH * 256), ("ml_gates", 4 * self.MLH),
                ("ml_o", self.MLH * 256), ("merge", 3 * D)]
        self.segs = segs
        self.off = {}
        s = 0
        for n, w in segs:
            self.off[n] = (s, w)
            s += w
        self.IN_COLS = s
        self.alpha = float((2 * DEPTH) ** 0.25)

    def tblocks(self, width=512):
        out = []
        t = 0
        while t < self.LAT:
            w = min(width, self.LAT - t)
            out.append((t, w, False))
            t += w
        t = self.LAT
        while t < self.TOK:
            w = min(width, self.TOK - t)
            out.append((t, w, True))
            t += w
        return out


def host_consts(cfg):
    c = {}
    ident = np.eye(128, dtype=np.float32)
    ones = np.ones((128, 128), np.float32)
    Rm = np.zeros((128, 128), np.float32)
    for a in range(2):
        for p in range(32):
            m0 = a * 64 + p
            m1 = a * 64 + 32 + p
            Rm[m1, m0] = -1.0
            Rm[m0, m1] = 1.0
    s = np.arange(128)[:, None]
    t = np.arange(128)[None, :]
    same = (s // 32) == (t // 32)
    maskF = (same & (s <= t)).astype(np.float32)
    maskB = (same & (s >= t)).astype(np.float32)
    c["cmat"] = np.concatenate([ident, ones, Rm, maskF, maskB], axis=1)
    sel = np.zeros((32, 32 * 128), np.float32)
    for k in range(32):
        sel[k, k * 128:(k + 1) * 128] = 1.0
    c["sel"] = sel
    rm = np.ones((128, cfg.TOK), np.float32)
    rm[:, ::32] = 0.0
    c["rmask"] = rm
    tt = np.arange(cfg.LAT)
    row = (tt // 64).astype(np.float32)
    col = (tt % 64).astype(np.float32)
    inv = (10000.0 ** (-np.arange(32, dtype=np.float32) / 32)).astype(np.float32)
    ang = np.stack([row[:, None] * inv, col[:, None] * inv], axis=1).astype(np.float32)
    cosT = np.zeros((128, cfg.LAT), np.float32)
    sinT = np.zeros((128, cfg.LAT), np.float32)
    for a in range(2):
        for h in range(2):
            cosT[a * 64 + h * 32:a * 64 + h * 32 + 32, :] = np.cos(ang[:, a, :]).T
            sinT[a * 64 + h * 32:a * 64 + h * 32 + 32, :] = np.sin(ang[:, a, :]).T
    c["rope"] = np.stack([cosT, sinT], 0).astype(np.float32)
    return c


class B:
    pass


def build(cfg, dbg=(), stop_after=None):
    nc = bass.Bass("TRN2", target_bir_lowering=False)
    D, KC, TOK, LAT, CTX, NB = cfg.D, cfg.KC, cfg.TOK, cfg.LAT, cfg.CTX, cfg.NB
    NR = NB + 1
    L = cfg.DEPTH

    def din(name, shape, dt=F32):
        return nc.dram_tensor(name, list(shape), dt, kind="ExternalInput").ap()

    I = {}
    I["x"] = din("x", [NB, LAT, D])
    I["c"] = din("c", [NB, D])
    I["ctx"] = din("ctx", [NB, CTX, D])
    I["c_ctx"] = din("c_ctx", [D])
    I["w_mod"] = din("w_mod", [L, D, 6 * D])
    I["b_mod"] = din("b_mod", [L, 6 * D])
    I["w_in"] = din("w_in", [L, D, cfg.IN_COLS])
    I["ml_gate_bias"] = din("ml_gate_bias", [L, 4 * cfg.MLH])
    I["attn_q_norm"] = din("attn_q_norm", [L, 128])
    I["attn_k_norm"] = din("attn_k_norm", [L, 128])
    I["hg_lb_logits"] = din("hg_lb_logits", [2, L, cfg.HGH * 128])
    I["hg_norm"] = din("hg_norm", [L, 128])
    I["ml_norm"] = din("ml_norm", [L, 256])
    I["w_branch"] = din("w_branch", [L, 3, cfg.BW, D])
    I["w_out"] = din("w_out", [L, D, D])
    for n in ("ln1_g", "ln1_b", "ln2_g", "ln2_b"):
        I[n] = din(n, [L, D])
    I["w_router"] = din("w_router", [D, 16])
    I["b_router"] = din("b_router", [16])
    I["w_exp_gate"] = din("w_exp_gate", [L, 16, D, cfg.DE])
    I["w_exp_up"] = din("w_exp_up", [L, 16, D, cfg.DE])
    I["w_exp_down"] = din("w_exp_down", [L, 16, cfg.DE, D])
    I["cmat"] = din("cmat", [128, 640])
    I["sel"] = din("sel", [32, 32 * 128])
    I["rmask"] = din("rmask", [128, TOK])
    I["rope"] = din("rope", [2, 128, LAT])
    out = nc.dram_tensor("out", [NB, LAT, D], F32, kind="ExternalOutput").ap()

    dbg_out = {}

    def dscr(name, shape, dt):
        if name in dbg:
            ap = nc.dram_tensor(name, list(shape), dt, kind="ExternalOutput").ap()
            dbg_out[name] = ap
            return ap
        return nc.dram_tensor(name, list(shape), dt, kind="Internal").ap()

    G = {}
    G["xT"] = dscr("xT", [NB, D, TOK], F32)
    NBLK = (LAT + 511) // 512 + (CTX + 511) // 512
    G["hT"] = dscr("hT", [NB, NBLK, 128, KC, 512], BF16)

    def hT_ap(b, t0, tn):
        if t0 >= LAT:
            blk, off = (LAT + 511) // 512 + (t0 - LAT) // 512, (t0 - LAT) % 512
        else:
            blk, off = t0 // 512, t0 % 512
        assert off + tn <= 512
        return G["hT"][b, blk, :, :, off:off + tn]
    G["qraw"] = dscr("qraw", [NB, cfg.AH * 128, TOK], F32)
    G["kraw"] = dscr("kraw", [NB, cfg.AKV * 128, TOK], F32)
    G["v_tok"] = dscr("v_tok", [NB, TOK, cfg.AKV * 128], BF16)
    G["QT"] = dscr("QT", [NB, cfg.AH * 128, TOK], BF16)
    G["KT"] = dscr("KT", [NB, cfg.AKV * 128, TOK], BF16)
    G["hgq"] = dscr("hgq", [NB, cfg.HGH * 128, TOK], F32)
    G["hgff"] = dscr("hgff", [NB, cfg.HGH * 128, TOK], F32)
    G["hgfb"] = dscr("hgfb", [NB, cfg.HGH * 128, TOK], F32)
    G["hgi_tok"] = dscr("hgi_tok", [NB, TOK, cfg.HGH * 128], BF16)
    G["hgg"] = dscr("hgg", [NB, cfg.HGH * 128, TOK], F32)
    G["mlq"] = dscr("mlq", [NB, cfg.MLH * 128, TOK], F32)
    G["mlk"] = dscr("mlk", [NB, cfg.MLH * 128, TOK], F32)
    G["mlv_tok"] = dscr("mlv_tok", [NB, TOK, cfg.MLH * 256], BF16)
    G["mlg"] = dscr("mlg", [NB, 4 * cfg.MLH, TOK], F32)
    G["mlo"] = dscr("mlo", [NB, cfg.MLH * 256, TOK], F32)
    G["gate"] = dscr("gate", [NB, 3 * D, TOK], BF16)
    G["yT"] = dscr("yT", [NB, 3 * cfg.BW, TOK], BF16)
    G["mergedT"] = dscr("mergedT", [NB, D, TOK], BF16)
    G["y2T"] = dscr("y2T", [NB, D, TOK], F32)
    G["combT"] = dscr("combT", [NB, 16, TOK], F32)
    G["faccT"] = dscr("faccT", [NB, D, TOK], F32)
    G["hid"] = dscr("hid", [NB, cfg.DE, TOK], BF16)

    with ExitStack() as es:
        S = Sched(nc, es)
        def P(name, shape, dt=F32):
            return es.enter_context(nc.sbuf_tensor("sb_" + name, list(shape), dt))

        cmat = P("cmat", [128, 640])
        cmatb = P("cmatb", [128, 640], BF16)
        sel = P("sel", [32, 32 * 128])
        ident = cmat[:, 0:128]
        ones = cmat[:, 128:256]
        Rm = cmat[:, 256:384]
        identb = cmatb[:, 0:128]
        onesb = cmatb[:, 128:256]
        maskFb = cmatb[:, 384:512]
        maskBb = cmatb[:, 512:640]
        sT = P("sT", [128, KC, NR])
        modT = P("modT", [128, 6 * KC, NR])
        mod1p = P("mod1p", [128, 6 * KC, NR])
        modga = P("modga", [128, 6 * KC, NR])
        bmT = P("bmT", [128, 6 * KC])
        lnv = P("lnv", [128, 4, KC])
        qn_g = P("qn_g", [128, 4])
        mln_g = P("mln_g", [128, 2])
        lbv = P("lbv", [128, 2, 3, cfg.HGH])
        mlb = P("mlb", [32, 2])
        wr = P("wr", [128, KC, 16])
        brb = P("brb", [128, 16])
        ARENA_BYTES = 176 * 1024
        SETTLE = SETTLE_N
        settle_t = P("settle", [SETTLE_ROWS, 16])
        arena = es.enter_context(nc.sbuf_tensor("arena", [128, ARENA_BYTES // 4], F32))
        arena_off = [0]
        arena_cnt = [0]

        a_base = nc.sbuf_base - ARENA_BYTES if False else None

        ps = [es.enter_context(nc.psum_tensor("ps%d" % i, [128, 512], F32)) for i in range(7)]
        psb = es.enter_context(nc.psum_tensor("psb", [128, 1024], BF16))
        ps_rr = [0]

        def nextps(lo=0, hi=7):
            i = lo + ps_rr[0] % (hi - lo)
            ps_rr[0] += 1
            return i

        class Arena:
            def __init__(self):
                self.off = 0

            def reset(self):
                S.barrier()
                self.off = 0
                for i in range(SETTLE):
                    dma("sync", settle_t[:, :], I["cmat"][0:SETTLE_ROWS, 0:16], r=["settle"], w=["settle"])
                if SETTLE:
                    S.barrier()

            def tile(self, shape, dt=F32):
                n = int(np.prod(shape[1:]))
                esz = 4 if dt == F32 else 2
                nbytes = (n * esz + 31) // 32 * 32
                assert self.off + nbytes <= ARENA_BYTES, ("arena overflow", self.off, nbytes)
                w0 = self.off // 4
                flat = arena[:, w0:w0 + nbytes // 4]
                self.off += nbytes
                if dt != F32:
                    flat = flat.bitcast(dt)
                flat = flat[:, 0:n]
                if len(shape) == 3:
                    flat = flat.rearrange("p (a b) -> p a b", a=shape[1])
                elif len(shape) == 4:
                    flat = flat.rearrange("p (a b c) -> p a b c", a=shape[1], b=shape[2])
                if shape[0] < 128:
                    flat = flat[0:shape[0]]
                arena_cnt[0] += 1
                return flat, ("ar", arena_cnt[0])

        A = Arena()

        def dma(q, out_, in_, r=(), w=(), **kw):
            r = [k for k in r if not (isinstance(k, tuple) and k[0] in G)]
            w = [k for k in w if not (isinstance(k, tuple) and k[0] in G)]

            def outer(ap_):
                for d_ in ap_.shape:
                    if d_ > 1:
                        return d_
                return 1
            odd = ODD_POOL and ((outer(out_) % 16 != 0) or (outer(in_) % 16 != 0))
            if odd:
                odd_log.append((tuple(out_.shape), tuple(in_.shape)))
            return S.op(q, lambda e: e.dma_start(out=out_, in_=in_, **kw), reads=r, writes=w, dma=True, odd=odd)

        def mm(out_, lhsT, rhs, start, stop, r=(), w=()):
            return S.op("tensor", lambda e: e.matmul(out_, lhsT=lhsT, rhs=rhs, start=start, stop=stop), reads=r, writes=w)

        def tr(out_, in_, idn, r=(), w=()):
            return S.op("tensor", lambda e: e.transpose(out_, in_, idn), reads=r, writes=w)

        def act(out_, in_, func, r=(), w=(), eng="scalar", **kw):
            return S.op("scalar", lambda e: e.activation(out=out_, in_=in_, func=func, **kw), reads=r, writes=w)

        def tt(out_, in0, in1, op, r=(), w=(), eng="vector"):
            return S.op(eng, lambda e: e.tensor_tensor(out=out_, in0=in0, in1=in1, op=op), reads=r, writes=w)

        def ts(out_, in0, s1, s2, op0, op1=None, r=(), w=(), eng="vector"):
            if op1 is None:
                return S.op(eng, lambda e: e.tensor_scalar(out=out_, in0=in0, scalar1=s1, scalar2=None, op0=op0), reads=r, writes=w)
            return S.op(eng, lambda e: e.tensor_scalar(out=out_, in0=in0, scalar1=s1, scalar2=s2, op0=op0, op1=op1), reads=r, writes=w)

        def stt(out_, in0, sc, in1, op0, op1, r=(), w=()):
            return S.op("vector", lambda e: e.scalar_tensor_tensor(out=out_, in0=in0, scalar=sc, in1=in1, op0=op0, op1=op1), reads=r, writes=w)

        def cp(out_, in_, r=(), w=(), eng="vector"):
            if eng == "scalar":
                return S.op("scalar", lambda e: e.copy(out=out_, in_=in_), reads=r, writes=w)
            return S.op(eng, lambda e: e.tensor_copy(out=out_, in_=in_), reads=r, writes=w)

        def recip(out_, in_, r=(), w=()):
            return S.op("vector", lambda e: e.reciprocal(out=out_, in_=in_), reads=r, writes=w)

        odd_log = []

        def PSK(i):
            return ("ps", i)

        dumped = set()

        def dbgdump(name, ap_, shape, dt, key):
            if name not in dbg or name in dumped:
                return
            dumped.add(name)
            o_ = nc.dram_tensor(name, list(shape), dt, kind="ExternalOutput").ap()
            dbg_out[name] = o_
            dma("sync", o_, ap_, r=[key], w=[("dbg", name)])

        dma("sync", cmat[:], I["cmat"][:, :], w=["cmat"])
        dma("sync", sel[:], I["sel"][:, :], w=["sel"])
        cp(cmatb[:], cmat[:], r=["cmat"], w=["cmatb"])

        def load_vecT(dst, vec_ap, n, key):
            t, tk = A.tile([n, 128])
            dma("sync", t, vec_ap.rearrange("(k p) -> k p", p=128), w=[tk])
            pi = nextps()
            tr(ps[pi][:, 0:n], t, ident[0:n, 0:n], r=[tk, "cmat"], w=[PSK(pi)])
            cp(dst, ps[pi][:, 0:n], r=[PSK(pi)], w=[key])

        def stage_s():
            A.reset()
            for rr in range(NR):
                src = I["c"][rr] if rr < NB else I["c_ctx"]
                t, tk = A.tile([KC, 128])
                dma("sync", t, src.rearrange("(k p) -> k p", p=128), w=[tk])
                pi = nextps()
                tr(ps[pi][:, 0:KC], t, ident[0:KC, 0:KC], r=[tk, "cmat"], w=[PSK(pi)])
                act(sT[:, :, rr], ps[pi][:, 0:KC], AF.Silu, r=[PSK(pi)], w=["sT"])

        def stage_mod(l):
            A.reset()
            n6 = 6 * KC
            j0 = 0
            while j0 < n6:
                nn = min(96, n6 - j0)
                load_vecT(bmT[:, j0:j0 + nn], I["b_mod"][l, j0 * 128:(j0 + nn) * 128], nn, "bmT")
                j0 += nn
            for i, nme in enumerate(("ln1_g", "ln1_b", "ln2_g", "ln2_b")):
                load_vecT(lnv[:, i, :], I[nme][l], KC, "lnv")
            wt = [A.tile([128, KC, 512]) for _ in range(2)]
            ncb = (6 * D) // 512
            for cb in range(ncb):
                w_t, wk = wt[cb % 2]
                for kk in range(0, KC, 8):
                    ke = min(KC, kk + 8)
                    dma("gpsimd" if (kk // 8) % 2 else "sync", w_t[:, kk:ke, :],
                        I["w_mod"][l, kk * 128:ke * 128, cb * 512:(cb + 1) * 512].rearrange("(k p) c -> p k c", p=128),
                        w=[(wk, kk // 8)])
                pi = nextps()
                for j in range(4):
                    for k in range(KC):
                        mm(ps[pi][:, j * NR:(j + 1) * NR], w_t[:, k, j * 128:(j + 1) * 128], sT[:, k, :],
                           k == 0, k == KC - 1, r=[(wk, k // 8), "sT"], w=[PSK(pi)])
                for j in range(4):
                    jj = cb * 4 + j
                    ts(modT[:, jj, :], ps[pi][:, j * NR:(j + 1) * NR], bmT[:, jj:jj + 1], None, ALU.add,
                       r=[PSK(pi), "bmT"], w=["modT"])
            ts(mod1p[:], modT[:], 1.0, None, ALU.add, r=["modT"], w=["mod1p"])
            ts(modga[:], modT[:], 1.0 / cfg.alpha, None, ALU.mult, r=["modT"], w=["modga"])

        def stage_init():
            A.reset()
            xin = [A.tile([128, 4, D]) for _ in range(1)]
            xo = [A.tile([128, KC, 512]) for _ in range(1)]
            for b in range(NB):
                for (t0, tn, isc) in cfg.tblocks(512):
                    xi, xik = xin[0]
                    nsub = tn // 128
                    src = I["ctx"][b, t0 - LAT:t0 - LAT + tn, :] if isc else I["x"][b, t0:t0 + tn, :]
                    dma("sync", xi[:, 0:nsub, :], src.rearrange("(s p) d -> p s d", p=128), w=[xik])
                    xt, xtk = xo[0]
                    for k in range(KC):
                        pi = nextps()
                        for s_ in range(nsub):
                            tr(ps[pi][:, s_ * 128:(s_ + 1) * 128], xi[:, s_, k * 128:(k + 1) * 128], ident,
                               r=[xik, "cmat"], w=[PSK(pi)])
                        if k % 2 == 0:
                            cp(xt[:, k, 0:tn], ps[pi][:, 0:tn], r=[PSK(pi)], w=[xtk])
                        else:
                            cp(xt[:, k, 0:tn], ps[pi][:, 0:tn], r=[PSK(pi)], w=[xtk], eng="scalar")
                    dma(STORE_Q, G["xT"][b, :, t0:t0 + tn].rearrange("(k p) t -> p k t", p=128), xt[:, :, 0:tn],
                        r=[xtk], w=[("xT", b)])

        def stage_modulate(i_shift, i_scale):
            A.reset()
            xb = [A.tile([128, KC, 256]) for _ in range(2)]
            hb = [A.tile([128, KC, 256], BF16) for _ in range(2)]
            it = 0
            for b in range(NB):
                for (t0, tn, isc) in cfg.tblocks(256):
                    xt, xk = xb[it % 2]
                    ht, hk = hb[it % 2]
                    it += 1
                    row = NB if isc else b
                    dma("sync", xt[:, :, 0:tn], G["xT"][b, :, t0:t0 + tn].rearrange("(k p) t -> p k t", p=128),
                        r=[("xT", b)], w=[xk])
                    for k in range(KC):
                        ts(ht[:, k, 0:tn], xt[:, k, 0:tn], mod1p[:, i_scale * KC + k, row:row + 1],
                           modT[:, i_shift * KC + k, row:row + 1], ALU.mult, ALU.add,
                           r=[xk, "mod1p", "modT"], w=[hk], eng="vector")
                    dma(STORE_Q, hT_ap(b, t0, tn), ht[:, :, 0:tn], r=[hk], w=[("hT", b)])

        def linear(Kc, src_fn, src_keys, w_fn, colblocks, kgroups=None, wcols=512, nabuf=2):
            if kgroups is None:
                kgroups = [(0, Kc)]
            wb = [A.tile([128, Kc, wcols], BF16) for _ in range(2)]
            ab = [A.tile([128, Kc, 512], BF16) for _ in range(nabuf)]
            wst = [A.tile([128, 4, wcols], F32) for _ in range(3)]
            wsc = [0]
            ai = 0
            tiles = []
            cur = []
            curw = 0
            for cbk in colblocks:
                if cur and (curw + cbk[1] > wcols or cbk[2] != cur[-1][2] or cbk[0] != cur[-1][0] + cur[-1][1]):
                    tiles.append(cur)
                    cur, curw = [], 0
                cur.append(cbk)
                curw += cbk[1]
            if cur:
                tiles.append(cur)
            for wi, tl in enumerate(tiles):
                w_t, wk = wb[wi % 2]
                c_lo = tl[0][0]
                c_w = sum(x[1] for x in tl)
                step = max(1, 8192 // max(c_w, 1) // 4)
                for kk in range(0, Kc, 4):
                    ke = min(Kc, kk + 4)
                    st_, stk_ = wst[wsc[0] % 3]
                    wsc[0] += 1
                    dma("sync", st_[:, 0:ke - kk, 0:c_w], w_fn(kk * 128, ke * 128, c_lo, c_lo + c_w).rearrange("(k p) c -> p k c", p=128),
                        w=[stk_])
                    cp(w_t[:, kk:ke, 0:c_w], st_[:, 0:ke - kk, 0:c_w], r=[stk_], w=[(wk, kk // 4)], eng="scalar" if wsc[0] % 2 else "vector")
                for b in range(NB):
                    for (t0, tn, isc) in cfg.tblocks(512):
                        a_t, ak = ab[ai % nabuf]
                        ai += 1
                        dma("sync", a_t[:, :, 0:tn], src_fn(b, t0, tn),
                            r=[(kname, b) for kname in src_keys], w=[ak])
                        for (c0, cw, layout, epi) in tl:
                            lc = c0 - c_lo
                            if layout == "F":
                                pis = []
                                for (k0, k1) in kgroups:
                                    pi = nextps()
                                    pis.append(pi)
                                    for k in range(k0, k1):
                                        mm(ps[pi][0:cw, 0:tn], w_t[:, k, lc:lc + cw], a_t[:, k, 0:tn], k == k0, k == k1 - 1,
                                           r=[(wk, k // 4), ak], w=[PSK(pi)])
                                epi(b, c0, cw, t0, tn, pis)
                            else:
                                for s_ in range(tn // 128):
                                    pi = nextps()
                                    for k in range(Kc):
                                        mm(ps[pi][:, 0:cw], a_t[:, k, s_ * 128:(s_ + 1) * 128], w_t[:, k, lc:lc + cw], k == 0, k == Kc - 1,
                                           r=[(wk, k // 4), ak], w=[PSK(pi)])
                                    epi(b, c0, cw, t0 + s_ * 128, 128, [pi])

        stg = {"bufs": None, "i": 0}

        def staging(dt):
            if stg["bufs"] is None or stg["arena"] != arena_cnt[0] - stg["n"]:
                pass
            return None

        def make_store_epi(dst, key, dt, layout, col_base, func=None, scale=None, pool=None):
            if pool is None:
                pool = {"bufs": [A.tile([128, 512], F32) for _ in range(4)], "cnt": [0]}
            bufs = [((t if dt == F32 else t.bitcast(BF16)[:, 0:512]), k) for (t, k) in pool["bufs"]]
            cnt = pool["cnt"]

            def epi(b, c0, cw, t0, tn, pis):
                pi = pis[0]
                st, sk = bufs[cnt[0] % 4]
                cnt[0] += 1
                if layout == "F":
                    src = ps[pi][0:cw, 0:tn]
                    dsts = st[0:cw, 0:tn]
                    dram = dst[b, c0 - col_base:c0 - col_base + cw, t0:t0 + tn]
                else:
                    src = ps[pi][:, 0:cw]
                    dsts = st[:, 0:cw]
                    dram = dst[b, t0:t0 + tn, c0 - col_base:c0 - col_base + cw]
                if func is not None:
                    act(dsts, src, func, r=[PSK(pi)], w=[sk])
                elif cnt[0] % 2:
                    cp(dsts, src, r=[PSK(pi)], w=[sk])
                else:
                    cp(dsts, src, r=[PSK(pi)], w=[sk], eng="scalar")
                dma("sync", dram, dsts, r=[sk], w=[(key, b)])
            return epi

        def stage_inproj(l):
            A.reset()
            spec = {
                "attn_q": ("qraw", F32, "F", None), "attn_k": ("kraw", F32, "F", None), "attn_v": ("v_tok", BF16, "T", None),
                "hg_q": ("hgq", F32, "F", None), "hg_f_fwd": ("hgff", F32, "F", None), "hg_f_bwd": ("hgfb", F32, "F", None),
                "hg_i": ("hgi_tok", BF16, "T", None), "hg_g": ("hgg", F32, "F", None),
                "ml_q": ("mlq", F32, "F", None), "ml_k": ("mlk", F32, "F", None), "ml_v": ("mlv_tok", BF16, "T", None),
                "ml_gates": ("mlg", F32, "F", None), "ml_o": ("mlo", F32, "F", None), "merge": ("gate", BF16, "F", AF.Sigmoid),
            }
            colblocks = []
            pool = {"bufs": [A.tile([128, 512], F32) for _ in range(4)], "cnt": [0]}
            for name, width in cfg.segs:
                o0, _ = cfg.off[name]
                gname, dt, lay, fn = spec[name]
                epi = make_store_epi(G[gname], gname, dt, lay, o0, func=fn, pool=pool)
                step = 128 if lay == "F" else 512
                c = 0
                while c < width:
                    cw = min(step, width - c)
                    colblocks.append((o0 + c, cw, lay, epi))
                    c += cw
            linear(KC, lambda b, t0, tn: hT_ap(b, t0, tn), ["hT"],
                   lambda k0, k1, c0, c1: I["w_in"][l, k0:k1, c0:c1], colblocks, wcols=512)


        def vmemset(ap_, val, w):
            return S.op("vector", lambda e: e.memset(ap_, val), writes=w)

        def scan(out_, d0, d1, r, w):
            return S.op("vector", lambda e: e.tensor_tensor_scan(out=out_, data0=d0, data1=d1, initial=0.0, op0=ALU.mult, op1=ALU.add), reads=r, writes=w)

        def load_col(dst, vec_ap, n, key):
            dma("sync", dst, vec_ap.rearrange("(p o) -> p o", o=1), w=[key])

        def rstd_from(ps_ap, n, tmp, out_, rk, wk_):
            ts(tmp, ps_ap, 1.0 / n, EPS, ALU.mult, ALU.add, r=rk, w=[wk_ + "_t"])
            act(out_, tmp, AF.Sqrt, r=[wk_ + "_t"], w=[wk_]); recip(out_, out_, r=[wk_], w=[wk_])

        def stage_qk(l):
            A.reset()
            load_col(qn_g[:, 0:1], I["attn_q_norm"][l], 128, "qn_g0")
            load_col(qn_g[:, 1:2], I["attn_k_norm"][l], 128, "qn_g1")
            T_ = lambda dt=F32: A.tile([128, 512], dt)
            q_t, sq_t, r1_t, rs_t, qn_t, cs_t, sn_t, a_t, b_t = [T_() for _ in range(9)]
            o_t = T_(BF16)
            for b in range(NB):
                for (src, dst, nh, gc) in (("qraw", "QT", cfg.AH, 0), ("kraw", "KT", cfg.AKV, 1)):
                    for h in range(nh):
                        for (t0, tn, isc) in cfg.tblocks(512):
                            dma("sync", q_t[0][:, 0:tn], G[src][b, h * 128:(h + 1) * 128, t0:t0 + tn], r=[(src, b)], w=[q_t[1]])
                            act(sq_t[0][:, 0:tn], q_t[0][:, 0:tn], AF.Square, r=[q_t[1]], w=[sq_t[1]])
                            pi = nextps()
                            mm(ps[pi][:, 0:tn], ones, sq_t[0][:, 0:tn], True, True, r=["cmat", sq_t[1]], w=[PSK(pi)])
                            ts(r1_t[0][:, 0:tn], ps[pi][:, 0:tn], 1.0 / 128, EPS, ALU.mult, ALU.add, r=[PSK(pi)], w=[r1_t[1]])
                            act(rs_t[0][:, 0:tn], r1_t[0][:, 0:tn], AF.Sqrt, r=[r1_t[1]], w=[rs_t[1]]); recip(rs_t[0][:, 0:tn], rs_t[0][:, 0:tn], r=[rs_t[1]], w=[rs_t[1]])
                            stt(qn_t[0][:, 0:tn], q_t[0][:, 0:tn], qn_g[:, gc:gc + 1], rs_t[0][:, 0:tn], ALU.mult, ALU.mult,
                                r=[q_t[1], rs_t[1], "qn_g%d" % gc], w=[qn_t[1]])
                            if not isc:
                                dma("sync", cs_t[0][:, 0:tn], I["rope"][0, :, t0:t0 + tn], w=[cs_t[1]])
                                dma("sync", sn_t[0][:, 0:tn], I["rope"][1, :, t0:t0 + tn], w=[sn_t[1]])
                                pr = nextps()
                                mm(ps[pr][:, 0:tn], Rm, qn_t[0][:, 0:tn], True, True, r=["cmat", qn_t[1]], w=[PSK(pr)])
                                tt(a_t[0][:, 0:tn], qn_t[0][:, 0:tn], cs_t[0][:, 0:tn], ALU.mult, r=[qn_t[1], cs_t[1]], w=[a_t[1]])
                                tt(b_t[0][:, 0:tn], ps[pr][:, 0:tn], sn_t[0][:, 0:tn], ALU.mult, r=[PSK(pr), sn_t[1]], w=[b_t[1]])
                                tt(o_t[0][:, 0:tn], a_t[0][:, 0:tn], b_t[0][:, 0:tn], ALU.add, r=[a_t[1], b_t[1]], w=[o_t[1]])
                            else:
                                cp(o_t[0][:, 0:tn], qn_t[0][:, 0:tn], r=[qn_t[1]], w=[o_t[1]])
                            dma("gpsimd", G[dst][b, h * 128:(h + 1) * 128, t0:t0 + tn], o_t[0][:, 0:tn], r=[o_t[1]], w=[(dst, b)])

        def stage_attn(l):
            A.reset()
            NSB = TOK // 128
            kt, ktk = A.tile([128, TOK], BF16)
            vt, vtk = A.tile([128, NSB, 128], BF16)
            qts = [A.tile([128, TOK], BF16) for _ in range(2)]
            ebs = [A.tile([128, 512], BF16) for _ in range(3)]
            rt, rtk = A.tile([128, 512])
            obs = [A.tile([128, 512], BF16) for _ in range(2)]
            cnt = [0, 0, 0]
            for b in range(NB):
                for hk in range(cfg.AKV):
                    dma("sync", kt, G["KT"][b, hk * 128:(hk + 1) * 128, :], r=[("KT", b)], w=[ktk])
                    dma("sync", vt, G["v_tok"][b, :, hk * 128:(hk + 1) * 128].rearrange("(j p) e -> p j e", p=128), r=[("v_tok", b)], w=[vtk])
                    for g_ in range(4):
                        h = hk * 4 + g_
                        qt, qtk = qts[h % 2]
                        dma("sync", qt, G["QT"][b, h * 128:(h + 1) * 128, :], r=[("QT", b)], w=[qtk])
                        for (t0, tn, isc) in cfg.tblocks(512):
                            sbl = list(range(LAT // 128, NSB)) if isc else list(range(NSB))
                            pN = 3 + cnt[1] % 2
                            pD = 5 + cnt[1] % 2
                            cnt[1] += 1
                            for ix, j in enumerate(sbl):
                                pS = cnt[0] % 3
                                eb, ebk = ebs[cnt[0] % 3]
                                cnt[0] += 1
                                mm(ps[pS][:, 0:tn], kt[:, j * 128:(j + 1) * 128], qt[:, t0:t0 + tn], True, True, r=[ktk, qtk], w=[PSK(pS)])
                                act(eb[:, 0:tn], ps[pS][:, 0:tn], AF.Exp, r=[PSK(pS)], w=[ebk], scale=float(128 ** -0.5))
                                mm(ps[pN][:, 0:tn], vt[:, j, :], eb[:, 0:tn], ix == 0, ix == len(sbl) - 1, r=[vtk, ebk], w=[PSK(pN)])
                                mm(ps[pD][:, 0:tn], onesb, eb[:, 0:tn], ix == 0, ix == len(sbl) - 1, r=["cmatb", ebk], w=[PSK(pD)])
                            recip(rt[:, 0:tn], ps[pD][:, 0:tn], r=[PSK(pD)], w=[rtk])
                            ob, obk = obs[cnt[2] % 2]
                            cnt[2] += 1
                            tt(ob[:, 0:tn], ps[pN][:, 0:tn], rt[:, 0:tn], ALU.mult, r=[PSK(pN), rtk], w=[obk])
                            dma("gpsimd", G["yT"][b, h * 128:(h + 1) * 128, t0:t0 + tn], ob[:, 0:tn], r=[obk], w=[("yT", b)])

        def gla_alloc(EV):
            T = {}
            for n in ("q", "lf", "kk", "pre", "bc", "d1", "tmp"):
                T[n] = A.tile([128, TOK])
            for n in ("Qs", "At", "Ke"):
                T[n] = A.tile([128, TOK], BF16)
            NSB = TOK // 128
            T["Ketok"] = A.tile([128, NSB, 128], BF16)
            T["V"] = A.tile([128, NSB, EV], BF16)
            T["dec"] = A.tile([128, TOK // 32])
            T["S"] = A.tile([128, EV])
            T["Sbf"] = [A.tile([128, EV], BF16) for _ in range(8)]
            T["WT"] = [A.tile([128, 128], BF16) for _ in range(2)]
            T["rm"] = A.tile([128, TOK])
            dma("sync", T["rm"][0], I["rmask"][:, :], w=[T["rm"][1]])
            return T

        def gla_dir(T, EV, dirn, post):
            NSB = TOK // 128
            NCH = TOK // 32
            q, qk_ = T["q"]; lf, lfk = T["lf"]; kk, kkk = T["kk"]; pre, prek = T["pre"]
            bc, bck = T["bc"]; d1, d1k = T["d1"]; tmp, tmpk = T["tmp"]
            Qs, Qsk = T["Qs"]; At, Atk = T["At"]; Ke, Kek = T["Ke"]; Ktok, Ktokk = T["Ketok"]
            V, Vk = T["V"]; dec, deck = T["dec"]; Sf, Sfk = T["S"]; rm, rmk = T["rm"]
            scan(pre, rm, lf, r=[rmk, lfk], w=[prek])
            pre3 = pre.rearrange("p (c s) -> p c s", s=32)
            totb = pre3[:, :, 31:32].to_broadcast([128, NCH, 32])
            v3 = lambda ap_: ap_.rearrange("p (c s) -> p c s", s=32)
            if dirn == 0:
                cp(bc, pre, r=[prek], w=[bck], eng="scalar")
            else:
                tt(tmp, lf, pre, ALU.subtract, r=[lfk, prek], w=[tmpk])
                tt(v3(bc), v3(tmp), totb, ALU.add, r=[tmpk, prek], w=[bck])
            tt(v3(d1), v3(bc), totb, ALU.subtract, r=[bck, prek], w=[d1k])
            act(tmp, bc, AF.Exp, r=[bck], w=[tmpk])
            tt(Qs, tmp, q, ALU.mult, r=[tmpk, qk_], w=[Qsk])
            act(tmp, d1, AF.Exp, r=[d1k], w=[tmpk])
            tt(At, tmp, q, ALU.mult, r=[tmpk, qk_], w=[Atk])
            act(tmp, d1, AF.Exp, r=[d1k], w=[tmpk], scale=-1.0)
            tt(Ke, tmp, kk, ALU.mult, r=[tmpk, kkk], w=[Kek])
            act(dec, pre3[:, :, 31], AF.Exp, r=[prek], w=[deck])
            for j0 in range(0, NSB, 8):
                j1 = min(NSB, j0 + 8)
                for j in range(j0, j1):
                    tr(psb[:, (j - j0) * 128:(j - j0 + 1) * 128], Ke[:, j * 128:(j + 1) * 128], identb, r=[Kek, "cmatb"], w=["psb"])
                cp(Ktok[:, j0:j1, :], psb[:, 0:(j1 - j0) * 128].rearrange("p (j d) -> p j d", d=128), r=["psb"], w=[Ktokk])
            sfx = "_d%d" % dirn
            dbgdump("g_pre" + sfx, pre, [128, TOK], F32, prek)
            dbgdump("g_bc" + sfx, bc, [128, TOK], F32, bck)
            dbgdump("g_d1" + sfx, d1, [128, TOK], F32, d1k)
            dbgdump("g_Qs" + sfx, Qs, [128, TOK], BF16, Qsk)
            dbgdump("g_At" + sfx, At, [128, TOK], BF16, Atk)
            dbgdump("g_Ke" + sfx, Ke, [128, TOK], BF16, Kek)
            dbgdump("g_dec" + sfx, dec, [128, TOK // 32], F32, deck)
            dbgdump("g_Ktok" + sfx, Ktok, [128, NSB, 128], BF16, Ktokk)
            dbgdump("g_lf" + sfx, lf, [128, TOK], F32, lfk)
            dbgdump("g_kk" + sfx, kk, [128, TOK], F32, kkk)
            dbgdump("g_q" + sfx, q, [128, TOK], F32, qk_)
            vmemset(Sf, 0.0, [Sfk])
            lat_b = list(range(0, LAT // 128))
            ctx_b = list(range(LAT // 128, NSB))
            order = (ctx_b + lat_b) if dirn == 0 else (ctx_b[::-1] + lat_b[::-1])
            corder = [0, 1, 2, 3] if dirn == 0 else [3, 2, 1, 0]
            maskb = maskFb if dirn == 0 else maskBb
            NEH = EV // 128
            ring = [0]
            wcnt = [0]
            for blk in order:
                for c in corder:
                    p0, p1 = (64, 128) if c == 3 else (32 * c, 32 * c + 32)
                    mm(ps[c][:, 0:EV], Ktok[p0:p1, blk, :], V[p0:p1, blk, :], True, True, r=[Ktokk, Vk], w=[PSK(c)])
                sb_of = {}
                for c in corder:
                    sb, sbk = T["Sbf"][ring[0] % 8]
                    ring[0] += 1
                    sb_of[c] = (sb, sbk)
                    cp(sb, Sf, r=[Sfk], w=[sbk], eng="scalar")
                    ci = blk * 4 + c
                    stt(Sf, Sf, dec[:, ci:ci + 1], ps[c][:, 0:EV], ALU.mult, ALU.add, r=[Sfk, deck, PSK(c)], w=[Sfk])
                    if c == 3:
                        tt(Sf, Sf, ps[2][:, 0:EV], ALU.subtract, r=[Sfk, PSK(2)], w=[Sfk])
                mm(ps[4][:, 0:128], Ke[:, blk * 128:(blk + 1) * 128], At[:, blk * 128:(blk + 1) * 128], True, True, r=[Kek, Atk], w=[PSK(4)])
                wt_, wtk = T["WT"][wcnt[0] % 2]
                wcnt[0] += 1
                tt(wt_, ps[4][:, 0:128], maskb, ALU.mult, r=[PSK(4), "cmatb"], w=[wtk])
                ehs = [NEH - 1] + list(range(NEH - 1)) if NEH == 3 else list(range(NEH))
                for eh in ehs:
                    pO = 5 + (wcnt[0] + eh) % 2
                    mm(ps[pO][:, 0:128], V[:, blk, eh * 128:(eh + 1) * 128], wt_, True, False, r=[Vk, wtk], w=[PSK(pO)])
                    for ix, c in enumerate(corder):
                        sb, sbk = sb_of[c]
                        mm(ps[pO][:, 32 * c:32 * c + 32], sb[:, eh * 128:(eh + 1) * 128],
                           Qs[:, blk * 128 + 32 * c:blk * 128 + 32 * c + 32], False, ix == 3, r=[sbk, Qsk], w=[PSK(pO)])
                    post(dirn, eh, pO, blk)

        def stage_hgrn(l):
            A.reset()
            T = gla_alloc(128)
            acc, acck = A.tile([128, TOK])
            gs, gsk = A.tile([128, TOK])
            sq, sqk = A.tile([128, TOK])
            yb, ybk = A.tile([128, TOK], BF16)
            r1, r1k = A.tile([128, 512]); rs, rsk = A.tile([128, 512])
            load_col(qn_g[:, 2:3], I["hg_norm"][l], 128, "qn_g2")
            for d_ in range(2):
                if l == 0:
                    vmemset(lbv[:, d_, 0, :], 0.0, ["lbv%d" % d_])
                    vmemset(lbv[:, d_, 1, :], 1.0, ["lbv%d" % d_])
                else:
                    load_vecT(lbv[:, d_, 0, :], I["hg_lb_logits"][d_, 0], cfg.HGH, "lbv%d" % d_)
                    load_vecT(lbv[:, d_, 1, :], I["hg_lb_logits"][d_, 1], cfg.HGH, "lbv%d" % d_)
                    tt(lbv[:, d_, 2, :], lbv[:, d_, 0, :], lbv[:, d_, 1, :], ALU.subtract, r=["lbv%d" % d_], w=["lbv%d" % d_])
                    act(lbv[:, d_, 2, :], lbv[:, d_, 2, :], AF.Exp, r=["lbv%d" % d_], w=["lbv%d" % d_])
                    ts(lbv[:, d_, 2, :], lbv[:, d_, 2, :], 1.0, None, ALU.add, r=["lbv%d" % d_], w=["lbv%d" % d_])
                    recip(lbv[:, d_, 0, :], lbv[:, d_, 2, :], r=["lbv%d" % d_], w=["lbv%d" % d_])
                    ts(lbv[:, d_, 1, :], lbv[:, d_, 0, :], -1.0, 1.0, ALU.mult, ALU.add, r=["lbv%d" % d_], w=["lbv%d" % d_])

            def post(dirn, eh, pO, blk):
                sl = acc[:, blk * 128:(blk + 1) * 128]
                if dirn == 0:
                    cp(sl, ps[pO][:, 0:128], r=[PSK(pO)], w=[acck], eng="scalar")
                else:
                    tt(sl, sl, ps[pO][:, 0:128], ALU.add, r=[PSK(pO), acck], w=[acck])

            for b in range(NB):
                for h in range(cfg.HGH):
                    rows = slice(h * 128, (h + 1) * 128)
                    q, qk_ = T["q"]
                    dma("sync", T["tmp"][0], G["hgq"][b, rows, :], r=[("hgq", b)], w=[T["tmp"][1]])
                    act(q, T["tmp"][0], AF.Silu, r=[T["tmp"][1]], w=[qk_])
                    ts(q, q, float(128 ** -0.5), None, ALU.mult, r=[qk_], w=[qk_])
                    dma("sync", T["V"][0], G["hgi_tok"][b, :, rows].rearrange("(j p) e -> p j e", p=128), r=[("hgi_tok", b)], w=[T["V"][1]])
                    for d_ in range(2):
                        lf, lfk = T["lf"]; kk, kkk = T["kk"]; tmp, tmpk = T["tmp"]
                        dma("sync", tmp, G["hgff" if d_ == 0 else "hgfb"][b, rows, :], r=[("hgff" if d_ == 0 else "hgfb", b)], w=[tmpk])
                        act(lf, tmp, AF.Exp, r=[tmpk], w=[lfk], scale=-1.0)
                        ts(lf, lf, 1.0, None, ALU.add, r=[lfk], w=[lfk])
                        recip(kk, lf, r=[lfk], w=[kkk])
                        ts(kk, kk, lbv[:, d_, 1, h:h + 1], lbv[:, d_, 0, h:h + 1], ALU.mult, ALU.add, r=[kkk, "lbv%d" % d_], w=[kkk])
                        act(lf, kk, AF.Ln, r=[kkk], w=[lfk])
                        ts(kk, kk, -1.0, 1.0, ALU.mult, ALU.add, r=[kkk], w=[kkk])
                        gla_dir(T, 128, d_, post)
                    dma("sync", T["tmp"][0], G["hgg"][b, rows, :], r=[("hgg", b)], w=[T["tmp"][1]])
                    act(gs, T["tmp"][0], AF.Silu, r=[T["tmp"][1]], w=[gsk])
                    act(sq, acc, AF.Square, r=[acck], w=[sqk])
                    for (t0, tn, isc) in cfg.tblocks(512):
                        pi = nextps()
                        mm(ps[pi][:, 0:tn], ones, sq[:, t0:t0 + tn], True, True, r=["cmat", sqk], w=[PSK(pi)])
                        ts(r1[:, 0:tn], ps[pi][:, 0:tn], 1.0 / 128, EPS, ALU.mult, ALU.add, r=[PSK(pi)], w=[r1k])
                        act(rs[:, 0:tn], r1[:, 0:tn], AF.Sqrt, r=[r1k], w=[rsk]); recip(rs[:, 0:tn], rs[:, 0:tn], r=[rsk], w=[rsk])
                        stt(r1[:, 0:tn], acc[:, t0:t0 + tn], qn_g[:, 2:3], rs[:, 0:tn], ALU.mult, ALU.mult, r=[acck, rsk, "qn_g2"], w=[r1k])
                        tt(yb[:, t0:t0 + tn], r1[:, 0:tn], gs[:, t0:t0 + tn], ALU.mult, r=[r1k, gsk], w=[ybk])
                    dma("gpsimd", G["yT"][b, cfg.BW + h * 128:cfg.BW + (h + 1) * 128, :], yb, r=[ybk], w=[("yT", b)])

        def stage_mlstm(l):
            A.reset()
            EV = 384
            T = gla_alloc(EV)
            NG = 4 * cfg.MLH
            accs = [A.tile([128, TOK]) for _ in range(2)]
            sq, sqk = A.tile([128, TOK])
            yb, ybk = A.tile([128, TOK], BF16)
            gI, gIk = A.tile([32, TOK]); gL, gLk = A.tile([32, TOK])
            r1, r1k = A.tile([128, 512]); rs, rsk = A.tile([128, 512])
            rinv, rinvk = A.tile([128, 128]); hn, hnk = A.tile([128, 128])
            dma("sync", mln_g[:, 0:2], I["ml_norm"][l].rearrange("(e p) -> p e", p=128), w=["mln_g"], allow_slow_non_contiguous=True)
            vmemset(mlb[:], 0.0, ["mlb"])
            dma("sync", mlb[0:NG, 0:1], I["ml_gate_bias"][l].rearrange("(p o) -> p o", o=1), r=["mlb"], w=["mlb"])
            ts(mlb[:, 1:2], mlb[:, 0:1], -1.0, None, ALU.mult, r=["mlb"], w=["mlb"])

            def post(dirn, eh, pO, blk):
                if eh == 2:
                    ts(hn, ps[pO][:, 0:128], -1.0, None, ALU.mult, r=[PSK(pO)], w=[hnk])
                    tt(rinv, hn, ps[pO][:, 0:128], ALU.max, r=[PSK(pO), hnk], w=[rinvk])
                    ts(rinv, rinv, 1.0, None, ALU.max, r=[rinvk], w=[rinvk])
                    recip(rinv, rinv, r=[rinvk], w=[rinvk])
                    return
                acc, acck = accs[eh]
                sl = acc[:, blk * 128:(blk + 1) * 128]
                if dirn == 0:
                    tt(sl, ps[pO][:, 0:128], rinv, ALU.mult, r=[PSK(pO), rinvk], w=[acck])
                else:
                    tt(hn, ps[pO][:, 0:128], rinv, ALU.mult, r=[PSK(pO), rinvk], w=[hnk])
                    tt(sl, sl, hn, ALU.add, r=[hnk, acck], w=[acck])

            for b in range(NB):
                vmemset(gI, 0.0, [gIk]); vmemset(gL, 0.0, [gLk])
                dma("sync", gI[0:NG, :], G["mlg"][b, :, :], r=[("mlg", b), gIk], w=[gIk])
                act(gL, gI, AF.Exp, r=[gIk, "mlb"], w=[gLk], bias=mlb[:, 1:2], scale=-1.0)
                ts(gL, gL, 1.0, None, ALU.add, r=[gLk], w=[gLk])
                act(gL, gL, AF.Ln, r=[gLk], w=[gLk])
                ts(gL, gL, -1.0, None, ALU.mult, r=[gLk], w=[gLk])
                act(gI, gI, AF.Exp, r=[gIk, "mlb"], w=[gIk], bias=mlb[:, 0:1], scale=1.0)
                for h in range(cfg.MLH):
                    rows = slice(h * 128, (h + 1) * 128)
                    V, Vk = T["V"]
                    vmemset(V[:, :, 256:384], 1.0, [Vk])
                    dma("sync", V[:, :, 0:256], G["mlv_tok"][b, :, h * 256:(h + 1) * 256].rearrange("(j p) e -> p j e", p=128), r=[("mlv_tok", b), Vk], w=[Vk])
                    dma("sync", T["q"][0], G["mlq"][b, rows, :], r=[("mlq", b)], w=[T["q"][1]])
                    for d_ in range(2):
                        lf, lfk = T["lf"]; kk, kkk = T["kk"]; tmp, tmpk = T["tmp"]
                        dma("sync", tmp, G["mlk"][b, rows, :], r=[("mlk", b)], w=[tmpk])
                        ri = d_ * cfg.MLH + h
                        rf = (2 + d_) * cfg.MLH + h
                        for (t0, tn, isc) in cfg.tblocks(512):
                            pi = nextps()
                            mm(ps[pi][:, 0:tn], sel[:, rf * 128:(rf + 1) * 128], gL[:, t0:t0 + tn], True, True, r=["sel", gLk], w=[PSK(pi)])
                            cp(lf[:, t0:t0 + tn], ps[pi][:, 0:tn], r=[PSK(pi)], w=[lfk], eng="scalar")
                            pj = nextps()
                            mm(ps[pj][:, 0:tn], sel[:, ri * 128:(ri + 1) * 128], gI[:, t0:t0 + tn], True, True, r=["sel", gIk], w=[PSK(pj)])
                            stt(kk[:, t0:t0 + tn], tmp[:, t0:t0 + tn], float(128 ** -0.5), ps[pj][:, 0:tn], ALU.mult, ALU.mult, r=[tmpk, PSK(pj)], w=[kkk])
                        gla_dir(T, EV, d_, post)
                    for eh in range(2):
                        act(sq, accs[eh][0], AF.Square, r=[accs[eh][1]], w=[sqk]) if eh == 0 else None
                    sq2, sq2k = T["pre"]
                    act(sq2, accs[1][0], AF.Square, r=[accs[1][1]], w=[sq2k])
                    for eh in range(2):
                        dma("sync", T["tmp"][0], G["mlo"][b, h * 256 + eh * 128:h * 256 + (eh + 1) * 128, :], r=[("mlo", b)], w=[T["tmp"][1]])
                        act(T["bc"][0], T["tmp"][0], AF.Sigmoid, r=[T["tmp"][1]], w=[T["bc"][1]])
                        for (t0, tn, isc) in cfg.tblocks(512):
                            pi = nextps()
                            mm(ps[pi][:, 0:tn], ones, sq[:, t0:t0 + tn], True, False, r=["cmat", sqk], w=[PSK(pi)])
                            mm(ps[pi][:, 0:tn], ones, sq2[:, t0:t0 + tn], False, True, r=["cmat", sq2k], w=[PSK(pi)])
                            ts(r1[:, 0:tn], ps[pi][:, 0:tn], 1.0 / 256, EPS, ALU.mult, ALU.add, r=[PSK(pi)], w=[r1k])
                            act(rs[:, 0:tn], r1[:, 0:tn], AF.Sqrt, r=[r1k], w=[rsk]); recip(rs[:, 0:tn], rs[:, 0:tn], r=[rsk], w=[rsk])
                            stt(r1[:, 0:tn], accs[eh][0][:, t0:t0 + tn], mln_g[:, eh:eh + 1], rs[:, 0:tn], ALU.mult, ALU.mult, r=[accs[eh][1], rsk, "mln_g"], w=[r1k])
                            tt(yb[:, t0:t0 + tn], r1[:, 0:tn], T["bc"][0][:, t0:t0 + tn], ALU.mult, r=[r1k, T["bc"][1]], w=[ybk])
                        r0 = 2 * cfg.BW + h * 256 + eh * 128
                        dma("gpsimd", G["yT"][b, r0:r0 + 128, :], yb, r=[ybk], w=[("yT", b)])


        def stage_merge(l):
            A.reset()
            gts = [A.tile([128, 512], BF16) for _ in range(3)]
            m0, m0k = A.tile([128, 512]); m1, m1k = A.tile([128, 512])
            ob, obk = A.tile([128, 512], BF16)
            KCb = cfg.BW // 128

            def epi(b, c0, cw, t0, tn, pis):
                for n in range(3):
                    dma("sync", gts[n][0][0:cw, 0:tn], G["gate"][b, n * D + c0:n * D + c0 + cw, t0:t0 + tn], r=[("gate", b)], w=[gts[n][1]])
                tt(m0[0:cw, 0:tn], ps[pis[0]][0:cw, 0:tn], gts[0][0][0:cw, 0:tn], ALU.mult, r=[PSK(pis[0]), gts[0][1]], w=[m0k])
                tt(m1[0:cw, 0:tn], ps[pis[1]][0:cw, 0:tn], gts[1][0][0:cw, 0:tn], ALU.mult, r=[PSK(pis[1]), gts[1][1]], w=[m1k])
                tt(m0[0:cw, 0:tn], m0[0:cw, 0:tn], m1[0:cw, 0:tn], ALU.add, r=[m0k, m1k], w=[m0k])
                tt(m1[0:cw, 0:tn], ps[pis[2]][0:cw, 0:tn], gts[2][0][0:cw, 0:tn], ALU.mult, r=[PSK(pis[2]), gts[2][1]], w=[m1k])
                tt(ob[0:cw, 0:tn], m0[0:cw, 0:tn], m1[0:cw, 0:tn], ALU.add, r=[m0k, m1k], w=[obk])
                dma("sync", G["mergedT"][b, c0:c0 + cw, t0:t0 + tn], ob[0:cw, 0:tn], r=[obk], w=[("mergedT", b)])

            cbs = [(c, 128, "F", epi) for c in range(0, D, 128)]
            wbr = I["w_branch"][l].rearrange("n w d -> (n w) d")
            linear(3 * KCb, lambda b, t0, tn: G["yT"][b, :, t0:t0 + tn].rearrange("(k p) t -> p k t", p=128), ["yT"],
                   lambda k0, k1, c0, c1: wbr[k0:k1, c0:c1], cbs,
                   kgroups=[(0, KCb), (KCb, 2 * KCb), (2 * KCb, 3 * KCb)], wcols=512, nabuf=1)

        def stage_wout(l):
            A.reset()
            epi = make_store_epi(G["y2T"], "y2T", F32, "F", 0)
            cbs = [(c, 128, "F", epi) for c in range(0, D, 128)]
            linear(KC, lambda b, t0, tn: G["mergedT"][b, :, t0:t0 + tn].rearrange("(k p) t -> p k t", p=128), ["mergedT"],
                   lambda k0, k1, c0, c1: I["w_out"][l, k0:k1, c0:c1], cbs)

        def stage_ln(l, which):
            A.reset()
            TB = 256
            gi = 2 if which == 0 else 5
            lg_i, lb_i = (0, 1) if which == 0 else (2, 3)
            srcn = "y2T" if which == 0 else "faccT"
            x_t, xk = A.tile([128, KC, TB]); y_t, yk = A.tile([128, KC, TB])
            sqs = [A.tile([128, TB]) for _ in range(2)]
            mean, mk = A.tile([128, TB]); rstd, rk_ = A.tile([128, TB]); t1s = [A.tile([128, TB]) for _ in range(2)]
            if which == 0:
                h2f, h2fk = A.tile([128, KC, TB]); h2b, h2bk = A.tile([128, KC, TB], BF16)
                lgT, lgTk = A.tile([16, TB]); cT, cTk = A.tile([16, TB])
                lg, lgk = A.tile([128, 16]); ee, eek = A.tile([128, 16]); selm, selmk = A.tile([128, 16])
                mx, mxk = A.tile([128, 4]); psm, psmk = A.tile([128, 4]); gsc, gsck = A.tile([128, 4]); gsel, gselk = A.tile([128, 4])
                cntt, cnttk = A.tile([128, 4]); cmpt, cmptk = A.tile([128, 4]); den, denk = A.tile([128, 4])
                dma("sync", wr[:], I["w_router"].rearrange("(k p) e -> p k e", p=128), w=["wr"])
                dma("sync", brb[:], I["b_router"].partition_broadcast(128), w=["brb"])
            for b in range(NB):
                for (t0, tn, isc) in cfg.tblocks(TB):
                    row = NB if isc else b
                    dma("sync", x_t[:, :, 0:tn], G["xT"][b, :, t0:t0 + tn].rearrange("(k p) t -> p k t", p=128), r=[("xT", b)], w=[xk])
                    dma(STORE_Q, y_t[:, :, 0:tn], G[srcn][b, :, t0:t0 + tn].rearrange("(k p) t -> p k t", p=128), r=[(srcn, b)], w=[yk])
                    for k in range(KC):
                        stt(x_t[:, k, 0:tn], y_t[:, k, 0:tn], modga[:, gi * KC + k, row:row + 1], x_t[:, k, 0:tn], ALU.mult, ALU.add,
                            r=[xk, yk, "modga"], w=[xk])
                    for k in range(KC):
                        mm(ps[0][:, 0:tn], ones, x_t[:, k, 0:tn], k == 0, k == KC - 1, r=["cmat", xk], w=[PSK(0)])
                    for k in range(KC):
                        sq, sqk = sqs[k % 2]
                        act(sq[:, 0:tn], x_t[:, k, 0:tn], AF.Square, r=[xk], w=[sqk])
                        mm(ps[1][:, 0:tn], ones, sq[:, 0:tn], k == 0, k == KC - 1, r=["cmat", sqk], w=[PSK(1)])
                    ts(mean[:, 0:tn], ps[0][:, 0:tn], 1.0 / D, None, ALU.mult, r=[PSK(0)], w=[mk])
                    tt(rstd[:, 0:tn], mean[:, 0:tn], mean[:, 0:tn], ALU.mult, r=[mk], w=[rk_])
                    stt(rstd[:, 0:tn], ps[1][:, 0:tn], 1.0 / D, rstd[:, 0:tn], ALU.mult, ALU.subtract, r=[PSK(1), rk_], w=[rk_])
                    ts(rstd[:, 0:tn], rstd[:, 0:tn], EPS / (cfg.alpha ** 2), None, ALU.add, r=[rk_], w=[rk_])
                    act(rstd[:, 0:tn], rstd[:, 0:tn], AF.Sqrt, r=[rk_], w=[rk_])
                    recip(rstd[:, 0:tn], rstd[:, 0:tn], r=[rk_], w=[rk_])
                    for k in range(KC):
                        t1, t1k = t1s[k % 2]
                        tt(t1[:, 0:tn], x_t[:, k, 0:tn], mean[:, 0:tn], ALU.subtract, r=[xk, mk], w=[t1k])
                        tt(t1[:, 0:tn], t1[:, 0:tn], rstd[:, 0:tn], ALU.mult, r=[t1k, rk_], w=[t1k])
                        ts(x_t[:, k, 0:tn], t1[:, 0:tn], lnv[:, lg_i, k:k + 1], lnv[:, lb_i, k:k + 1], ALU.mult, ALU.add, r=[t1k, "lnv"], w=[xk])
                    dma("sync", G["xT"][b, :, t0:t0 + tn].rearrange("(k p) t -> p k t", p=128), x_t[:, :, 0:tn], r=[xk], w=[("xT", b)])
                    if which != 0:
                        continue
                    for k in range(KC):
                        ts(h2f[:, k, 0:tn], x_t[:, k, 0:tn], mod1p[:, 4 * KC + k, row:row + 1], modT[:, 3 * KC + k, row:row + 1], ALU.mult, ALU.add,
                           r=[xk, "mod1p", "modT"], w=[h2fk])
                    cp(h2b[:, :, 0:tn], h2f[:, :, 0:tn], r=[h2fk], w=[h2bk], eng="scalar")
                    dma(STORE_Q, hT_ap(b, t0, tn), h2b[:, :, 0:tn], r=[h2bk], w=[("hT", b)])
                    for k in range(KC):
                        mm(ps[2][0:16, 0:tn], wr[:, k, :], h2f[:, k, 0:tn], k == 0, k == KC - 1, r=["wr", h2fk], w=[PSK(2)])
                    cp(lgT[:, 0:tn], ps[2][0:16, 0:tn], r=[PSK(2)], w=[lgTk])
                    for s_ in range(tn // 128):
                        tr(ps[3][:, 0:16], lgT[:, s_ * 128:(s_ + 1) * 128], ident[0:16, 0:16], r=[lgTk, "cmat"], w=[PSK(3)])
                        tt(lg, ps[3][:, 0:16], brb[:], ALU.add, r=[PSK(3), "brb"], w=[lgk])
                        S.op("vector", lambda e: e.reduce_max(out=mx[:, 0:1], in_=lg, axis=AX.X), reads=[lgk], writes=[mxk])
                        ts(mx[:, 0:1], mx[:, 0:1], -1.0, None, ALU.mult, r=[mxk], w=[mxk])
                        act(ee, lg, AF.Exp, r=[lgk, mxk], w=[eek], bias=mx[:, 0:1], scale=1.0)
                        e3 = ee.rearrange("p (g i) -> p g i", i=4)
                        first = True
                        for i_ in range(4):
                            for j_ in range(i_ + 1, 4):
                                tt(psm, e3[:, :, i_], e3[:, :, j_], ALU.add, r=[eek], w=[psmk])
                                if first:
                                    cp(gsc, psm, r=[psmk], w=[gsck]); first = False
                                else:
                                    tt(gsc, gsc, psm, ALU.max, r=[gsck, psmk], w=[gsck])
                        S.op("vector", lambda e: e.reduce_max(out=mx[:, 1:2], in_=gsc, axis=AX.X), reads=[gsck], writes=[mxk])
                        ts(gsel, gsc, mx[:, 1:2], None, ALU.is_ge, r=[gsck, mxk], w=[gselk])
                        s3 = selm.rearrange("p (g i) -> p g i", i=4)
                        for i_ in range(4):
                            vmemset(cntt, 0.0, [cnttk])
                            for j_ in range(4):
                                if j_ == i_:
                                    continue
                                tt(cmpt, e3[:, :, j_], e3[:, :, i_], ALU.is_gt, r=[eek], w=[cmptk])
                                tt(cntt, cntt, cmpt, ALU.add, r=[cnttk, cmptk], w=[cnttk])
                            ts(cntt, cntt, 1.5, None, ALU.is_lt, r=[cnttk], w=[cnttk])
                            tt(s3[:, :, i_], cntt, gsel, ALU.mult, r=[cnttk, gselk], w=[selmk])
                        tt(selm, selm, ee, ALU.mult, r=[selmk, eek], w=[selmk])
                        S.op("vector", lambda e: e.reduce_sum(out=den[:, 0:1], in_=selm, axis=AX.X), reads=[selmk], writes=[denk])
                        recip(den[:, 0:1], den[:, 0:1], r=[denk], w=[denk])
                        ts(selm, selm, den[:, 0:1], None, ALU.mult, r=[selmk, denk], w=[selmk])
                        tr(ps[4][0:16, 0:128], selm, ident, r=[selmk, "cmat"], w=[PSK(4)])
                        cp(cT[:, s_ * 128:(s_ + 1) * 128], ps[4][0:16, 0:128], r=[PSK(4)], w=[cTk])
                    dma("sync", G["combT"][b, :, t0:t0 + tn], cT[:, 0:tn], r=[cTk], w=[("combT", b)])

        def stage_moe(l):
            DEc = cfg.DE // 128
            for e_ in range(16):
                A.reset()
                cbt, cbk = A.tile([32, 512]); cbb, cbbk = A.tile([128, 512])
                sg, sgk = A.tile([128, 512]); hb, hbk = A.tile([128, 512], BF16)
                vmemset(cbt, 0.0, [cbk])
                act(sg[:, 0:128], ones, AF.Silu, r=["cmat"], w=[sgk])
                state = {"key": None}

                def epi_gu(b, c0, cw, t0, tn, pis, e_=e_):
                    if state["key"] != (b, t0):
                        state["key"] = (b, t0)
                        dma("sync", cbt[0:16, 0:tn], G["combT"][b, :, t0:t0 + tn], r=[("combT", b), cbk], w=[cbk])
                        mm(ps[6][:, 0:tn], sel[:, e_ * 128:(e_ + 1) * 128], cbt[:, 0:tn], True, True, r=["sel", cbk], w=[PSK(6)])
                        cp(cbb[:, 0:tn], ps[6][:, 0:tn], r=[PSK(6)], w=[cbbk])
                    act(sg[0:cw, 0:tn], ps[pis[0]][0:cw, 0:tn], AF.Silu, r=[PSK(pis[0])], w=[sgk])
                    tt(sg[0:cw, 0:tn], sg[0:cw, 0:tn], ps[pis[1]][0:cw, 0:tn], ALU.mult, r=[sgk, PSK(pis[1])], w=[sgk])
                    tt(hb[0:cw, 0:tn], sg[0:cw, 0:tn], cbb[0:cw, 0:tn], ALU.mult, r=[sgk, cbbk], w=[hbk])
                    dma("sync", G["hid"][b, c0:c0 + cw, t0:t0 + tn], hb[0:cw, 0:tn], r=[hbk], w=[("hid", b)])

                gu_linear(l, e_, epi_gu)
                if "stop_e0" in dbg and l == 1:
                    return
                A.reset()
                ac, ack = A.tile([128, 512]); o2, o2k = A.tile([128, 512])

                def epi_d(b, c0, cw, t0, tn, pis, e_=e_):
                    if e_ == 0:
                        cp(o2[0:cw, 0:tn], ps[pis[0]][0:cw, 0:tn], r=[PSK(pis[0])], w=[o2k])
                    else:
                        dma("sync", ac[0:cw, 0:tn], G["faccT"][b, c0:c0 + cw, t0:t0 + tn], r=[("faccT", b)], w=[ack])
                        tt(o2[0:cw, 0:tn], ac[0:cw, 0:tn], ps[pis[0]][0:cw, 0:tn], ALU.add, r=[ack, PSK(pis[0])], w=[o2k])
                    dma("sync", G["faccT"][b, c0:c0 + cw, t0:t0 + tn], o2[0:cw, 0:tn], r=[o2k], w=[("faccT", b)])

                cbs = [(c, 128, "F", epi_d) for c in range(0, D, 128)]
                linear(DEc, lambda b, t0, tn: G["hid"][b, :, t0:t0 + tn].rearrange("(k p) t -> p k t", p=128), ["hid"],
                       lambda k0, k1, c0, c1, e_=e_: I["w_exp_down"][l, e_, k0:k1, c0:c1], cbs, wcols=1024)
                if "snap" in dbg and l == 1:
                    if "snap" not in dbg_out:
                        dbg_out["snap"] = nc.dram_tensor("snap", [16, 128, 8], F32, kind="ExternalOutput").ap()
                        dbg_out["snaph"] = nc.dram_tensor("snaph", [16, 128, 8], BF16, kind="ExternalOutput").ap()
                    S.barrier()
                    dma("sync", dbg_out["snap"][e_], G["faccT"][0, 0:128, 0:8], w=[("snap", e_)])
                    dma("sync", dbg_out["snaph"][e_], G["hid"][0, 0:128, 0:8], w=[("snaph", e_)])

        def gu_linear(l, e_, epi):
            HW = min(256, cfg.DE)
            wg = [A.tile([128, KC, 2 * HW], BF16) for _ in range(2)]
            ab = [A.tile([128, KC, 512], BF16) for _ in range(2)]
            wst = [A.tile([128, 4, HW], F32) for _ in range(4)]
            wsc = [0]
            ai = 0
            pc = 0
            for hi, h0 in enumerate(range(0, cfg.DE, HW)):
                w_t, wk = wg[hi % 2]
                for kk in range(0, KC, 4):
                    ke = min(KC, kk + 4)
                    for (wsrc, tag, c0_) in ((I["w_exp_gate"], "g", 0), (I["w_exp_up"], "u", HW)):
                        st_, stk_ = wst[wsc[0] % 4]
                        wsc[0] += 1
                        dma("sync", st_[:, 0:ke - kk, :], wsrc[l, e_, kk * 128:ke * 128, h0:h0 + HW].rearrange("(k p) c -> p k c", p=128), w=[stk_])
                        cp(w_t[:, kk:ke, c0_:c0_ + HW], st_[:, 0:ke - kk, :], r=[stk_], w=[(wk, tag, kk // 4)], eng="scalar" if wsc[0] % 2 else "vector")
                for b in range(NB):
                    for (t0, tn, isc) in cfg.tblocks(512):
                        a_t, ak = ab[ai % 2]
                        ai += 1
                        dma("sync", a_t[:, :, 0:tn], hT_ap(b, t0, tn), r=[("hT", b)], w=[ak])
                        for j in range(HW // 128):
                            pg, pu = (pc % 2) * 2, (pc % 2) * 2 + 1
                            pc += 1
                            for k in range(KC):
                                mm(ps[pg][:, 0:tn], w_t[:, k, j * 128:(j + 1) * 128], a_t[:, k, 0:tn], k == 0, k == KC - 1, r=[(wk, "g", k // 4), ak], w=[PSK(pg)])
                            for k in range(KC):
                                mm(ps[pu][:, 0:tn], w_t[:, k, HW + j * 128:HW + (j + 1) * 128], a_t[:, k, 0:tn], k == 0, k == KC - 1, r=[(wk, "u", k // 4), ak], w=[PSK(pu)])
                            epi(b, h0 + j * 128, 128, t0, tn, [pg, pu])

        def stage_final():
            A.reset()
            xt, xtk = A.tile([128, KC, 512]); ot, otk = A.tile([128, D])
            outs = []
            for b in range(NB):
                for (t0, tn, isc) in cfg.tblocks(512):
                    if isc:
                        continue
                    dma("sync", xt[:, :, 0:tn], G["xT"][b, :, t0:t0 + tn].rearrange("(k p) t -> p k t", p=128), r=[("xT", b)], w=[xtk])
                    for s_ in range(tn // 128):
                        for k4 in range(0, KC, 4):
                            pi = nextps()
                            for k in range(k4, min(KC, k4 + 4)):
                                tr(ps[pi][:, (k - k4) * 128:(k - k4 + 1) * 128], xt[:, k, s_ * 128:(s_ + 1) * 128], ident, r=[xtk, "cmat"], w=[PSK(pi)])
                            nk = min(KC, k4 + 4) - k4
                            cp(ot[:, k4 * 128:(k4 + nk) * 128], ps[pi][:, 0:nk * 128], r=[PSK(pi)], w=[otk], eng="scalar" if (k4 // 4) % 2 else "vector")
                        outs.append(dma("sync", out[b, t0 + s_ * 128:t0 + (s_ + 1) * 128, :], ot[:, :], r=[otk], w=[("out", b)]))
            S.finish(outs)

        stages = []
        stages.append(("s", stage_s))
        stages.append(("init", stage_init))
        for l in range(L):
            stages.append(("mod%d" % l, lambda l=l: stage_mod(l)))
            stages.append(("modulate%d" % l, lambda: stage_modulate(0, 1)))
            stages.append(("inproj%d" % l, lambda l=l: stage_inproj(l)))
            stages.append(("qk%d" % l, lambda l=l: stage_qk(l)))
            stages.append(("attn%d" % l, lambda l=l: stage_attn(l)))
            stages.append(("hgrn%d" % l, lambda l=l: stage_hgrn(l)))
            stages.append(("mlstm%d" % l, lambda l=l: stage_mlstm(l)))
            stages.append(("merge%d" % l, lambda l=l: stage_merge(l)))
            stages.append(("wout%d" % l, lambda l=l: stage_wout(l)))
            stages.append(("ln1_%d" % l, lambda l=l: stage_ln(l, 0)))
            stages.append(("moe%d" % l, lambda l=l: stage_moe(l)))
            stages.append(("ln2_%d" % l, lambda l=l: stage_ln(l, 1)))
        stages.append(("final", stage_final))
        for name, fn in stages:
            fn()
            if dbg and 'verbose' in dbg:
                print(name, {e: (len(v), sum(1 for o in v if o.signals)) for e, v in S.ops.items()}, flush=True)
            if stop_after == name:
                break

        S.barrier()
        if stop_after not in (None, "final"):
            fin = S.op("sync", lambda e: e.dma_start(out=out[0, 0:1, 0:16], in_=I["x"][0, 0:1, 0:16]), dma=True)
            S.finish([fin])
        S.emit()
        if dbg and 'verbose' in dbg:
            print('odd DMAs', len(odd_log), sorted(set(odd_log))[:20])
    return nc, dbg_out


N_CORES_USED = 4


def kernel(**inputs):
    cfg = Cfg(D=4096, LAT=2048, CTX=256, NB=1, DEPTH=2)
    nc, _ = build(cfg)
    consts = host_consts(cfg)
    in_maps = []
    for b in range(N_CORES_USED):
        m = {}
        for k, v in inputs.items():
            v = np.asarray(v)
            if k in ("x", "c", "ctx"):
                m[k] = np.ascontiguousarray(v[b:b + 1])
            else:
                m[k] = v
        m.update(consts)
        in_maps.append(m)
    res = run_bass_kernel_spmd(nc, in_maps, core_ids=list(range(N_CORES_USED)))
    return np.concatenate([np.asarray(r["out"]) for r in res.results], axis=0).astype(np.float32)
```

```python
import numpy as np
import concourse.bass as bass
import concourse.mybir as mybir
from concourse.bass_utils import run_bass_kernel_spmd
from contextlib import ExitStack

F32 = mybir.dt.float32
BF16 = mybir.dt.bfloat16
AF = mybir.ActivationFunctionType
ALU = mybir.AluOpType
AX = mybir.AxisListType

ENGS = ("tensor", "vector", "scalar", "gpsimd", "sync")
MAXV = 8000
NDMASEM = {"sync": 56, "gpsimd": 16, "tensor": 4, "vector": 4, "scalar": 4}
EPS = 1e-6
PRUNE_WAR = True
DMA_WINDOW = 24
SETTLE_N = 16
SETTLE_ROWS = 16
STORE_Q = "sync"
ODD_POOL = True


class Op:
    __slots__ = ("eng", "fn", "deps", "signals", "sem", "val", "is_dma", "prev_same_sem", "throttle")

    def __init__(self, eng, fn, is_dma):
        self.eng = eng
        self.fn = fn
        self.deps = []
        self.signals = False
        self.sem = None
        self.val = 0
        self.is_dma = is_dma
        self.prev_same_sem = None
        self.throttle = None


class Sched:
    def __init__(self, nc, es):
        self.nc = nc
        self.es = es
        self.ops = {e: [] for e in ENGS}
        self.last_w = {}
        self.readers = {}
        self.sems = {}
        self.dma_sems = {}
        self.dma_cnt = {e: 0 for e in ENGS}
        self.dma_last = {}
        self.dma_hist = {}
        self.final_waits = []
        self.bar = []
        self.since_bar = []

    def _newsem(self, name):
        return self.es.enter_context(self.nc.semaphore(name))

    def op(self, eng, fn, reads=(), writes=(), dma=False, odd=False):
        o = Op(eng, fn, dma)
        deps = list(self.bar)
        for r in reads:
            w = self.last_w.get(r)
            if w is not None:
                deps.append(w)
        for r in writes:
            w = self.last_w.get(r)
            if w is not None:
                deps.append(w)
            lastrd = {}
            for rd in self.readers.get(r, ()):
                if rd.is_dma or not PRUNE_WAR:
                    deps.append(rd)
                else:
                    lastrd[rd.eng] = rd
            deps.extend(lastrd.values())
        seen = set()
        for d in deps:
            if id(d) in seen:
                continue
            seen.add(id(d))
            if d.eng == "tensor" and eng == "tensor" and not d.is_dma and not dma:
                continue
            o.deps.append(d)
            d.signals = True
        for r in writes:
            self.last_w[r] = o
            self.readers[r] = []
        for r in reads:
            self.readers.setdefault(r, []).append(o)
        if dma:
            pool = eng + ("_odd" if odd else "")
            self.dma_cnt.setdefault(pool, 0)
            k = self.dma_cnt[pool] % (4 if odd else NDMASEM[eng])
            self.dma_cnt[pool] += 1
            key = (pool, k)
            hist = self.dma_hist.setdefault(eng, [])
            o.throttle = hist[-DMA_WINDOW] if len(hist) >= DMA_WINDOW else None
            hist.append(o)
            o.prev_same_sem = self.dma_last.get(key)
            self.dma_last[key] = o
            o.sem = key
            o.signals = True
        self.ops[eng].append(o)
        self.since_bar.append(o)
        return o

    def barrier(self):
        last = {}
        dmas = []
        for o in self.since_bar:
            if o.is_dma:
                dmas.append(o)
            else:
                last[o.eng] = o
        self.bar = list(last.values()) + dmas
        for o in self.bar:
            o.signals = True
        self.since_bar = []
        self.last_w = {}
        self.readers = {}

    def finish(self, ops):
        self.final_waits.extend(ops)
        for o in ops:
            o.signals = True

    def emit(self):
        nc = self.nc
        cnt = {e: 0 for e in ENGS}
        dcnt = {}
        for e in ENGS:
            for o in self.ops[e]:
                if o.is_dma:
                    dcnt[o.sem] = dcnt.get(o.sem, 0) + 1
                    o.val = 16 * dcnt[o.sem]
                    if o.sem not in self.dma_sems:
                        self.dma_sems[o.sem] = self._newsem("d%s%d" % (o.sem[0], o.sem[1]))
                elif o.signals:
                    n = cnt[e]
                    cnt[e] += 1
                    key = (e, n // MAXV)
                    if key not in self.sems:
                        self.sems[key] = self._newsem("c%s%d" % (e[:2], key[1]))
                    o.sem = key
                    o.val = n % MAXV + 1
        allsem = dict(self.sems)
        allsem.update(self.dma_sems)
        self.stats = {"compute_sems": len(self.sems), "dma_sems": len(self.dma_sems), "signals": dict(cnt),
                      "max_dma_val": max([16 * v for v in dcnt.values()] + [0])}

        def run(ename):
            def body(eng):
                seen = {}

                def wait(d):
                    if seen.get(d.sem, 0) >= d.val:
                        return
                    seen[d.sem] = d.val
                    eng.wait_ge(allsem[d.sem], d.val)

                for o in self.ops[ename]:
                    for d in o.deps:
                        wait(d)
                    if o.is_dma and o.prev_same_sem is not None:
                        wait(o.prev_same_sem)
                    if o.is_dma and o.throttle is not None:
                        wait(o.throttle)
                    ins = o.fn(eng)
                    if o.signals:
                        ins.then_inc(allsem[o.sem], 16 if o.is_dma else 1)
                if ename == "sync":
                    for d in self.final_waits:
                        wait(d)
            return body

        with nc.Block() as block:
            for e in ENGS:
                if self.ops[e] or (e == "sync" and self.final_waits):
                    getattr(block, e)(run(e))


class Cfg:
    def __init__(self, D=4096, LAT=2048, CTX=256, NB=1, DEPTH=2):
        self.D, self.LAT, self.CTX, self.NB, self.DEPTH = D, LAT, CTX, NB, DEPTH
        self.TOK = LAT + CTX
        self.KC = D // 128
        self.AH = D // 256
        self.AKV = self.AH // 4
        self.HGH = D // 256
        self.MLH = D // 512
        self.BW = self.AH * 128
        self.NE = 16
        self.DE = D // 4
        segs = [("attn_q", self.AH * 128), ("attn_k", self.AKV * 128), ("attn_v", self.AKV * 128),
                ("hg_q", self.HGH * 128), ("hg_f_fwd", self.HGH * 128), ("hg_f_bwd", self.HGH * 128),
                ("hg_i", self.HGH * 128), ("hg_g", self.HGH * 128), ("ml_q", self.MLH * 128),
                ("ml_k", self.MLH * 128), ("ml_v", self.MLH * 256), ("ml_gates", 4 * self.MLH),
                ("ml_o", self.MLH * 256), ("merge", 3 * D)]
        self.segs = segs
        self.off = {}
        s = 0
        for n, w in segs:
            self.off[n] = (s, w)
            s += w
        self.IN_COLS = s
        self.alpha = float((2 * DEPTH) ** 0.25)

    def tblocks(self, width=512):
        out = []
        t = 0
        while t < self.LAT:
            w = min(width, self.LAT - t)
            out.append((t, w, False))
            t += w
        t = self.LAT
        while t < self.TOK:
            w = min(width, self.TOK - t)
            out.append((t, w, True))
            t += w
        return out


def host_consts(cfg):
    c = {}
    ident = np.eye(128, dtype=np.float32)
    ones = np.ones((128, 128), np.float32)
    Rm = np.zeros((128, 128), np.float32)
    for a in range(2):
        for p in range(32):
            m0 = a * 64 + p
            m1 = a * 64 + 32 + p
            Rm[m1, m0] = -1.0
            Rm[m0, m1] = 1.0
    s = np.arange(128)[:, None]
    t = np.arange(128)[None, :]
    same = (s // 32) == (t // 32)
    maskF = (same & (s <= t)).astype(np.float32)
    maskB = (same & (s >= t)).astype(np.float32)
    c["cmat"] = np.concatenate([ident, ones, Rm, maskF, maskB], axis=1)
    sel = np.zeros((32, 32 * 128), np.float32)
    for k in range(32):
        sel[k, k * 128:(k + 1) * 128] = 1.0
    c["sel"] = sel
    rm = np.ones((128, cfg.TOK), np.float32)
    rm[:, ::32] = 0.0
    c["rmask"] = rm
    tt = np.arange(cfg.LAT)
    row = (tt // 64).astype(np.float32)
    col = (tt % 64).astype(np.float32)
    inv = (10000.0 ** (-np.arange(32, dtype=np.float32) / 32)).astype(np.float32)
    ang = np.stack([row[:, None] * inv, col[:, None] * inv], axis=1).astype(np.float32)
    cosT = np.zeros((128, cfg.LAT), np.float32)
    sinT = np.zeros((128, cfg.LAT), np.float32)
    for a in range(2):
        for h in range(2):
            cosT[a * 64 + h * 32:a * 64 + h * 32 + 32, :] = np.cos(ang[:, a, :]).T
            sinT[a * 64 + h * 32:a * 64 + h * 32 + 32, :] = np.sin(ang[:, a, :]).T
    c["rope"] = np.stack([cosT, sinT], 0).astype(np.float32)
    return c


class B:
    pass


def build(cfg, dbg=(), stop_after=None):
    nc = bass.Bass("TRN2", target_bir_lowering=False)
    D, KC, TOK, LAT, CTX, NB = cfg.D, cfg.KC, cfg.TOK, cfg.LAT, cfg.CTX, cfg.NB
    NR = NB + 1
    L = cfg.DEPTH

    def din(name, shape, dt=F32):
        return nc.dram_tensor(name, list(shape), dt, kind="ExternalInput").ap()

    I = {}
    I["x"] = din("x", [NB, LAT, D])
    I["c"] = din("c", [NB, D])
    I["ctx"] = din("ctx", [NB, CTX, D])
    I["c_ctx"] = din("c_ctx", [D])
    I["w_mod"] = din("w_mod", [L, D, 6 * D])
    I["b_mod"] = din("b_mod", [L, 6 * D])
    I["w_in"] = din("w_in", [L, D, cfg.IN_COLS])
    I["ml_gate_bias"] = din("ml_gate_bias", [L, 4 * cfg.MLH])
    I["attn_q_norm"] = din("attn_q_norm", [L, 128])
    I["attn_k_norm"] = din("attn_k_norm", [L, 128])
    I["hg_lb_logits"] = din("hg_lb_logits", [2, L, cfg.HGH * 128])
    I["hg_norm"] = din("hg_norm", [L, 128])
    I["ml_norm"] = din("ml_norm", [L, 256])
    I["w_branch"] = din("w_branch", [L, 3, cfg.BW, D])
    I["w_out"] = din("w_out", [L, D, D])
    for n in ("ln1_g", "ln1_b", "ln2_g", "ln2_b"):
        I[n] = din(n, [L, D])
    I["w_router"] = din("w_router", [D, 16])
    I["b_router"] = din("b_router", [16])
    I["w_exp_gate"] = din("w_exp_gate", [L, 16, D, cfg.DE])
    I["w_exp_up"] = din("w_exp_up", [L, 16, D, cfg.DE])
    I["w_exp_down"] = din("w_exp_down", [L, 16, cfg.DE, D])
    I["cmat"] = din("cmat", [128, 640])
    I["sel"] = din("sel", [32, 32 * 128])
    I["rmask"] = din("rmask", [128, TOK])
    I["rope"] = din("rope", [2, 128, LAT])
    out = nc.dram_tensor("out", [NB, LAT, D], F32, kind="ExternalOutput").ap()

    dbg_out = {}

    def dscr(name, shape, dt):
        if name in dbg:
            ap = nc.dram_tensor(name, list(shape), dt, kind="ExternalOutput").ap()
            dbg_out[name] = ap
            return ap
        return nc.dram_tensor(name, list(shape), dt, kind="Internal").ap()

    G = {}
    G["xT"] = dscr("xT", [NB, D, TOK], F32)
    NBLK = (LAT + 511) // 512 + (CTX + 511) // 512
    G["hT"] = dscr("hT", [NB, NBLK, 128, KC, 512], BF16)

    def hT_ap(b, t0, tn):
        if t0 >= LAT:
            blk, off = (LAT + 511) // 512 + (t0 - LAT) // 512, (t0 - LAT) % 512
        else:
            blk, off = t0 // 512, t0 % 512
        assert off + tn <= 512
        return G["hT"][b, blk, :, :, off:off + tn]
    G["qraw"] = dscr("qraw", [NB, cfg.AH * 128, TOK], F32)
    G["kraw"] = dscr("kraw", [NB, cfg.AKV * 128, TOK], F32)
    G["v_tok"] = dscr("v_tok", [NB, TOK, cfg.AKV * 128], BF16)
    G["QT"] = dscr("QT", [NB, cfg.AH * 128, TOK], BF16)
    G["KT"] = dscr("KT", [NB, cfg.AKV * 128, TOK], BF16)
    G["hgq"] = dscr("hgq", [NB, cfg.HGH * 128, TOK], F32)
    G["hgff"] = dscr("hgff", [NB, cfg.HGH * 128, TOK], F32)
    G["hgfb"] = dscr("hgfb", [NB, cfg.HGH * 128, TOK], F32)
    G["hgi_tok"] = dscr("hgi_tok", [NB, TOK, cfg.HGH * 128], BF16)
    G["hgg"] = dscr("hgg", [NB, cfg.HGH * 128, TOK], F32)
    G["mlq"] = dscr("mlq", [NB, cfg.MLH * 128, TOK], F32)
    G["mlk"] = dscr("mlk", [NB, cfg.MLH * 128, TOK], F32)
    G["mlv_tok"] = dscr("mlv_tok", [NB, TOK, cfg.MLH * 256], BF16)
    G["mlg"] = dscr("mlg", [NB, 4 * cfg.MLH, TOK], F32)
    G["mlo"] = dscr("mlo", [NB, cfg.MLH * 256, TOK], F32)
    G["gate"] = dscr("gate", [NB, 3 * D, TOK], BF16)
    G["yT"] = dscr("yT", [NB, 3 * cfg.BW, TOK], BF16)
    G["mergedT"] = dscr("mergedT", [NB, D, TOK], BF16)
    G["y2T"] = dscr("y2T", [NB, D, TOK], F32)
    G["combT"] = dscr("combT", [NB, 16, TOK], F32)
    G["faccT"] = dscr("faccT", [NB, D, TOK], F32)
    EG = 4
    G["hid"] = dscr("hid", [NB, EG * cfg.DE, TOK], BF16)

    with ExitStack() as es:
        S = Sched(nc, es)
        def P(name, shape, dt=F32):
            return es.enter_context(nc.sbuf_tensor("sb_" + name, list(shape), dt))

        cmat = P("cmat", [128, 640])
        cmatb = P("cmatb", [128, 640], BF16)
        sel = P("sel", [32, 32 * 128])
        ident = cmat[:, 0:128]
        ones = cmat[:, 128:256]
        Rm = cmat[:, 256:384]
        identb = cmatb[:, 0:128]
        onesb = cmatb[:, 128:256]
        maskFb = cmatb[:, 384:512]
        maskBb = cmatb[:, 512:640]
        sT = P("sT", [128, KC, NR])
        modT = P("modT", [128, 6 * KC, NR])
        mod1p = P("mod1p", [128, 6 * KC, NR])
        modga = P("modga", [128, 6 * KC, NR])
        bmT = P("bmT", [128, 6 * KC])
        lnv = P("lnv", [128, 4, KC])
        qn_g = P("qn_g", [128, 4])
        mln_g = P("mln_g", [128, 2])
        lbv = P("lbv", [128, 2, 3, cfg.HGH])
        mlb = P("mlb", [32, 2])
        wr = P("wr", [128, KC, 16])
        brb = P("brb", [128, 16])
        ARENA_BYTES = 176 * 1024
        SETTLE = SETTLE_N
        settle_t = P("settle", [SETTLE_ROWS, 16])
        arena = es.enter_context(nc.sbuf_tensor("arena", [128, ARENA_BYTES // 4], F32))
        arena_off = [0]
        arena_cnt = [0]

        a_base = nc.sbuf_base - ARENA_BYTES if False else None

        ps = [es.enter_context(nc.psum_tensor("ps%d" % i, [128, 512], F32)) for i in range(7)]
        psb = es.enter_context(nc.psum_tensor("psb", [128, 1024], BF16))
        ps_rr = [0]

        def nextps(lo=0, hi=7):
            i = lo + ps_rr[0] % (hi - lo)
            ps_rr[0] += 1
            return i

        class Arena:
            def __init__(self):
                self.off = 0

            def reset(self):
                S.barrier()
                self.off = 0
                for i in range(SETTLE):
                    dma("sync", settle_t[:, :], I["cmat"][0:SETTLE_ROWS, 0:16], r=["settle"], w=["settle"])
                if SETTLE:
                    S.barrier()

            def tile(self, shape, dt=F32):
                n = int(np.prod(shape[1:]))
                esz = 4 if dt == F32 else 2
                nbytes = (n * esz + 31) // 32 * 32
                assert self.off + nbytes <= ARENA_BYTES, ("arena overflow", self.off, nbytes)
                w0 = self.off // 4
                flat = arena[:, w0:w0 + nbytes // 4]
                self.off += nbytes
                if dt != F32:
                    flat = flat.bitcast(dt)
                flat = flat[:, 0:n]
                if len(shape) == 3:
                    flat = flat.rearrange("p (a b) -> p a b", a=shape[1])
                elif len(shape) == 4:
                    flat = flat.rearrange("p (a b c) -> p a b c", a=shape[1], b=shape[2])
                if shape[0] < 128:
                    flat = flat[0:shape[0]]
                arena_cnt[0] += 1
                return flat, ("ar", arena_cnt[0])

        A = Arena()

        def dma(q, out_, in_, r=(), w=(), **kw):
            r = [k for k in r if not (isinstance(k, tuple) and k[0] in G)]
            w = [k for k in w if not (isinstance(k, tuple) and k[0] in G)]

            def outer(ap_):
                for d_ in ap_.shape:
                    if d_ > 1:
                        return d_
                return 1
            odd = ODD_POOL and ((outer(out_) % 16 != 0) or (outer(in_) % 16 != 0))
            if odd:
                odd_log.append((tuple(out_.shape), tuple(in_.shape)))
            return S.op(q, lambda e: e.dma_start(out=out_, in_=in_, **kw), reads=r, writes=w, dma=True, odd=odd)

        def mm(out_, lhsT, rhs, start, stop, r=(), w=()):
            return S.op("tensor", lambda e: e.matmul(out_, lhsT=lhsT, rhs=rhs, start=start, stop=stop), reads=r, writes=w)

        def tr(out_, in_, idn, r=(), w=()):
            return S.op("tensor", lambda e: e.transpose(out_, in_, idn), reads=r, writes=w)

        def act(out_, in_, func, r=(), w=(), eng="scalar", **kw):
            return S.op("scalar", lambda e: e.activation(out=out_, in_=in_, func=func, **kw), reads=r, writes=w)

        def tt(out_, in0, in1, op, r=(), w=(), eng="vector"):
            return S.op(eng, lambda e: e.tensor_tensor(out=out_, in0=in0, in1=in1, op=op), reads=r, writes=w)

        def ts(out_, in0, s1, s2, op0, op1=None, r=(), w=(), eng="vector"):
            if op1 is None:
                return S.op(eng, lambda e: e.tensor_scalar(out=out_, in0=in0, scalar1=s1, scalar2=None, op0=op0), reads=r, writes=w)
            return S.op(eng, lambda e: e.tensor_scalar(out=out_, in0=in0, scalar1=s1, scalar2=s2, op0=op0, op1=op1), reads=r, writes=w)

        def stt(out_, in0, sc, in1, op0, op1, r=(), w=()):
            return S.op("vector", lambda e: e.scalar_tensor_tensor(out=out_, in0=in0, scalar=sc, in1=in1, op0=op0, op1=op1), reads=r, writes=w)

        def cp(out_, in_, r=(), w=(), eng="vector"):
            if eng == "scalar":
                return S.op("scalar", lambda e: e.copy(out=out_, in_=in_), reads=r, writes=w)
            return S.op(eng, lambda e: e.tensor_copy(out=out_, in_=in_), reads=r, writes=w)

        def recip(out_, in_, r=(), w=()):
            return S.op("vector", lambda e: e.reciprocal(out=out_, in_=in_), reads=r, writes=w)

        odd_log = []

        def PSK(i):
            return ("ps", i)

        dumped = set()

        def dbgdump(name, ap_, shape, dt, key):
            if name not in dbg or name in dumped:
                return
            dumped.add(name)
            o_ = nc.dram_tensor(name, list(shape), dt, kind="ExternalOutput").ap()
            dbg_out[name] = o_
            dma("sync", o_, ap_, r=[key], w=[("dbg", name)])

        dma("sync", cmat[:], I["cmat"][:, :], w=["cmat"])
        dma("sync", sel[:], I["sel"][:, :], w=["sel"])
        cp(cmatb[:], cmat[:], r=["cmat"], w=["cmatb"])

        def load_vecT(dst, vec_ap, n, key):
            t, tk = A.tile([n, 128])
            dma("sync", t, vec_ap.rearrange("(k p) -> k p", p=128), w=[tk])
            pi = nextps()
            tr(ps[pi][:, 0:n], t, ident[0:n, 0:n], r=[tk, "cmat"], w=[PSK(pi)])
            cp(dst, ps[pi][:, 0:n], r=[PSK(pi)], w=[key])

        def stage_s():
            A.reset()
            for rr in range(NR):
                src = I["c"][rr] if rr < NB else I["c_ctx"]
                t, tk = A.tile([KC, 128])
                dma("sync", t, src.rearrange("(k p) -> k p", p=128), w=[tk])
                pi = nextps()
                tr(ps[pi][:, 0:KC], t, ident[0:KC, 0:KC], r=[tk, "cmat"], w=[PSK(pi)])
                act(sT[:, :, rr], ps[pi][:, 0:KC], AF.Silu, r=[PSK(pi)], w=["sT"])

        def stage_mod(l):
            A.reset()
            n6 = 6 * KC
            j0 = 0
            while j0 < n6:
                nn = min(96, n6 - j0)
                load_vecT(bmT[:, j0:j0 + nn], I["b_mod"][l, j0 * 128:(j0 + nn) * 128], nn, "bmT")
                j0 += nn
            for i, nme in enumerate(("ln1_g", "ln1_b", "ln2_g", "ln2_b")):
                load_vecT(lnv[:, i, :], I[nme][l], KC, "lnv")
            wt = [A.tile([128, KC, 512]) for _ in range(2)]
            ncb = (6 * D) // 512
            for cb in range(ncb):
                w_t, wk = wt[cb % 2]
                for kk in range(0, KC, 8):
                    ke = min(KC, kk + 8)
                    dma("gpsimd" if (kk // 8) % 2 else "sync", w_t[:, kk:ke, :],
                        I["w_mod"][l, kk * 128:ke * 128, cb * 512:(cb + 1) * 512].rearrange("(k p) c -> p k c", p=128),
                        w=[(wk, kk // 8)])
                pi = nextps()
                for j in range(4):
                    for k in range(KC):
                        mm(ps[pi][:, j * NR:(j + 1) * NR], w_t[:, k, j * 128:(j + 1) * 128], sT[:, k, :],
                           k == 0, k == KC - 1, r=[(wk, k // 8), "sT"], w=[PSK(pi)])
                for j in range(4):
                    jj = cb * 4 + j
                    ts(modT[:, jj, :], ps[pi][:, j * NR:(j + 1) * NR], bmT[:, jj:jj + 1], None, ALU.add,
                       r=[PSK(pi), "bmT"], w=["modT"])
            ts(mod1p[:], modT[:], 1.0, None, ALU.add, r=["modT"], w=["mod1p"])
            ts(modga[:], modT[:], 1.0 / cfg.alpha, None, ALU.mult, r=["modT"], w=["modga"])

        def stage_init():
            A.reset()
            xin = [A.tile([128, 4, D]) for _ in range(1)]
            xo = [A.tile([128, KC, 512]) for _ in range(1)]
            for b in range(NB):
                for (t0, tn, isc) in cfg.tblocks(512):
                    xi, xik = xin[0]
                    nsub = tn // 128
                    src = I["ctx"][b, t0 - LAT:t0 - LAT + tn, :] if isc else I["x"][b, t0:t0 + tn, :]
                    dma("sync", xi[:, 0:nsub, :], src.rearrange("(s p) d -> p s d", p=128), w=[xik])
                    xt, xtk = xo[0]
                    for k in range(KC):
                        pi = nextps()
                        for s_ in range(nsub):
                            tr(ps[pi][:, s_ * 128:(s_ + 1) * 128], xi[:, s_, k * 128:(k + 1) * 128], ident,
                               r=[xik, "cmat"], w=[PSK(pi)])
                        if k % 2 == 0:
                            cp(xt[:, k, 0:tn], ps[pi][:, 0:tn], r=[PSK(pi)], w=[xtk])
                        else:
                            cp(xt[:, k, 0:tn], ps[pi][:, 0:tn], r=[PSK(pi)], w=[xtk], eng="scalar")
                    dma(STORE_Q, G["xT"][b, :, t0:t0 + tn].rearrange("(k p) t -> p k t", p=128), xt[:, :, 0:tn],
                        r=[xtk], w=[("xT", b)])

        def stage_modulate(i_shift, i_scale):
            A.reset()
            xb = [A.tile([128, KC, 256]) for _ in range(2)]
            hb = [A.tile([128, KC, 256], BF16) for _ in range(2)]
            it = 0
            for b in range(NB):
                for (t0, tn, isc) in cfg.tblocks(256):
                    xt, xk = xb[it % 2]
                    ht, hk = hb[it % 2]
                    it += 1
                    row = NB if isc else b
                    dma("sync", xt[:, :, 0:tn], G["xT"][b, :, t0:t0 + tn].rearrange("(k p) t -> p k t", p=128),
                        r=[("xT", b)], w=[xk])
                    for k in range(KC):
                        ts(ht[:, k, 0:tn], xt[:, k, 0:tn], mod1p[:, i_scale * KC + k, row:row + 1],
                           modT[:, i_shift * KC + k, row:row + 1], ALU.mult, ALU.add,
                           r=[xk, "mod1p", "modT"], w=[hk], eng="vector")
                    dma(STORE_Q, hT_ap(b, t0, tn), ht[:, :, 0:tn], r=[hk], w=[("hT", b)])

        def linear(Kc, src_fn, src_keys, w_fn, colblocks, kgroups=None, wcols=512, nabuf=2):
            if kgroups is None:
                kgroups = [(0, Kc)]
            wb = [A.tile([128, Kc, wcols], BF16) for _ in range(2)]
            ab = [A.tile([128, Kc, 512], BF16) for _ in range(nabuf)]
            ai = 0
            tiles = []
            cur = []
            curw = 0
            for cbk in colblocks:
                if cur and (curw + cbk[1] > wcols or cbk[2] != cur[-1][2] or cbk[0] != cur[-1][0] + cur[-1][1]):
                    tiles.append(cur)
                    cur, curw = [], 0
                cur.append(cbk)
                curw += cbk[1]
            if cur:
                tiles.append(cur)
            for wi, tl in enumerate(tiles):
                w_t, wk = wb[wi % 2]
                c_lo = tl[0][0]
                c_w = sum(x[1] for x in tl)
                step = max(1, 8192 // max(c_w, 1) // 4)
                for kk in range(0, Kc, 8):
                    ke = min(Kc, kk + 8)
                    dma("gpsimd", w_t[:, kk:ke, 0:c_w], w_fn(kk * 128, ke * 128, c_lo, c_lo + c_w).rearrange("(k p) c -> p k c", p=128),
                        w=[(wk, kk // 8)])
                for b in range(NB):
                    for (t0, tn, isc) in cfg.tblocks(512):
                        a_t, ak = ab[ai % nabuf]
                        ai += 1
                        dma("sync", a_t[:, :, 0:tn], src_fn(b, t0, tn),
                            r=[(kname, b) for kname in src_keys], w=[ak])
                        for (c0, cw, layout, epi) in tl:
                            lc = c0 - c_lo
                            if layout == "F":
                                pis = []
                                for (k0, k1) in kgroups:
                                    pi = nextps()
                                    pis.append(pi)
                                    for k in range(k0, k1):
                                        mm(ps[pi][0:cw, 0:tn], w_t[:, k, lc:lc + cw], a_t[:, k, 0:tn], k == k0, k == k1 - 1,
                                           r=[(wk, k // 8), ak], w=[PSK(pi)])
                                epi(b, c0, cw, t0, tn, pis)
                            else:
                                for s_ in range(tn // 128):
                                    pi = nextps()
                                    for k in range(Kc):
                                        mm(ps[pi][:, 0:cw], a_t[:, k, s_ * 128:(s_ + 1) * 128], w_t[:, k, lc:lc + cw], k == 0, k == Kc - 1,
                                           r=[(wk, k // 8), ak], w=[PSK(pi)])
                                    epi(b, c0, cw, t0 + s_ * 128, 128, [pi])

        stg = {"bufs": None, "i": 0}

        def staging(dt):
            if stg["bufs"] is None or stg["arena"] != arena_cnt[0] - stg["n"]:
                pass
            return None

        def make_store_epi(dst, key, dt, layout, col_base, func=None, scale=None, pool=None):
            if pool is None:
                pool = {"bufs": [A.tile([128, 512], F32) for _ in range(4)], "cnt": [0]}
            bufs = [((t if dt == F32 else t.bitcast(BF16)[:, 0:512]), k) for (t, k) in pool["bufs"]]
            cnt = pool["cnt"]

            def epi(b, c0, cw, t0, tn, pis):
                pi = pis[0]
                st, sk = bufs[cnt[0] % 4]
                cnt[0] += 1
                if layout == "F":
                    src = ps[pi][0:cw, 0:tn]
                    dsts = st[0:cw, 0:tn]
                    dram = dst[b, c0 - col_base:c0 - col_base + cw, t0:t0 + tn]
                else:
                    src = ps[pi][:, 0:cw]
                    dsts = st[:, 0:cw]
                    dram = dst[b, t0:t0 + tn, c0 - col_base:c0 - col_base + cw]
                if func is not None:
                    act(dsts, src, func, r=[PSK(pi)], w=[sk])
                elif cnt[0] % 2:
                    cp(dsts, src, r=[PSK(pi)], w=[sk])
                else:
                    cp(dsts, src, r=[PSK(pi)], w=[sk], eng="scalar")
                dma("sync", dram, dsts, r=[sk], w=[(key, b)])
            return epi

        def stage_inproj(l):
            A.reset()
            spec = {
                "attn_q": ("qraw", F32, "F", None), "attn_k": ("kraw", F32, "F", None), "attn_v": ("v_tok", BF16, "T", None),
                "hg_q": ("hgq", F32, "F", None), "hg_f_fwd": ("hgff", F32, "F", None), "hg_f_bwd": ("hgfb", F32, "F", None),
                "hg_i": ("hgi_tok", BF16, "T", None), "hg_g": ("hgg", F32, "F", None),
                "ml_q": ("mlq", F32, "F", None), "ml_k": ("mlk", F32, "F", None), "ml_v": ("mlv_tok", BF16, "T", None),
                "ml_gates": ("mlg", F32, "F", None), "ml_o": ("mlo", F32, "F", None), "merge": ("gate", BF16, "F", AF.Sigmoid),
            }
            colblocks = []
            pool = {"bufs": [A.tile([128, 512], F32) for _ in range(4)], "cnt": [0]}
            for name, width in cfg.segs:
                o0, _ = cfg.off[name]
                gname, dt, lay, fn = spec[name]
                epi = make_store_epi(G[gname], gname, dt, lay, o0, func=fn, pool=pool)
                step = 128 if lay == "F" else 512
                c = 0
                while c < width:
                    cw = min(step, width - c)
                    colblocks.append((o0 + c, cw, lay, epi))
                    c += cw
            linear(KC, lambda b, t0, tn: hT_ap(b, t0, tn), ["hT"],
                   lambda k0, k1, c0, c1: I["w_in"][l, k0:k1, c0:c1], colblocks, wcols=768)


        def vmemset(ap_, val, w):
            return S.op("vector", lambda e: e.memset(ap_, val), writes=w)

        def scan(out_, d0, d1, r, w):
            return S.op("vector", lambda e: e.tensor_tensor_scan(out=out_, data0=d0, data1=d1, initial=0.0, op0=ALU.mult, op1=ALU.add), reads=r, writes=w)

        def load_col(dst, vec_ap, n, key):
            dma("sync", dst, vec_ap.rearrange("(p o) -> p o", o=1), w=[key])

        def rstd_from(ps_ap, n, tmp, out_, rk, wk_):
            ts(tmp, ps_ap, 1.0 / n, EPS, ALU.mult, ALU.add, r=rk, w=[wk_ + "_t"])
            act(out_, tmp, AF.Sqrt, r=[wk_ + "_t"], w=[wk_]); recip(out_, out_, r=[wk_], w=[wk_])

        def stage_qk(l):
            A.reset()
            load_col(qn_g[:, 0:1], I["attn_q_norm"][l], 128, "qn_g0")
            load_col(qn_g[:, 1:2], I["attn_k_norm"][l], 128, "qn_g1")
            T_ = lambda dt=F32: A.tile([128, 512], dt)
            q_t, sq_t, r1_t, rs_t, qn_t, cs_t, sn_t, a_t, b_t = [T_() for _ in range(9)]
            o_t = T_(BF16)
            for b in range(NB):
                for (src, dst, nh, gc) in (("qraw", "QT", cfg.AH, 0), ("kraw", "KT", cfg.AKV, 1)):
                    for h in range(nh):
                        for (t0, tn, isc) in cfg.tblocks(512):
                            dma("sync", q_t[0][:, 0:tn], G[src][b, h * 128:(h + 1) * 128, t0:t0 + tn], r=[(src, b)], w=[q_t[1]])
                            act(sq_t[0][:, 0:tn], q_t[0][:, 0:tn], AF.Square, r=[q_t[1]], w=[sq_t[1]])
                            pi = nextps()
                            mm(ps[pi][:, 0:tn], ones, sq_t[0][:, 0:tn], True, True, r=["cmat", sq_t[1]], w=[PSK(pi)])
                            ts(r1_t[0][:, 0:tn], ps[pi][:, 0:tn], 1.0 / 128, EPS, ALU.mult, ALU.add, r=[PSK(pi)], w=[r1_t[1]])
                            act(rs_t[0][:, 0:tn], r1_t[0][:, 0:tn], AF.Sqrt, r=[r1_t[1]], w=[rs_t[1]]); recip(rs_t[0][:, 0:tn], rs_t[0][:, 0:tn], r=[rs_t[1]], w=[rs_t[1]])
                            stt(qn_t[0][:, 0:tn], q_t[0][:, 0:tn], qn_g[:, gc:gc + 1], rs_t[0][:, 0:tn], ALU.mult, ALU.mult,
                                r=[q_t[1], rs_t[1], "qn_g%d" % gc], w=[qn_t[1]])
                            if not isc:
                                dma("sync", cs_t[0][:, 0:tn], I["rope"][0, :, t0:t0 + tn], w=[cs_t[1]])
                                dma("sync", sn_t[0][:, 0:tn], I["rope"][1, :, t0:t0 + tn], w=[sn_t[1]])
                                pr = nextps()
                                mm(ps[pr][:, 0:tn], Rm, qn_t[0][:, 0:tn], True, True, r=["cmat", qn_t[1]], w=[PSK(pr)])
                                tt(a_t[0][:, 0:tn], qn_t[0][:, 0:tn], cs_t[0][:, 0:tn], ALU.mult, r=[qn_t[1], cs_t[1]], w=[a_t[1]])
                                tt(b_t[0][:, 0:tn], ps[pr][:, 0:tn], sn_t[0][:, 0:tn], ALU.mult, r=[PSK(pr), sn_t[1]], w=[b_t[1]])
                                tt(o_t[0][:, 0:tn], a_t[0][:, 0:tn], b_t[0][:, 0:tn], ALU.add, r=[a_t[1], b_t[1]], w=[o_t[1]])
                            else:
                                cp(o_t[0][:, 0:tn], qn_t[0][:, 0:tn], r=[qn_t[1]], w=[o_t[1]])
                            dma("gpsimd", G[dst][b, h * 128:(h + 1) * 128, t0:t0 + tn], o_t[0][:, 0:tn], r=[o_t[1]], w=[(dst, b)])

        def stage_attn(l):
            A.reset()
            NSB = TOK // 128
            kt, ktk = A.tile([128, TOK], BF16)
            vt, vtk = A.tile([128, NSB, 128], BF16)
            qts = [A.tile([128, TOK], BF16) for _ in range(2)]
            ebs = [A.tile([128, 512], BF16) for _ in range(3)]
            rt, rtk = A.tile([128, 512])
            obs = [A.tile([128, 512], BF16) for _ in range(2)]
            cnt = [0, 0, 0]
            for b in range(NB):
                for hk in range(cfg.AKV):
                    dma("sync", kt, G["KT"][b, hk * 128:(hk + 1) * 128, :], r=[("KT", b)], w=[ktk])
                    dma("sync", vt, G["v_tok"][b, :, hk * 128:(hk + 1) * 128].rearrange("(j p) e -> p j e", p=128), r=[("v_tok", b)], w=[vtk])
                    for g_ in range(4):
                        h = hk * 4 + g_
                        qt, qtk = qts[h % 2]
                        dma("sync", qt, G["QT"][b, h * 128:(h + 1) * 128, :], r=[("QT", b)], w=[qtk])
                        for (t0, tn, isc) in cfg.tblocks(512):
                            sbl = list(range(LAT // 128, NSB)) if isc else list(range(NSB))
                            pN = 3 + cnt[1] % 2
                            pD = 5 + cnt[1] % 2
                            cnt[1] += 1
                            for ix, j in enumerate(sbl):
                                pS = cnt[0] % 3
                                eb, ebk = ebs[cnt[0] % 3]
                                cnt[0] += 1
                                mm(ps[pS][:, 0:tn], kt[:, j * 128:(j + 1) * 128], qt[:, t0:t0 + tn], True, True, r=[ktk, qtk], w=[PSK(pS)])
                                act(eb[:, 0:tn], ps[pS][:, 0:tn], AF.Exp, r=[PSK(pS)], w=[ebk], scale=float(128 ** -0.5))
                                mm(ps[pN][:, 0:tn], vt[:, j, :], eb[:, 0:tn], ix == 0, ix == len(sbl) - 1, r=[vtk, ebk], w=[PSK(pN)])
                                mm(ps[pD][:, 0:tn], onesb, eb[:, 0:tn], ix == 0, ix == len(sbl) - 1, r=["cmatb", ebk], w=[PSK(pD)])
                            recip(rt[:, 0:tn], ps[pD][:, 0:tn], r=[PSK(pD)], w=[rtk])
                            ob, obk = obs[cnt[2] % 2]
                            cnt[2] += 1
                            tt(ob[:, 0:tn], ps[pN][:, 0:tn], rt[:, 0:tn], ALU.mult, r=[PSK(pN), rtk], w=[obk])
                            dma("gpsimd", G["yT"][b, h * 128:(h + 1) * 128, t0:t0 + tn], ob[:, 0:tn], r=[obk], w=[("yT", b)])

        def gla_alloc(EV):
            T = {}
            for n in ("q", "lf", "kk", "pre", "bc", "d1", "tmp"):
                T[n] = A.tile([128, TOK])
            for n in ("Qs", "At", "Ke"):
                T[n] = A.tile([128, TOK], BF16)
            NSB = TOK // 128
            T["Ketok"] = A.tile([128, NSB, 128], BF16)
            T["V"] = A.tile([128, NSB, EV], BF16)
            T["dec"] = A.tile([128, TOK // 32])
            T["S"] = A.tile([128, EV])
            T["Sbf"] = [A.tile([128, EV], BF16) for _ in range(8)]
            T["WT"] = [A.tile([128, 128], BF16) for _ in range(2)]
            T["rm"] = A.tile([128, TOK])
            dma("sync", T["rm"][0], I["rmask"][:, :], w=[T["rm"][1]])
            return T

        def gla_dir(T, EV, dirn, post):
            NSB = TOK // 128
            NCH = TOK // 32
            q, qk_ = T["q"]; lf, lfk = T["lf"]; kk, kkk = T["kk"]; pre, prek = T["pre"]
            bc, bck = T["bc"]; d1, d1k = T["d1"]; tmp, tmpk = T["tmp"]
            Qs, Qsk = T["Qs"]; At, Atk = T["At"]; Ke, Kek = T["Ke"]; Ktok, Ktokk = T["Ketok"]
            V, Vk = T["V"]; dec, deck = T["dec"]; Sf, Sfk = T["S"]; rm, rmk = T["rm"]
            scan(pre, rm, lf, r=[rmk, lfk], w=[prek])
            pre3 = pre.rearrange("p (c s) -> p c s", s=32)
            totb = pre3[:, :, 31:32].to_broadcast([128, NCH, 32])
            v3 = lambda ap_: ap_.rearrange("p (c s) -> p c s", s=32)
            if dirn == 0:
                cp(bc, pre, r=[prek], w=[bck], eng="scalar")
            else:
                tt(tmp, lf, pre, ALU.subtract, r=[lfk, prek], w=[tmpk])
                tt(v3(bc), v3(tmp), totb, ALU.add, r=[tmpk, prek], w=[bck])
            tt(v3(d1), v3(bc), totb, ALU.subtract, r=[bck, prek], w=[d1k])
            act(tmp, bc, AF.Exp, r=[bck], w=[tmpk])
            tt(Qs, tmp, q, ALU.mult, r=[tmpk, qk_], w=[Qsk])
            act(tmp, d1, AF.Exp, r=[d1k], w=[tmpk])
            tt(At, tmp, q, ALU.mult, r=[tmpk, qk_], w=[Atk])
            act(tmp, d1, AF.Exp, r=[d1k], w=[tmpk], scale=-1.0)
            tt(Ke, tmp, kk, ALU.mult, r=[tmpk, kkk], w=[Kek])
            act(dec, pre3[:, :, 31], AF.Exp, r=[prek], w=[deck])
            for j0 in range(0, NSB, 8):
                j1 = min(NSB, j0 + 8)
                for j in range(j0, j1):
                    tr(psb[:, (j - j0) * 128:(j - j0 + 1) * 128], Ke[:, j * 128:(j + 1) * 128], identb, r=[Kek, "cmatb"], w=["psb"])
                cp(Ktok[:, j0:j1, :], psb[:, 0:(j1 - j0) * 128].rearrange("p (j d) -> p j d", d=128), r=["psb"], w=[Ktokk])
            sfx = "_d%d" % dirn
            dbgdump("g_pre" + sfx, pre, [128, TOK], F32, prek)
            dbgdump("g_bc" + sfx, bc, [128, TOK], F32, bck)
            dbgdump("g_d1" + sfx, d1, [128, TOK], F32, d1k)
            dbgdump("g_Qs" + sfx, Qs, [128, TOK], BF16, Qsk)
            dbgdump("g_At" + sfx, At, [128, TOK], BF16, Atk)
            dbgdump("g_Ke" + sfx, Ke, [128, TOK], BF16, Kek)
            dbgdump("g_dec" + sfx, dec, [128, TOK // 32], F32, deck)
            dbgdump("g_Ktok" + sfx, Ktok, [128, NSB, 128], BF16, Ktokk)
            dbgdump("g_lf" + sfx, lf, [128, TOK], F32, lfk)
            dbgdump("g_kk" + sfx, kk, [128, TOK], F32, kkk)
            dbgdump("g_q" + sfx, q, [128, TOK], F32, qk_)
            vmemset(Sf, 0.0, [Sfk])
            lat_b = list(range(0, LAT // 128))
            ctx_b = list(range(LAT // 128, NSB))
            order = (ctx_b + lat_b) if dirn == 0 else (ctx_b[::-1] + lat_b[::-1])
            corder = [0, 1, 2, 3] if dirn == 0 else [3, 2, 1, 0]
            maskb = maskFb if dirn == 0 else maskBb
            NEH = EV // 128
            ring = [0]
            wcnt = [0]
            for blk in order:
                for c in corder:
                    p0, p1 = (64, 128) if c == 3 else (32 * c, 32 * c + 32)
                    mm(ps[c][:, 0:EV], Ktok[p0:p1, blk, :], V[p0:p1, blk, :], True, True, r=[Ktokk, Vk], w=[PSK(c)])
                sb_of = {}
                for c in corder:
                    sb, sbk = T["Sbf"][ring[0] % 8]
                    ring[0] += 1
                    sb_of[c] = (sb, sbk)
                    cp(sb, Sf, r=[Sfk], w=[sbk], eng="scalar")
                    ci = blk * 4 + c
                    stt(Sf, Sf, dec[:, ci:ci + 1], ps[c][:, 0:EV], ALU.mult, ALU.add, r=[Sfk, deck, PSK(c)], w=[Sfk])
                    if c == 3:
                        tt(Sf, Sf, ps[2][:, 0:EV], ALU.subtract, r=[Sfk, PSK(2)], w=[Sfk])
                mm(ps[4][:, 0:128], Ke[:, blk * 128:(blk + 1) * 128], At[:, blk * 128:(blk + 1) * 128], True, True, r=[Kek, Atk], w=[PSK(4)])
                wt_, wtk = T["WT"][wcnt[0] % 2]
                wcnt[0] += 1
                tt(wt_, ps[4][:, 0:128], maskb, ALU.mult, r=[PSK(4), "cmatb"], w=[wtk])
                ehs = [NEH - 1] + list(range(NEH - 1)) if NEH == 3 else list(range(NEH))
                for eh in ehs:
                    pO = 5 + (wcnt[0] + eh) % 2
                    mm(ps[pO][:, 0:128], V[:, blk, eh * 128:(eh + 1) * 128], wt_, True, False, r=[Vk, wtk], w=[PSK(pO)])
                    for ix, c in enumerate(corder):
                        sb, sbk = sb_of[c]
                        mm(ps[pO][:, 32 * c:32 * c + 32], sb[:, eh * 128:(eh + 1) * 128],
                           Qs[:, blk * 128 + 32 * c:blk * 128 + 32 * c + 32], False, ix == 3, r=[sbk, Qsk], w=[PSK(pO)])
                    post(dirn, eh, pO, blk)

        def stage_hgrn(l):
            A.reset()
            T = gla_alloc(128)
            acc, acck = A.tile([128, TOK])
            gs, gsk = A.tile([128, TOK])
            sq, sqk = A.tile([128, TOK])
            yb, ybk = A.tile([128, TOK], BF16)
            r1, r1k = A.tile([128, 512]); rs, rsk = A.tile([128, 512])
            load_col(qn_g[:, 2:3], I["hg_norm"][l], 128, "qn_g2")
            for d_ in range(2):
                if l == 0:
                    vmemset(lbv[:, d_, 0, :], 0.0, ["lbv%d" % d_])
                    vmemset(lbv[:, d_, 1, :], 1.0, ["lbv%d" % d_])
                else:
                    load_vecT(lbv[:, d_, 0, :], I["hg_lb_logits"][d_, 0], cfg.HGH, "lbv%d" % d_)
                    load_vecT(lbv[:, d_, 1, :], I["hg_lb_logits"][d_, 1], cfg.HGH, "lbv%d" % d_)
                    tt(lbv[:, d_, 2, :], lbv[:, d_, 0, :], lbv[:, d_, 1, :], ALU.subtract, r=["lbv%d" % d_], w=["lbv%d" % d_])
                    act(lbv[:, d_, 2, :], lbv[:, d_, 2, :], AF.Exp, r=["lbv%d" % d_], w=["lbv%d" % d_])
                    ts(lbv[:, d_, 2, :], lbv[:, d_, 2, :], 1.0, None, ALU.add, r=["lbv%d" % d_], w=["lbv%d" % d_])
                    recip(lbv[:, d_, 0, :], lbv[:, d_, 2, :], r=["lbv%d" % d_], w=["lbv%d" % d_])
                    ts(lbv[:, d_, 1, :], lbv[:, d_, 0, :], -1.0, 1.0, ALU.mult, ALU.add, r=["lbv%d" % d_], w=["lbv%d" % d_])

            def post(dirn, eh, pO, blk):
                sl = acc[:, blk * 128:(blk + 1) * 128]
                if dirn == 0:
                    cp(sl, ps[pO][:, 0:128], r=[PSK(pO)], w=[acck], eng="scalar")
                else:
                    tt(sl, sl, ps[pO][:, 0:128], ALU.add, r=[PSK(pO), acck], w=[acck])

            for b in range(NB):
                for h in range(cfg.HGH):
                    rows = slice(h * 128, (h + 1) * 128)
                    q, qk_ = T["q"]
                    dma("sync", T["tmp"][0], G["hgq"][b, rows, :], r=[("hgq", b)], w=[T["tmp"][1]])
                    act(q, T["tmp"][0], AF.Silu, r=[T["tmp"][1]], w=[qk_])
                    ts(q, q, float(128 ** -0.5), None, ALU.mult, r=[qk_], w=[qk_])
                    dma("sync", T["V"][0], G["hgi_tok"][b, :, rows].rearrange("(j p) e -> p j e", p=128), r=[("hgi_tok", b)], w=[T["V"][1]])
                    for d_ in range(2):
                        lf, lfk = T["lf"]; kk, kkk = T["kk"]; tmp, tmpk = T["tmp"]
                        dma("sync", tmp, G["hgff" if d_ == 0 else "hgfb"][b, rows, :], r=[("hgff" if d_ == 0 else "hgfb", b)], w=[tmpk])
                        act(lf, tmp, AF.Exp, r=[tmpk], w=[lfk], scale=-1.0)
                        ts(lf, lf, 1.0, None, ALU.add, r=[lfk], w=[lfk])
                        recip(kk, lf, r=[lfk], w=[kkk])
                        ts(kk, kk, lbv[:, d_, 1, h:h + 1], lbv[:, d_, 0, h:h + 1], ALU.mult, ALU.add, r=[kkk, "lbv%d" % d_], w=[kkk])
                        act(lf, kk, AF.Ln, r=[kkk], w=[lfk])
                        ts(kk, kk, -1.0, 1.0, ALU.mult, ALU.add, r=[kkk], w=[kkk])
                        gla_dir(T, 128, d_, post)
                    dma("sync", T["tmp"][0], G["hgg"][b, rows, :], r=[("hgg", b)], w=[T["tmp"][1]])
                    act(gs, T["tmp"][0], AF.Silu, r=[T["tmp"][1]], w=[gsk])
                    act(sq, acc, AF.Square, r=[acck], w=[sqk])
                    for (t0, tn, isc) in cfg.tblocks(512):
                        pi = nextps()
                        mm(ps[pi][:, 0:tn], ones, sq[:, t0:t0 + tn], True, True, r=["cmat", sqk], w=[PSK(pi)])
                        ts(r1[:, 0:tn], ps[pi][:, 0:tn], 1.0 / 128, EPS, ALU.mult, ALU.add, r=[PSK(pi)], w=[r1k])
                        act(rs[:, 0:tn], r1[:, 0:tn], AF.Sqrt, r=[r1k], w=[rsk]); recip(rs[:, 0:tn], rs[:, 0:tn], r=[rsk], w=[rsk])
                        stt(r1[:, 0:tn], acc[:, t0:t0 + tn], qn_g[:, 2:3], rs[:, 0:tn], ALU.mult, ALU.mult, r=[acck, rsk, "qn_g2"], w=[r1k])
                        tt(yb[:, t0:t0 + tn], r1[:, 0:tn], gs[:, t0:t0 + tn], ALU.mult, r=[r1k, gsk], w=[ybk])
                    dma("gpsimd", G["yT"][b, cfg.BW + h * 128:cfg.BW + (h + 1) * 128, :], yb, r=[ybk], w=[("yT", b)])

        def stage_mlstm(l):
            A.reset()
            EV = 384
            T = gla_alloc(EV)
            NG = 4 * cfg.MLH
            accs = [A.tile([128, TOK]) for _ in range(2)]
            sq, sqk = A.tile([128, TOK])
            yb, ybk = A.tile([128, TOK], BF16)
            gI, gIk = A.tile([32, TOK]); gL, gLk = A.tile([32, TOK])
            r1, r1k = A.tile([128, 512]); rs, rsk = A.tile([128, 512])
            rinv, rinvk = A.tile([128, 128]); hn, hnk = A.tile([128, 128])
            dma("sync", mln_g[:, 0:2], I["ml_norm"][l].rearrange("(e p) -> p e", p=128), w=["mln_g"], allow_slow_non_contiguous=True)
            vmemset(mlb[:], 0.0, ["mlb"])
            dma("sync", mlb[0:NG, 0:1], I["ml_gate_bias"][l].rearrange("(p o) -> p o", o=1), r=["mlb"], w=["mlb"])
            ts(mlb[:, 1:2], mlb[:, 0:1], -1.0, None, ALU.mult, r=["mlb"], w=["mlb"])

            def post(dirn, eh, pO, blk):
                if eh == 2:
                    ts(hn, ps[pO][:, 0:128], -1.0, None, ALU.mult, r=[PSK(pO)], w=[hnk])
                    tt(rinv, hn, ps[pO][:, 0:128], ALU.max, r=[PSK(pO), hnk], w=[rinvk])
                    ts(rinv, rinv, 1.0, None, ALU.max, r=[rinvk], w=[rinvk])
                    recip(rinv, rinv, r=[rinvk], w=[rinvk])
                    return
                acc, acck = accs[eh]
                sl = acc[:, blk * 128:(blk + 1) * 128]
                if dirn == 0:
                    tt(sl, ps[pO][:, 0:128], rinv, ALU.mult, r=[PSK(pO), rinvk], w=[acck])
                else:
                    tt(hn, ps[pO][:, 0:128], rinv, ALU.mult, r=[PSK(pO), rinvk], w=[hnk])
                    tt(sl, sl, hn, ALU.add, r=[hnk, acck], w=[acck])

            for b in range(NB):
                vmemset(gI, 0.0, [gIk]); vmemset(gL, 0.0, [gLk])
                dma("sync", gI[0:NG, :], G["mlg"][b, :, :], r=[("mlg", b), gIk], w=[gIk])
                act(gL, gI, AF.Exp, r=[gIk, "mlb"], w=[gLk], bias=mlb[:, 1:2], scale=-1.0)
                ts(gL, gL, 1.0, None, ALU.add, r=[gLk], w=[gLk])
                act(gL, gL, AF.Ln, r=[gLk], w=[gLk])
                ts(gL, gL, -1.0, None, ALU.mult, r=[gLk], w=[gLk])
                act(gI, gI, AF.Exp, r=[gIk, "mlb"], w=[gIk], bias=mlb[:, 0:1], scale=1.0)
                for h in range(cfg.MLH):
                    rows = slice(h * 128, (h + 1) * 128)
                    V, Vk = T["V"]
                    vmemset(V[:, :, 256:384], 1.0, [Vk])
                    dma("sync", V[:, :, 0:256], G["mlv_tok"][b, :, h * 256:(h + 1) * 256].rearrange("(j p) e -> p j e", p=128), r=[("mlv_tok", b), Vk], w=[Vk])
                    dma("sync", T["q"][0], G["mlq"][b, rows, :], r=[("mlq", b)], w=[T["q"][1]])
                    for d_ in range(2):
                        lf, lfk = T["lf"]; kk, kkk = T["kk"]; tmp, tmpk = T["tmp"]
                        dma("sync", tmp, G["mlk"][b, rows, :], r=[("mlk", b)], w=[tmpk])
                        ri = d_ * cfg.MLH + h
                        rf = (2 + d_) * cfg.MLH + h
                        for (t0, tn, isc) in cfg.tblocks(512):
                            pi = nextps()
                            mm(ps[pi][:, 0:tn], sel[:, rf * 128:(rf + 1) * 128], gL[:, t0:t0 + tn], True, True, r=["sel", gLk], w=[PSK(pi)])
                            cp(lf[:, t0:t0 + tn], ps[pi][:, 0:tn], r=[PSK(pi)], w=[lfk], eng="scalar")
                            pj = nextps()
                            mm(ps[pj][:, 0:tn], sel[:, ri * 128:(ri + 1) * 128], gI[:, t0:t0 + tn], True, True, r=["sel", gIk], w=[PSK(pj)])
                            stt(kk[:, t0:t0 + tn], tmp[:, t0:t0 + tn], float(128 ** -0.5), ps[pj][:, 0:tn], ALU.mult, ALU.mult, r=[tmpk, PSK(pj)], w=[kkk])
                        gla_dir(T, EV, d_, post)
                    for eh in range(2):
                        act(sq, accs[eh][0], AF.Square, r=[accs[eh][1]], w=[sqk]) if eh == 0 else None
                    sq2, sq2k = T["pre"]
                    act(sq2, accs[1][0], AF.Square, r=[accs[1][1]], w=[sq2k])
                    for eh in range(2):
                        dma("sync", T["tmp"][0], G["mlo"][b, h * 256 + eh * 128:h * 256 + (eh + 1) * 128, :], r=[("mlo", b)], w=[T["tmp"][1]])
                        act(T["bc"][0], T["tmp"][0], AF.Sigmoid, r=[T["tmp"][1]], w=[T["bc"][1]])
                        for (t0, tn, isc) in cfg.tblocks(512):
                            pi = nextps()
                            mm(ps[pi][:, 0:tn], ones, sq[:, t0:t0 + tn], True, False, r=["cmat", sqk], w=[PSK(pi)])
                            mm(ps[pi][:, 0:tn], ones, sq2[:, t0:t0 + tn], False, True, r=["cmat", sq2k], w=[PSK(pi)])
                            ts(r1[:, 0:tn], ps[pi][:, 0:tn], 1.0 / 256, EPS, ALU.mult, ALU.add, r=[PSK(pi)], w=[r1k])
                            act(rs[:, 0:tn], r1[:, 0:tn], AF.Sqrt, r=[r1k], w=[rsk]); recip(rs[:, 0:tn], rs[:, 0:tn], r=[rsk], w=[rsk])
                            stt(r1[:, 0:tn], accs[eh][0][:, t0:t0 + tn], mln_g[:, eh:eh + 1], rs[:, 0:tn], ALU.mult, ALU.mult, r=[accs[eh][1], rsk, "mln_g"], w=[r1k])
                            tt(yb[:, t0:t0 + tn], r1[:, 0:tn], T["bc"][0][:, t0:t0 + tn], ALU.mult, r=[r1k, T["bc"][1]], w=[ybk])
                        r0 = 2 * cfg.BW + h * 256 + eh * 128
                        dma("gpsimd", G["yT"][b, r0:r0 + 128, :], yb, r=[ybk], w=[("yT", b)])


        def stage_merge(l):
            A.reset()
            gts = [A.tile([128, 512], BF16) for _ in range(3)]
            m0, m0k = A.tile([128, 512]); m1, m1k = A.tile([128, 512])
            ob, obk = A.tile([128, 512], BF16)
            KCb = cfg.BW // 128

            def epi(b, c0, cw, t0, tn, pis):
                for n in range(3):
                    dma("sync", gts[n][0][0:cw, 0:tn], G["gate"][b, n * D + c0:n * D + c0 + cw, t0:t0 + tn], r=[("gate", b)], w=[gts[n][1]])
                tt(m0[0:cw, 0:tn], ps[pis[0]][0:cw, 0:tn], gts[0][0][0:cw, 0:tn], ALU.mult, r=[PSK(pis[0]), gts[0][1]], w=[m0k])
                tt(m1[0:cw, 0:tn], ps[pis[1]][0:cw, 0:tn], gts[1][0][0:cw, 0:tn], ALU.mult, r=[PSK(pis[1]), gts[1][1]], w=[m1k])
                tt(m0[0:cw, 0:tn], m0[0:cw, 0:tn], m1[0:cw, 0:tn], ALU.add, r=[m0k, m1k], w=[m0k])
                tt(m1[0:cw, 0:tn], ps[pis[2]][0:cw, 0:tn], gts[2][0][0:cw, 0:tn], ALU.mult, r=[PSK(pis[2]), gts[2][1]], w=[m1k])
                tt(ob[0:cw, 0:tn], m0[0:cw, 0:tn], m1[0:cw, 0:tn], ALU.add, r=[m0k, m1k], w=[obk])
                dma("sync", G["mergedT"][b, c0:c0 + cw, t0:t0 + tn], ob[0:cw, 0:tn], r=[obk], w=[("mergedT", b)])

            cbs = [(c, 128, "F", epi) for c in range(0, D, 128)]
            wbr = I["w_branch"][l].rearrange("n w d -> (n w) d")
            linear(3 * KCb, lambda b, t0, tn: G["yT"][b, :, t0:t0 + tn].rearrange("(k p) t -> p k t", p=128), ["yT"],
                   lambda k0, k1, c0, c1: wbr[k0:k1, c0:c1], cbs,
                   kgroups=[(0, KCb), (KCb, 2 * KCb), (2 * KCb, 3 * KCb)], wcols=512, nabuf=1)

        def stage_wout(l):
            A.reset()
            epi = make_store_epi(G["y2T"], "y2T", F32, "F", 0)
            cbs = [(c, 128, "F", epi) for c in range(0, D, 128)]
            linear(KC, lambda b, t0, tn: G["mergedT"][b, :, t0:t0 + tn].rearrange("(k p) t -> p k t", p=128), ["mergedT"],
                   lambda k0, k1, c0, c1: I["w_out"][l, k0:k1, c0:c1], cbs)

        def stage_ln(l, which):
            A.reset()
            TB = 256
            gi = 2 if which == 0 else 5
            lg_i, lb_i = (0, 1) if which == 0 else (2, 3)
            srcn = "y2T" if which == 0 else "faccT"
            x_t, xk = A.tile([128, KC, TB]); y_t, yk = A.tile([128, KC, TB])
            sqs = [A.tile([128, TB]) for _ in range(2)]
            mean, mk = A.tile([128, TB]); rstd, rk_ = A.tile([128, TB]); t1s = [A.tile([128, TB]) for _ in range(2)]
            if which == 0:
                h2f, h2fk = A.tile([128, KC, TB]); h2b, h2bk = A.tile([128, KC, TB], BF16)
                lgT, lgTk = A.tile([16, TB]); cT, cTk = A.tile([16, TB])
                lg, lgk = A.tile([128, 16]); ee, eek = A.tile([128, 16]); selm, selmk = A.tile([128, 16])
                mx, mxk = A.tile([128, 4]); psm, psmk = A.tile([128, 4]); gsc, gsck = A.tile([128, 4]); gsel, gselk = A.tile([128, 4])
                cntt, cnttk = A.tile([128, 4]); cmpt, cmptk = A.tile([128, 4]); den, denk = A.tile([128, 4])
                dma("sync", wr[:], I["w_router"].rearrange("(k p) e -> p k e", p=128), w=["wr"])
                dma("sync", brb[:], I["b_router"].partition_broadcast(128), w=["brb"])
            for b in range(NB):
                for (t0, tn, isc) in cfg.tblocks(TB):
                    row = NB if isc else b
                    dma("sync", x_t[:, :, 0:tn], G["xT"][b, :, t0:t0 + tn].rearrange("(k p) t -> p k t", p=128), r=[("xT", b)], w=[xk])
                    dma(STORE_Q, y_t[:, :, 0:tn], G[srcn][b, :, t0:t0 + tn].rearrange("(k p) t -> p k t", p=128), r=[(srcn, b)], w=[yk])
                    for k in range(KC):
                        stt(x_t[:, k, 0:tn], y_t[:, k, 0:tn], modga[:, gi * KC + k, row:row + 1], x_t[:, k, 0:tn], ALU.mult, ALU.add,
                            r=[xk, yk, "modga"], w=[xk])
                    for k in range(KC):
                        mm(ps[0][:, 0:tn], ones, x_t[:, k, 0:tn], k == 0, k == KC - 1, r=["cmat", xk], w=[PSK(0)])
                    for k in range(KC):
                        sq, sqk = sqs[k % 2]
                        act(sq[:, 0:tn], x_t[:, k, 0:tn], AF.Square, r=[xk], w=[sqk])
                        mm(ps[1][:, 0:tn], ones, sq[:, 0:tn], k == 0, k == KC - 1, r=["cmat", sqk], w=[PSK(1)])
                    ts(mean[:, 0:tn], ps[0][:, 0:tn], 1.0 / D, None, ALU.mult, r=[PSK(0)], w=[mk])
                    tt(rstd[:, 0:tn], mean[:, 0:tn], mean[:, 0:tn], ALU.mult, r=[mk], w=[rk_])
                    stt(rstd[:, 0:tn], ps[1][:, 0:tn], 1.0 / D, rstd[:, 0:tn], ALU.mult, ALU.subtract, r=[PSK(1), rk_], w=[rk_])
                    ts(rstd[:, 0:tn], rstd[:, 0:tn], EPS / (cfg.alpha ** 2), None, ALU.add, r=[rk_], w=[rk_])
                    act(rstd[:, 0:tn], rstd[:, 0:tn], AF.Sqrt, r=[rk_], w=[rk_])
                    recip(rstd[:, 0:tn], rstd[:, 0:tn], r=[rk_], w=[rk_])
                    for k in range(KC):
                        t1, t1k = t1s[k % 2]
                        tt(t1[:, 0:tn], x_t[:, k, 0:tn], mean[:, 0:tn], ALU.subtract, r=[xk, mk], w=[t1k])
                        tt(t1[:, 0:tn], t1[:, 0:tn], rstd[:, 0:tn], ALU.mult, r=[t1k, rk_], w=[t1k])
                        ts(x_t[:, k, 0:tn], t1[:, 0:tn], lnv[:, lg_i, k:k + 1], lnv[:, lb_i, k:k + 1], ALU.mult, ALU.add, r=[t1k, "lnv"], w=[xk])
                    dma("sync", G["xT"][b, :, t0:t0 + tn].rearrange("(k p) t -> p k t", p=128), x_t[:, :, 0:tn], r=[xk], w=[("xT", b)])
                    if which != 0:
                        continue
                    for k in range(KC):
                        ts(h2f[:, k, 0:tn], x_t[:, k, 0:tn], mod1p[:, 4 * KC + k, row:row + 1], modT[:, 3 * KC + k, row:row + 1], ALU.mult, ALU.add,
                           r=[xk, "mod1p", "modT"], w=[h2fk])
                    cp(h2b[:, :, 0:tn], h2f[:, :, 0:tn], r=[h2fk], w=[h2bk], eng="scalar")
                    dma(STORE_Q, hT_ap(b, t0, tn), h2b[:, :, 0:tn], r=[h2bk], w=[("hT", b)])
                    for k in range(KC):
                        mm(ps[2][0:16, 0:tn], wr[:, k, :], h2f[:, k, 0:tn], k == 0, k == KC - 1, r=["wr", h2fk], w=[PSK(2)])
                    cp(lgT[:, 0:tn], ps[2][0:16, 0:tn], r=[PSK(2)], w=[lgTk])
                    for s_ in range(tn // 128):
                        tr(ps[3][:, 0:16], lgT[:, s_ * 128:(s_ + 1) * 128], ident[0:16, 0:16], r=[lgTk, "cmat"], w=[PSK(3)])
                        tt(lg, ps[3][:, 0:16], brb[:], ALU.add, r=[PSK(3), "brb"], w=[lgk])
                        S.op("vector", lambda e: e.reduce_max(out=mx[:, 0:1], in_=lg, axis=AX.X), reads=[lgk], writes=[mxk])
                        ts(mx[:, 0:1], mx[:, 0:1], -1.0, None, ALU.mult, r=[mxk], w=[mxk])
                        act(ee, lg, AF.Exp, r=[lgk, mxk], w=[eek], bias=mx[:, 0:1], scale=1.0)
                        e3 = ee.rearrange("p (g i) -> p g i", i=4)
                        first = True
                        for i_ in range(4):
                            for j_ in range(i_ + 1, 4):
                                tt(psm, e3[:, :, i_], e3[:, :, j_], ALU.add, r=[eek], w=[psmk])
                                if first:
                                    cp(gsc, psm, r=[psmk], w=[gsck]); first = False
                                else:
                                    tt(gsc, gsc, psm, ALU.max, r=[gsck, psmk], w=[gsck])
                        S.op("vector", lambda e: e.reduce_max(out=mx[:, 1:2], in_=gsc, axis=AX.X), reads=[gsck], writes=[mxk])
                        ts(gsel, gsc, mx[:, 1:2], None, ALU.is_ge, r=[gsck, mxk], w=[gselk])
                        s3 = selm.rearrange("p (g i) -> p g i", i=4)
                        for i_ in range(4):
                            vmemset(cntt, 0.0, [cnttk])
                            for j_ in range(4):
                                if j_ == i_:
                                    continue
                                tt(cmpt, e3[:, :, j_], e3[:, :, i_], ALU.is_gt, r=[eek], w=[cmptk])
                                tt(cntt, cntt, cmpt, ALU.add, r=[cnttk, cmptk], w=[cnttk])
                            ts(cntt, cntt, 1.5, None, ALU.is_lt, r=[cnttk], w=[cnttk])
                            tt(s3[:, :, i_], cntt, gsel, ALU.mult, r=[cnttk, gselk], w=[selmk])
                        tt(selm, selm, ee, ALU.mult, r=[selmk, eek], w=[selmk])
                        S.op("vector", lambda e: e.reduce_sum(out=den[:, 0:1], in_=selm, axis=AX.X), reads=[selmk], writes=[denk])
                        recip(den[:, 0:1], den[:, 0:1], r=[denk], w=[denk])
                        ts(selm, selm, den[:, 0:1], None, ALU.mult, r=[selmk, denk], w=[selmk])
                        tr(ps[4][0:16, 0:128], selm, ident, r=[selmk, "cmat"], w=[PSK(4)])
                        cp(cT[:, s_ * 128:(s_ + 1) * 128], ps[4][0:16, 0:128], r=[PSK(4)], w=[cTk])
                    dma("sync", G["combT"][b, :, t0:t0 + tn], cT[:, 0:tn], r=[cTk], w=[("combT", b)])

        def stage_moe(l):
            DEc = cfg.DE // 128
            for e_ in range(16):
                A.reset()
                cbt, cbk = A.tile([32, 512]); cbb, cbbk = A.tile([128, 512])
                sg, sgk = A.tile([128, 512]); hb, hbk = A.tile([128, 512], BF16)
                vmemset(cbt, 0.0, [cbk])
                act(sg[:, 0:128], ones, AF.Silu, r=["cmat"], w=[sgk])
                state = {"key": None}

                def epi_gu(b, c0, cw, t0, tn, pis, e_=e_):
                    if state["key"] != (b, t0):
                        state["key"] = (b, t0)
                        dma("sync", cbt[0:16, 0:tn], G["combT"][b, :, t0:t0 + tn], r=[("combT", b), cbk], w=[cbk])
                        mm(ps[6][:, 0:tn], sel[:, e_ * 128:(e_ + 1) * 128], cbt[:, 0:tn], True, True, r=["sel", cbk], w=[PSK(6)])
                        cp(cbb[:, 0:tn], ps[6][:, 0:tn], r=[PSK(6)], w=[cbbk])
                    act(sg[0:cw, 0:tn], ps[pis[0]][0:cw, 0:tn], AF.Silu, r=[PSK(pis[0])], w=[sgk])
                    tt(sg[0:cw, 0:tn], sg[0:cw, 0:tn], ps[pis[1]][0:cw, 0:tn], ALU.mult, r=[sgk, PSK(pis[1])], w=[sgk])
                    tt(hb[0:cw, 0:tn], sg[0:cw, 0:tn], cbb[0:cw, 0:tn], ALU.mult, r=[sgk, cbbk], w=[hbk])
                    r0_ = (e_ % EG) * cfg.DE + c0
                    dma("sync", G["hid"][b, r0_:r0_ + cw, t0:t0 + tn], hb[0:cw, 0:tn], r=[hbk], w=[("hid", b)])

                gu_linear(l, e_, epi_gu)
                if e_ % EG != EG - 1:
                    continue
                if "stop_e0" in dbg and l == 1:
                    return
                A.reset()
                ac, ack = A.tile([128, 512]); o2, o2k = A.tile([128, 512])

                def epi_d(b, c0, cw, t0, tn, pis, e_=e_):
                    if e_ == EG - 1:
                        cp(o2[0:cw, 0:tn], ps[pis[0]][0:cw, 0:tn], r=[PSK(pis[0])], w=[o2k])
                    else:
                        dma("sync", ac[0:cw, 0:tn], G["faccT"][b, c0:c0 + cw, t0:t0 + tn], r=[("faccT", b)], w=[ack])
                        tt(o2[0:cw, 0:tn], ac[0:cw, 0:tn], ps[pis[0]][0:cw, 0:tn], ALU.add, r=[ack, PSK(pis[0])], w=[o2k])
                    dma("sync", G["faccT"][b, c0:c0 + cw, t0:t0 + tn], o2[0:cw, 0:tn], r=[o2k], w=[("faccT", b)])

                cbs = [(c, 128, "F", epi_d) for c in range(0, D, 128)]
                wd_all = I["w_exp_down"][l].rearrange("e k d -> (e k) d")
                eg0 = (e_ - (EG - 1)) * cfg.DE
                linear(EG * DEc, lambda b, t0, tn: G["hid"][b, :, t0:t0 + tn].rearrange("(k p) t -> p k t", p=128), ["hid"],
                       lambda k0, k1, c0, c1, eg0=eg0: wd_all[eg0 + k0:eg0 + k1, c0:c1], cbs, wcols=512)
                if "snap" in dbg and l == 1:
                    if "snap" not in dbg_out:
                        dbg_out["snap"] = nc.dram_tensor("snap", [16, 128, 8], F32, kind="ExternalOutput").ap()
                        dbg_out["snaph"] = nc.dram_tensor("snaph", [16, 128, 8], BF16, kind="ExternalOutput").ap()
                    S.barrier()
                    dma("sync", dbg_out["snap"][e_], G["faccT"][0, 0:128, 0:8], w=[("snap", e_)])
                    dma("sync", dbg_out["snaph"][e_], G["hid"][0, 0:128, 0:8], w=[("snaph", e_)])

        def gu_linear(l, e_, epi):
            HW = min(256, cfg.DE)
            wg = [A.tile([128, KC, 2 * HW], BF16) for _ in range(2)]
            ab = [A.tile([128, KC, 512], BF16) for _ in range(2)]
            ai = 0
            pc = 0
            for hi, h0 in enumerate(range(0, cfg.DE, HW)):
                w_t, wk = wg[hi % 2]
                for kk in range(0, KC, 8):
                    ke = min(KC, kk + 8)
                    dma("gpsimd", w_t[:, kk:ke, 0:HW], I["w_exp_gate"][l, e_, kk * 128:ke * 128, h0:h0 + HW].rearrange("(k p) c -> p k c", p=128), w=[(wk, "g", kk // 8)])
                    dma("gpsimd", w_t[:, kk:ke, HW:2 * HW], I["w_exp_up"][l, e_, kk * 128:ke * 128, h0:h0 + HW].rearrange("(k p) c -> p k c", p=128), w=[(wk, "u", kk // 8)])
                for b in range(NB):
                    for (t0, tn, isc) in cfg.tblocks(512):
                        a_t, ak = ab[ai % 2]
                        ai += 1
                        dma("sync", a_t[:, :, 0:tn], hT_ap(b, t0, tn), r=[("hT", b)], w=[ak])
                        for j in range(HW // 128):
                            pg, pu = (pc % 2) * 2, (pc % 2) * 2 + 1
                            pc += 1
                            for k in range(KC):
                                mm(ps[pg][:, 0:tn], w_t[:, k, j * 128:(j + 1) * 128], a_t[:, k, 0:tn], k == 0, k == KC - 1, r=[(wk, "g", k // 8), ak], w=[PSK(pg)])
                            for k in range(KC):
                                mm(ps[pu][:, 0:tn], w_t[:, k, HW + j * 128:HW + (j + 1) * 128], a_t[:, k, 0:tn], k == 0, k == KC - 1, r=[(wk, "u", k // 8), ak], w=[PSK(pu)])
                            epi(b, h0 + j * 128, 128, t0, tn, [pg, pu])

        def stage_final():
            A.reset()
            xt, xtk = A.tile([128, KC, 512]); ot, otk = A.tile([128, D])
            outs = []
            for b in range(NB):
                for (t0, tn, isc) in cfg.tblocks(512):
                    if isc:
                        continue
                    dma("sync", xt[:, :, 0:tn], G["xT"][b, :, t0:t0 + tn].rearrange("(k p) t -> p k t", p=128), r=[("xT", b)], w=[xtk])
                    for s_ in range(tn // 128):
                        for k4 in range(0, KC, 4):
                            pi = nextps()
                            for k in range(k4, min(KC, k4 + 4)):
                                tr(ps[pi][:, (k - k4) * 128:(k - k4 + 1) * 128], xt[:, k, s_ * 128:(s_ + 1) * 128], ident, r=[xtk, "cmat"], w=[PSK(pi)])
                            nk = min(KC, k4 + 4) - k4
                            cp(ot[:, k4 * 128:(k4 + nk) * 128], ps[pi][:, 0:nk * 128], r=[PSK(pi)], w=[otk], eng="scalar" if (k4 // 4) % 2 else "vector")
                        outs.append(dma("sync", out[b, t0 + s_ * 128:t0 + (s_ + 1) * 128, :], ot[:, :], r=[otk], w=[("out", b)]))
            S.finish(outs)

        stages = []
        stages.append(("s", stage_s))
        stages.append(("init", stage_init))
        for l in range(L):
            stages.append(("mod%d" % l, lambda l=l: stage_mod(l)))
            stages.append(("modulate%d" % l, lambda: stage_modulate(0, 1)))
            stages.append(("inproj%d" % l, lambda l=l: stage_inproj(l)))
            stages.append(("qk%d" % l, lambda l=l: stage_qk(l)))
            stages.append(("attn%d" % l, lambda l=l: stage_attn(l)))
            stages.append(("hgrn%d" % l, lambda l=l: stage_hgrn(l)))
            stages.append(("mlstm%d" % l, lambda l=l: stage_mlstm(l)))
            stages.append(("merge%d" % l, lambda l=l: stage_merge(l)))
            stages.append(("wout%d" % l, lambda l=l: stage_wout(l)))
            stages.append(("ln1_%d" % l, lambda l=l: stage_ln(l, 0)))
            stages.append(("moe%d" % l, lambda l=l: stage_moe(l)))
            stages.append(("ln2_%d" % l, lambda l=l: stage_ln(l, 1)))
        stages.append(("final", stage_final))
        for name, fn in stages:
            fn()
            if dbg and 'verbose' in dbg:
                print(name, {e: (len(v), sum(1 for o in v if o.signals)) for e, v in S.ops.items()}, flush=True)
            if stop_after == name:
                break

        S.barrier()
        if stop_after not in (None, "final"):
            fin = S.op("sync", lambda e: e.dma_start(out=out[0, 0:1, 0:16], in_=I["x"][0, 0:1, 0:16]), dma=True)
            S.finish([fin])
        S.emit()
        if dbg and 'verbose' in dbg:
            print('odd DMAs', len(odd_log), sorted(set(odd_log))[:20])
    return nc, dbg_out


N_CORES_USED = 4


def kernel(**inputs):
    cfg = Cfg(D=4096, LAT=2048, CTX=256, NB=1, DEPTH=2)
    nc, _ = build(cfg)
    consts = host_consts(cfg)
    in_maps = []
    for b in range(N_CORES_USED):
        m = {}
        for k, v in inputs.items():
            v = np.asarray(v)
            if k in ("x", "c", "ctx"):
                m[k] = np.ascontiguousarray(v[b:b + 1])
            else:
                m[k] = v
        m.update(consts)
        in_maps.append(m)
    res = run_bass_kernel_spmd(nc, in_maps, core_ids=list(range(N_CORES_USED)))
    return np.concatenate([np.asarray(r["out"]) for r in res.results], axis=0).astype(np.float32)
```
